# Optimizing a Trainium2 kernel written in Bass

```python
import math
import jax, jax.numpy as jnp
from jax import lax
import numpy as np

D_MODEL = 1024
BATCH = 4
SEQ = 4096
DEPTH = 2

N_EVEN = (DEPTH + 1) // 2
N_ODD = DEPTH // 2
HEAD_DIM = 64
NORM_EPS = 1e-6

RWKV_HEADS = 8
RWKV_DIM = RWKV_HEADS * HEAD_DIM
DECAY_LORA = 64
ICLR_LORA = 64
GATE_LORA = 128
RWKV_GN_EPS = 64e-5
RWKV_COLS = 3 * RWKV_DIM + DECAY_LORA + ICLR_LORA + GATE_LORA
RWKV_SPLITS = (RWKV_DIM, 2 * RWKV_DIM, 3 * RWKV_DIM, 3 * RWKV_DIM + DECAY_LORA,
               3 * RWKV_DIM + DECAY_LORA + ICLR_LORA)

SSD_HEADS = 8
SSD_DIM = SSD_HEADS * HEAD_DIM
SSD_GROUPS = 2
SSD_STATE = 128
SSD_CONV = 4
SSD_CHUNK = 128
SSD_XBC = SSD_DIM + 2 * SSD_GROUPS * SSD_STATE
SSD_COLS = SSD_DIM + SSD_XBC + SSD_HEADS
IN0_COLS = RWKV_COLS + SSD_COLS
MIX0_DIM = RWKV_DIM + SSD_DIM

ATTN_Q_HEADS = 16
ATTN_KV_HEADS = 4
GQA_GROUP = ATTN_Q_HEADS // ATTN_KV_HEADS
Q_DIM = ATTN_Q_HEADS * HEAD_DIM
KV_DIM = ATTN_KV_HEADS * HEAD_DIM
QKV_COLS = Q_DIM + 2 * KV_DIM
WINDOW = 128
ROPE_THETA = 500000.0
ROPE_DIM = HEAD_DIM // 4

FFN_DENSE = 2816
N_EXPERTS = 8
TOP_K = 2
FFN_EXPERT = 1408

kernel_name = "hybrid_rwkv7_ssd_swa_moe_block"


def rmsnorm(x, g, eps=NORM_EPS):
    xf = x.astype(jnp.float32)
    y = xf * lax.rsqrt(jnp.mean(xf * xf, axis=-1, keepdims=True) + eps)
    return (y * g.astype(jnp.float32)).astype(x.dtype)


def token_shift(u):
    return jnp.pad(u[:, :-1], ((0, 0), (1, 0), (0, 0)))


def causal_depthwise_conv(u, w, b):
    c = u.shape[-1]
    y = lax.conv_general_dilated(u, w[:, None, :], window_strides=(1,),
                                 padding=[(SSD_CONV - 1, 0)],
                                 dimension_numbers=('NWC', 'WIO', 'NWC'),
                                 feature_group_count=c)
    return y + b


def rwkv7_scan(r, decay, k, v, a_vec, b_vec):
    out_dtype = v.dtype
    bsz, _, nh, n = r.shape

    def seq_major(t):
        return jnp.moveaxis(t.astype(jnp.float32), 1, 0)

    def step(s, inp):
        r_t, w_t, k_t, v_t, a_t, bb_t = inp
        sa = jnp.einsum('bhvk,bhk->bhv', s, a_t)
        s = s * w_t[:, :, None, :] + sa[..., :, None] * bb_t[..., None, :] + v_t[..., :, None] * k_t[..., None, :]
        return s, jnp.einsum('bhvk,bhk->bhv', s, r_t)

    s0 = jnp.zeros((bsz, nh, n, n), jnp.float32)
    _, y = lax.scan(step, s0, tuple(map(seq_major, (r, decay, k, v, a_vec, b_vec))))
    return jnp.moveaxis(y, 0, 1).astype(out_dtype)


def ssd_chunked(x, dt, a, bm, cm):
    bsz, seq, nh, p = x.shape
    g, n = bm.shape[2], bm.shape[3]
    hpg = nh // g
    nc, q = seq // SSD_CHUNK, SSD_CHUNK
    f32 = jnp.float32
    xc = x.astype(f32).reshape(bsz, nc, q, g, hpg, p)
    dtc = dt.reshape(bsz, nc, q, g, hpg)
    bc = bm.astype(f32).reshape(bsz, nc, q, g, n)
    cc = cm.astype(f32).reshape(bsz, nc, q, g, n)
    cum = jnp.cumsum(dtc * a.reshape(g, hpg), axis=2)
    seg = cum[:, :, :, None] - cum[:, :, None]
    causal = jnp.tril(jnp.ones((q, q), bool))[None, None, :, :, None, None]
    ldec = jnp.exp(jnp.where(causal, seg, -jnp.inf))
    cb = jnp.einsum('bclgn,bcsgn->bclsg', cc, bc)
    wts = cb[..., None] * ldec * dtc[:, :, None]
    y_diag = jnp.einsum('bclsgh,bcsghp->bclghp', wts, xc)
    to_end = jnp.exp(cum[:, :, -1:] - cum) * dtc
    states = jnp.einsum('bcsgn,bcsghp->bcghpn', bc, xc * to_end[..., None])
    chunk_decay = jnp.exp(cum[:, :, -1])

    def step(hs, inp):
        st, dec = inp
        return hs * dec[..., None, None] + st, hs

    h0 = jnp.zeros((bsz, g, hpg, p, n), f32)
    _, h_prev = lax.scan(step, h0, (jnp.moveaxis(states, 1, 0), jnp.moveaxis(chunk_decay, 1, 0)))
    h_prev = jnp.moveaxis(h_prev, 0, 1)
    y_off = jnp.einsum('bclgn,bcghpn->bclghp', cc, h_prev) * jnp.exp(cum)[..., None]
    return (y_diag + y_off).reshape(bsz, seq, nh, p)


def rwkv_ssd_mixer(h, w_in, mu_shift, w0, w_decay_up, a0, w_iclr_up, w_gate_up, k_k, k_a, r_k,
                   gn_w, gn_b, conv_w, conv_b, dt_bias, a_log, d_skip, ssd_norm, w_out):
    bsz, seq, _ = h.shape
    f32 = jnp.float32
    proj = h @ w_in
    pa, pb = proj[..., :RWKV_COLS], proj[..., RWKV_COLS:]

    pa = pa + (token_shift(pa) - pa) * mu_shift
    r, k, v, wl, al, gl = jnp.split(pa, RWKV_SPLITS, axis=-1)
    w_raw = (w0 + jnp.tanh(wl) @ w_decay_up).astype(f32)
    decay = jnp.exp(-jnp.exp(-jax.nn.softplus(-w_raw) - 0.5))
    iclr = jax.nn.sigmoid(a0 + al @ w_iclr_up)
    g_out = jax.nn.sigmoid(gl) @ w_gate_up

    def heads(t):
        return t.reshape(bsz, seq, RWKV_HEADS, HEAD_DIM)

    kk = heads(k * k_k).astype(f32)
    kk = kk * lax.rsqrt(jnp.sum(kk * kk, axis=-1, keepdims=True) + 1e-12)
    k = k * (1.0 + (iclr - 1.0) * k_a)
    r_h, k_h, v_h, a_h = heads(r), heads(k), heads(v), heads(iclr)
    y = rwkv7_scan(r_h, heads(decay), k_h, v_h, -kk, kk * a_h).astype(f32)
    mu = jnp.mean(y, axis=-1, keepdims=True)
    var = jnp.mean(jnp.square(y - mu), axis=-1, keepdims=True)
    y = (y - mu) * lax.rsqrt(var + RWKV_GN_EPS) * gn_w.reshape(RWKV_HEADS, HEAD_DIM) + gn_b.reshape(RWKV_HEADS, HEAD_DIM)
    bonus = jnp.sum((r_h * k_h * r_k).astype(f32), axis=-1, keepdims=True) * v_h
    ya = ((y + bonus).reshape(bsz, seq, RWKV_DIM) * g_out).astype(h.dtype)

    z, xbc, dt = jnp.split(pb, (SSD_DIM, SSD_DIM + SSD_XBC), axis=-1)
    xbc = jax.nn.silu(causal_depthwise_conv(xbc, conv_w, conv_b))
    xs, bm, cm = jnp.split(xbc, (SSD_DIM, SSD_DIM + SSD_GROUPS * SSD_STATE), axis=-1)
    dt = jax.nn.softplus((dt + dt_bias).astype(f32))
    a = -jnp.exp(a_log.astype(f32))
    xs_h = xs.reshape(bsz, seq, SSD_HEADS, HEAD_DIM)
    ys = ssd_chunked(xs_h, dt, a,
                     bm.reshape(bsz, seq, SSD_GROUPS, SSD_STATE),
                     cm.reshape(bsz, seq, SSD_GROUPS, SSD_STATE))
    ys = ys + d_skip[:, None] * xs_h
    ys = ys.reshape(bsz, seq, SSD_DIM) * jax.nn.silu(z)
    ys = rmsnorm(ys.reshape(bsz, seq, SSD_GROUPS, SSD_DIM // SSD_GROUPS),
                 ssd_norm.reshape(SSD_GROUPS, SSD_DIM // SSD_GROUPS)).reshape(bsz, seq, SSD_DIM)

    return jnp.concatenate([ya, ys.astype(h.dtype)], axis=-1) @ w_out


def partial_rope(t, cos, sin):
    rot, rest = t[..., :ROPE_DIM], t[..., ROPE_DIM:]
    x1, x2 = rot[..., :ROPE_DIM // 2], rot[..., ROPE_DIM // 2:]
    shape = cos.shape[:2] + (1,) * (t.ndim - 3) + (cos.shape[-1],)
    c, s = cos.reshape(shape), sin.reshape(shape)
    return jnp.concatenate([x1 * c - x2 * s, x2 * c + x1 * s, rest], axis=-1)


def swa_sink_attention(h, cos, sin, w_qkv, b_qkv, q_norm, k_norm, sinks, w_o, b_o):
    bsz, seq, _ = h.shape
    nb = seq // WINDOW
    qkv = h @ w_qkv + b_qkv
    q, k, v = jnp.split(qkv, (Q_DIM, Q_DIM + KV_DIM), axis=-1)
    q = q.reshape(bsz, seq, ATTN_KV_HEADS, GQA_GROUP, HEAD_DIM)
    k = k.reshape(bsz, seq, ATTN_KV_HEADS, HEAD_DIM)
    v = v.reshape(bsz, seq, ATTN_KV_HEADS, HEAD_DIM)
    q = partial_rope(rmsnorm(q, q_norm), cos, sin)
    k = partial_rope(rmsnorm(k, k_norm), cos, sin)
    qb = q.reshape(bsz, nb, WINDOW, ATTN_KV_HEADS, GQA_GROUP, HEAD_DIM)
    kb = k.reshape(bsz, nb, WINDOW, ATTN_KV_HEADS, HEAD_DIM)
    vb = v.reshape(bsz, nb, WINDOW, ATTN_KV_HEADS, HEAD_DIM)

    def with_prev_block(t):
        prev = jnp.pad(t[:, :-1], ((0, 0), (1, 0), (0, 0), (0, 0), (0, 0)))
        return jnp.concatenate([prev, t], axis=2)

    kw, vw = with_prev_block(kb), with_prev_block(vb)
    qi = jnp.arange(WINDOW)[:, None]
    kj = jnp.arange(2 * WINDOW)[None, :]
    diff = WINDOW + qi - kj
    band = (diff >= 0) & (diff < WINDOW)
    blk = jnp.arange(nb)[:, None, None]
    valid = band[None] & ((blk > 0) | (kj[None] >= WINDOW))
    scores = jnp.einsum('bnqkgd,bnskd->bkgnqs', qb, kw).astype(jnp.float32) * (HEAD_DIM ** -0.5)
    scores = jnp.where(valid[None, None, None], scores, -jnp.inf)
    sink = jnp.broadcast_to(sinks.astype(jnp.float32).reshape(1, ATTN_KV_HEADS, GQA_GROUP, 1, 1, 1),
                            scores.shape[:-1] + (1,))
    probs = jax.nn.softmax(jnp.concatenate([scores, sink], axis=-1), axis=-1)[..., :-1]
    out = jnp.einsum('bkgnqs,bnskd->bnqkgd', probs.astype(vw.dtype), vw)
    return out.reshape(bsz, seq, Q_DIM) @ w_o + b_o


def swiglu(h, w_gate, w_up, w_down):
    return (jax.nn.silu(h @ w_gate) * (h @ w_up)) @ w_down


def moe_swiglu(h, w_router, w_gate, w_up, w_down):
    bsz, seq, d = h.shape
    t = h.reshape(-1, d)
    logits = (t @ w_router).astype(jnp.float32)
    top_vals, top_idx = lax.top_k(logits, TOP_K)
    top_w = jax.nn.softmax(top_vals, axis=-1)
    combine = jnp.sum(jax.nn.one_hot(top_idx, N_EXPERTS, dtype=jnp.float32) * top_w[..., None], axis=1)
    combine = combine.astype(t.dtype)
    out = jnp.zeros_like(t)
    for e in range(N_EXPERTS):
        out = out + combine[:, e:e + 1] * swiglu(t, w_gate[e], w_up[e], w_down[e])
    return out.reshape(bsz, seq, d)


def setup_inputs(seed: int = 0) -> dict:
    key = jax.random.key(seed)
    keys = jax.random.split(key, 48)
    ctr = [0]
    f32 = jnp.float32

    def nxt():
        kk = keys[ctr[0]]
        ctr[0] += 1
        return kk

    def nrm(shape, scale):
        return jax.random.normal(nxt(), shape, f32) * scale

    def gain(shape):
        return 1.0 + nrm(shape, 0.02)

    ne, no = N_EVEN, N_ODD
    x = nrm((BATCH, SEQ, D_MODEL), 1.0)
    positions = jnp.broadcast_to(jnp.arange(SEQ, dtype=jnp.int32), (BATCH, SEQ))
    ev_norm_mix = gain((ne, D_MODEL))
    ev_w_in = nrm((ne, D_MODEL, IN0_COLS), D_MODEL ** -0.5)
    ev_mu_shift = jax.random.uniform(nxt(), (ne, RWKV_COLS), f32)
    ev_w0 = nrm((ne, RWKV_DIM), 0.5)
    ev_w_decay_up = nrm((ne, DECAY_LORA, RWKV_DIM), 0.5 * DECAY_LORA ** -0.5)
    ev_a0 = nrm((ne, RWKV_DIM), 0.1)
    ev_w_iclr_up = nrm((ne, ICLR_LORA, RWKV_DIM), ICLR_LORA ** -0.5)
    ev_w_gate_up = nrm((ne, GATE_LORA, RWKV_DIM), GATE_LORA ** -0.5)
    ev_k_k = 0.85 + nrm((ne, RWKV_DIM), 0.05)
    ev_k_a = 1.0 + nrm((ne, RWKV_DIM), 0.05)
    ev_r_k = nrm((ne, RWKV_HEADS, HEAD_DIM), 0.1)
    ev_gn_w = gain((ne, RWKV_DIM))
    ev_gn_b = nrm((ne, RWKV_DIM), 0.02)
    ev_conv_w = nrm((ne, SSD_CONV, SSD_XBC), SSD_CONV ** -0.5)
    ev_conv_b = nrm((ne, SSD_XBC), 0.02)
    dt_init = jnp.exp(jax.random.uniform(nxt(), (ne, SSD_HEADS), f32, math.log(1e-3), math.log(1e-1)))
    ev_dt_bias = dt_init + jnp.log(-jnp.expm1(-dt_init))
    ev_a_log = jnp.log(jax.random.uniform(nxt(), (ne, SSD_HEADS), f32, 1.0, 16.0))
    ev_d_skip = 1.0 + nrm((ne, SSD_HEADS), 0.1)
    ev_ssd_norm = gain((ne, SSD_DIM))
    ev_w_out = nrm((ne, MIX0_DIM, D_MODEL), MIX0_DIM ** -0.5)
    ev_norm_ffn = gain((ne, D_MODEL))
    ev_ffn_gate = nrm((ne, D_MODEL, FFN_DENSE), D_MODEL ** -0.5)
    ev_ffn_up = nrm((ne, D_MODEL, FFN_DENSE), D_MODEL ** -0.5)
    ev_ffn_down = nrm((ne, FFN_DENSE, D_MODEL), FFN_DENSE ** -0.5)
    od_norm_mix = gain((no, D_MODEL))
    od_w_qkv = nrm((no, D_MODEL, QKV_COLS), D_MODEL ** -0.5)
    od_b_qkv = nrm((no, QKV_COLS), 0.02)
    od_q_norm = gain((no, HEAD_DIM))
    od_k_norm = gain((no, HEAD_DIM))
    od_sinks = nrm((no, ATTN_Q_HEADS), 1.0)
    od_w_o = nrm((no, Q_DIM, D_MODEL), Q_DIM ** -0.5)
    od_b_o = nrm((no, D_MODEL), 0.02)
    od_norm_ffn = gain((no, D_MODEL))
    od_router = nrm((no, D_MODEL, N_EXPERTS), D_MODEL ** -0.5)
    od_exp_gate = nrm((no, N_EXPERTS, D_MODEL, FFN_EXPERT), D_MODEL ** -0.5)
    od_exp_up = nrm((no, N_EXPERTS, D_MODEL, FFN_EXPERT), D_MODEL ** -0.5)
    od_exp_down = nrm((no, N_EXPERTS, FFN_EXPERT, D_MODEL), FFN_EXPERT ** -0.5)
    return {
        "x": x, "positions": positions,
        "ev_norm_mix": ev_norm_mix, "ev_w_in": ev_w_in, "ev_mu_shift": ev_mu_shift, "ev_w0": ev_w0,
        "ev_w_decay_up": ev_w_decay_up, "ev_a0": ev_a0, "ev_w_iclr_up": ev_w_iclr_up,
        "ev_w_gate_up": ev_w_gate_up, "ev_k_k": ev_k_k, "ev_k_a": ev_k_a, "ev_r_k": ev_r_k,
        "ev_gn_w": ev_gn_w, "ev_gn_b": ev_gn_b, "ev_conv_w": ev_conv_w, "ev_conv_b": ev_conv_b,
        "ev_dt_bias": ev_dt_bias, "ev_a_log": ev_a_log, "ev_d_skip": ev_d_skip, "ev_ssd_norm": ev_ssd_norm,
        "ev_w_out": ev_w_out, "ev_norm_ffn": ev_norm_ffn, "ev_ffn_gate": ev_ffn_gate,
        "ev_ffn_up": ev_ffn_up, "ev_ffn_down": ev_ffn_down,
        "od_norm_mix": od_norm_mix, "od_w_qkv": od_w_qkv, "od_b_qkv": od_b_qkv, "od_q_norm": od_q_norm,
        "od_k_norm": od_k_norm, "od_sinks": od_sinks, "od_w_o": od_w_o, "od_b_o": od_b_o,
        "od_norm_ffn": od_norm_ffn, "od_router": od_router, "od_exp_gate": od_exp_gate,
        "od_exp_up": od_exp_up, "od_exp_down": od_exp_down,
    }


def reference(x, positions,
              ev_norm_mix, ev_w_in, ev_mu_shift, ev_w0, ev_w_decay_up, ev_a0, ev_w_iclr_up,
              ev_w_gate_up, ev_k_k, ev_k_a, ev_r_k, ev_gn_w, ev_gn_b, ev_conv_w, ev_conv_b,
              ev_dt_bias, ev_a_log, ev_d_skip, ev_ssd_norm, ev_w_out, ev_norm_ffn, ev_ffn_gate,
              ev_ffn_up, ev_ffn_down,
              od_norm_mix, od_w_qkv, od_b_qkv, od_q_norm, od_k_norm, od_sinks, od_w_o, od_b_o,
              od_norm_ffn, od_router, od_exp_gate, od_exp_up, od_exp_down):
    inv_freq = ROPE_THETA ** (-jnp.arange(0, ROPE_DIM, 2, dtype=jnp.float32) / ROPE_DIM)
    ang = positions.astype(jnp.float32)[..., None] * inv_freq
    cos, sin = jnp.cos(ang).astype(x.dtype), jnp.sin(ang).astype(x.dtype)
    h = x
    for layer in range(DEPTH):
        i = layer // 2
        if layer % 2 == 0:
            h = h + rwkv_ssd_mixer(rmsnorm(h, ev_norm_mix[i]), ev_w_in[i], ev_mu_shift[i], ev_w0[i],
                                   ev_w_decay_up[i], ev_a0[i], ev_w_iclr_up[i], ev_w_gate_up[i],
                                   ev_k_k[i], ev_k_a[i], ev_r_k[i], ev_gn_w[i], ev_gn_b[i],
                                   ev_conv_w[i], ev_conv_b[i], ev_dt_bias[i], ev_a_log[i],
                                   ev_d_skip[i], ev_ssd_norm[i], ev_w_out[i])
            h = h + swiglu(rmsnorm(h, ev_norm_ffn[i]), ev_ffn_gate[i], ev_ffn_up[i], ev_ffn_down[i])
        else:
            h = h + swa_sink_attention(rmsnorm(h, od_norm_mix[i]), cos, sin, od_w_qkv[i], od_b_qkv[i],
                                       od_q_norm[i], od_k_norm[i], od_sinks[i], od_w_o[i], od_b_o[i])
            h = h + moe_swiglu(rmsnorm(h, od_norm_ffn[i]), od_router[i], od_exp_gate[i],
                               od_exp_up[i], od_exp_down[i])
    return h
```

```python
import os
import numpy as np
import concourse.bass as bass
import concourse.mybir as mybir
from concourse.bass_utils import run_bass_kernel_spmd

F32 = mybir.dt.float32
BF16 = mybir.dt.bfloat16
AF = mybir.ActivationFunctionType
ALU = mybir.AluOpType
AX = mybir.AxisListType

DM = 1024
NSLOT = 4096
NCH = 32
OWN0 = 15
NOWN = NCH - OWN0
TOWN = NOWN * 128
IN0 = 3336
FF0 = 2816
FFE = 1408
NEXP = 8
LWS = 0.6065306597126334

PC = {}
_o = 0
for _n, _w in [("g_mix0", 8), ("g_ffn0", 8), ("g_mix1", 8), ("g_ffn1", 8), ("a0", 4), ("kk", 4), ("ka", 4), ("rk", 4), ("conv_w", 32), ("conv_b", 8),
               ("b_qkv", 12), ("b_o", 8), ("flag", 1), ("dskp", 8)]:
    PC[_n] = _o
    _o += _w
NPRM = _o
RC = {}
_o = 0
for _n, _w in [("w0", 512), ("gn_w", 512), ("gn_b", 512), ("ssdn", 512), ("dtb", 8), ("alog", 8),
               ("qn", 64), ("kn", 64), ("sink", 16)]:
    RC[_n] = _o
    _o += _w
NREP = _o
C_ID, C_MUI, C_MUS, C_MUI2, C_SL, C_ONES, C_BLK, C_HSEL = 0, 128, 256, 384, 512, 640, 768, 896
C_M2 = 898
C_FRQ = 1154
NCST = 1162
I32 = mybir.dt.int32


class KB:
    def __init__(self, nc, ndma=16):
        self.nc = nc
        self.E = {'pe': nc.tensor, 'dve': nc.vector, 'act': nc.scalar, 'pool': nc.gpsimd, 'sp': nc.sync}
        self.sem = {e: nc.alloc_semaphore('s_' + e) for e in self.E}
        self.cnt = {e: 0 for e in self.E}
        self.seen = {e: {} for e in self.E}
        self.lastw = {}
        self.reads = {}
        self.ndma = ndma
        self.dsem = [nc.alloc_semaphore('d_%d' % i) for i in range(ndma)]
        self.dcnt = [0] * ndma
        self.dn = 0
        self.dnq = {}
        self.ninst = 0
        self.split = {}

    def _semof(self, key):
        return self.sem[key] if isinstance(key, str) else self.dsem[key]

    def _wait(self, e, key, val):
        if val <= 0 or self.seen[e].get(key, 0) >= val:
            return
        if key == e and e == 'pe':
            return
        self.E[e].wait_ge(self._semof(key), val)
        self.seen[e][key] = val

    def _deps(self, e, ins, outs):
        for n in ins:
            w = self.lastw.get(n)
            if w is not None:
                self._wait(e, w[0], w[1])
        for n in outs:
            w = self.lastw.get(n)
            if w is not None:
                self._wait(e, w[0], w[1])
            for (k, v) in self.reads.get(n, ()):
                self._wait(e, k, v)

    def _commit(self, key, val, ins, outs):
        for n in ins:
            self.reads.setdefault(n, []).append((key, val))
        for n in outs:
            self.lastw[n] = (key, val)
            self.reads[n] = []

    def keys(self, a):
        n = a.tensor.name
        sp = self.split.get(n)
        if sp is None:
            return [n]
        row, inner, piece = sp
        col = (a.offset % row) % inner
        ext = 1
        for (st, cn) in list(a.ap)[1:]:
            if st < inner:
                ext += (cn - 1) * st
        lo, hi = col // piece, min(col + ext - 1, inner - 1) // piece
        return ["%s#%d" % (n, i) for i in range(lo, hi + 1)]

    def op(self, e, fn, ins, outs):
        xs = [k for a in ins if a.tensor.name in self.split for k in self.keys(a)]
        ins = [k for a in ins for k in self.keys(a)]
        outs = [k for a in outs for k in self.keys(a)] + xs
        self._deps(e, ins, outs)
        inst = fn()
        self.cnt[e] += 1
        inst.then_inc(self.sem[e], 1)
        self.seen[e][e] = max(self.seen[e].get(e, 0), 0)
        self._commit(e, self.cnt[e], ins, outs)
        self.ninst += 1
        return inst

    def dma(self, e, out, in_, **kw):
        half = self.ndma // 2
        base = half if e == 'pool' else 0
        k = self.dnq.get(e, 0)
        self.dnq[e] = k + 1
        slot = base + (k % half)
        self._wait(e, slot, self.dcnt[slot])
        ins = self.keys(in_)
        outs = self.keys(out)
        self._deps(e, ins, outs)
        inst = self.E[e].dma_start(out=out, in_=in_, **kw)
        self.dcnt[slot] += 16
        inst.then_inc(self.dsem[slot], 16)
        self._commit(slot, self.dcnt[slot], ins, outs)
        self.ninst += 1
        return inst

    def barrier(self):
        for e in self.E:
            for s in range(self.ndma):
                self._wait(e, s, self.dcnt[s])
            for k in self.E:
                if k != e:
                    self._wait(e, k, self.cnt[k])
        for e in ('dve', 'act', 'pool'):
            self._wait(e, e, self.cnt[e])

    def finish(self, e='sp'):
        for s in range(self.ndma):
            self._wait(e, s, self.dcnt[s])
        for k in self.E:
            if k != e:
                self._wait(e, k, self.cnt[k])


def build_nc(debug=""):
    nc = bass.Bass("TRN2", target_bir_lowering=False)
    kb = KB(nc)

    def din(name, shape, dt=F32):
        return nc.dram_tensor(name, list(shape), dt, kind="ExternalInput").ap()

    xT_d = din("xT", [128, 8, NSLOT])
    prm_d = din("prm", [128, NPRM])
    rep_d = din("rep", [128, NREP])
    cst_d = din("cst", [128, NCST])
    bc_d = din("bc", [128, 14])
    w_in_d = din("w_in", [DM, IN0])
    wdu_d = din("wdu", [64, 512])
    wiu_d = din("wiu", [64, 512])
    wgu_d = din("wgu", [128, 512])
    w_out_d = din("w_out", [DM, DM])
    ffg_d = din("ffg", [DM, FF0])
    ffu_d = din("ffu", [DM, FF0])
    ffd_d = din("ffd", [FF0, DM])
    wqkv_d = din("wqkv", [DM, 1536])
    wo_d = din("wo", [DM, DM])
    bq_d = din("bq", [128, 1536])
    pos_d = din("pos", [128, NOWN], I32)
    wr_d = din("wr", [DM, NEXP])
    eg_d = din("eg", [NEXP, DM, FFE])
    eu_d = din("eu", [NEXP, DM, FFE])
    ed_d = din("ed", [NEXP, FFE, DM])
    out_d = nc.dram_tensor("outT", [128, 8, 2048], F32, kind="ExternalOutput").ap()
    dbg_d = None
    if debug:
        dbg_d = nc.dram_tensor("dbg", [128, 8, TOWN], F32, kind="ExternalOutput").ap()

    V = {'dve': nc.vector, 'pool': nc.gpsimd}
    rr = {'el': 0, 'ev': 0, 'ph': 0, 'pb': 0, 'dq': 0}

    def el():
        rr['el'] ^= 1
        return 'dve' if rr['el'] else 'pool'

    def ev():
        rr['ev'] ^= 1
        return 'dve' if rr['ev'] else 'act'

    def aps(*xs):
        return [x for x in xs if hasattr(x, 'tensor')]

    def mm(out, lhsT, rhs, start=True, stop=True):
        kb.op('pe', lambda: nc.tensor.matmul(out, lhsT=lhsT, rhs=rhs, start=start, stop=stop), [lhsT, rhs], [out])

    def tr(out, in_, ident):
        kb.op('pe', lambda: nc.tensor.transpose(out, in_, ident), [in_, ident], [out])

    def tt(e, out, a, b, op):
        kb.op(e, lambda: V[e].tensor_tensor(out=out, in0=a, in1=b, op=op), [a, b], [out])

    def ts(e, out, a, s1, op0, s2=None, op1=None):
        if op1 is None:
            kb.op(e, lambda: V[e].tensor_scalar(out=out, in0=a, scalar1=s1, scalar2=None, op0=op0), aps(a, s1), [out])
        else:
            kb.op(e, lambda: V[e].tensor_scalar(out=out, in0=a, scalar1=s1, scalar2=s2, op0=op0, op1=op1),
                  aps(a, s1, s2), [out])

    def stt(e, out, a, s, b, op0, op1):
        e = 'dve'
        kb.op(e, lambda: V[e].scalar_tensor_tensor(out=out, in0=a, scalar=s, in1=b, op0=op0, op1=op1),
              aps(a, s, b), [out])

    def act(out, in_, func, bias=None, scale=1.0):
        if bias is None:
            kb.op('act', lambda: nc.scalar.activation(out=out, in_=in_, func=func, scale=scale), aps(in_, scale), [out])
        else:
            kb.op('act', lambda: nc.scalar.activation(out=out, in_=in_, func=func, bias=bias, scale=scale),
                  aps(in_, bias, scale), [out])

    def rsq(x):
        act(x, x, AF.Ln)
        act(x, x, AF.Exp, scale=-0.5)

    def cp(e, out, in_):
        if e == 'act':
            kb.op('act', lambda: nc.scalar.copy(out=out, in_=in_), [in_], [out])
        else:
            kb.op(e, lambda: V[e].tensor_copy(out=out, in_=in_), [in_], [out])

    def rsum(e, out, in_):
        kb.op(e, lambda: V[e].reduce_sum(out=out, in_=in_, axis=AX.X), [in_], [out])

    def dma(out, in_, q=None):
        kb.dma(q or 'sp', out, in_)

    def sb(name, shape, dt=F32):
        return nc.alloc_sbuf_tensor(name, list(shape), dt)

    BANKS = [nc.alloc_psum_tensor("PS%d" % i, [128, 512], F32) for i in range(8)]
    for b_ in BANKS:
        kb.split[b_.name] = (512, 512, 512)
    def pb():
        rr['pb'] = (rr['pb'] + 1) % 8
        return BANKS[rr['pb']][:, :]

    def ph():
        return pb()[:, 0:256]

    PRM = sb("PRM", [128, NPRM])
    CST = sb("CST", [128, NCST])
    mix_scr = nc.dram_tensor("mix_scr", [128, 8, TOWN], BF16).ap()
    dma(PRM[:], prm_d)
    dma(CST[:], cst_d)
    IDENT = CST[:, C_ID:C_ID + 128]
    MUI = CST[:, C_MUI:C_MUI + 128]
    TRI2 = CST[:, C_MUI:C_MUI + 256]
    MU2 = CST[:, C_MUS:C_MUS + 256]
    SLm = CST[:, C_SL:C_SL + 128]
    ONES = CST[:, C_ONES:C_ONES + 128]
    BLK = CST[:, C_BLK:C_BLK + 128]
    HSEL = CST[:, C_HSEL:C_HSEL + 2]

    def P_(n, w=1, o=0):
        return PRM[:, PC[n] + o:PC[n] + o + w]

    def R_(n, w, o=0):
        return REP[:, RC[n] + o:RC[n] + o + w]

    p1 = []

    def sb1(name, shape, dt=F32):
        t = nc.sbuf_tensor(name, list(shape), dt)
        h = t.__enter__()
        p1.append(t)
        return h

    REP = sb1("REP", [128, NREP])
    dma(REP[:], rep_d)
    WIN = sb1("WIN", [128, 8, IN0], BF16)
    for kc in range(8):
        kb.dma('pool', WIN[:, kc, :], w_in_d[kc * 128:(kc + 1) * 128, :])
    WDU = sb1("WDU", [64, 512], BF16)
    kb.dma('pool', WDU[:], wdu_d)
    WIU = sb1("WIU", [128, 512], BF16)
    kb.dma('pool', WIU[64:128, :], wiu_d)
    WGU = sb1("WGU", [128, 512], BF16)
    kb.dma('pool', WGU[:], wgu_d)
    MUC = sb1("MUC", [128, 14])
    dma(MUC[:], bc_d)
    IDB = sb1("IDB", [128, 128], BF16)
    cp('dve', IDB[:], IDENT)
    AREP = sb1("AREP", [128, 8])
    act(AREP[:], R_("alog", 8), AF.Exp)
    ts('dve', AREP[:], AREP[:], -1.0, ALU.mult)

    XT = sb1("XT0", [128, 8, 128])
    RSTD = sb1("RSTD", [128, 128])
    XN = sb1("XN", [128, 8, 128], BF16)
    PA = sb1("PA0", [128, 14, 129])
    XB = sb1("XB0", [128, 8, 131])
    DTMP = sb1("DTMP", [128, 14, 128])
    PL = DTMP
    SQ = DTMP
    CT2 = sb1("CT2", [128, 4, 128])
    XC = sb1("XC", [128, 4, 128])
    TW = sb1("TW", [64, 128], BF16)
    ALB = sb1("ALB", [128, 128], BF16)
    SGL = sb1("SGL", [128, 128], BF16)
    SG = sb1("SG", [128, 512])
    ICLR = sb1("ICLR", [128, 4, 128])
    KQ = sb1("KQ", [128, 4, 128])
    SQK = sb1("SQK", [128, 4, 128])
    RN = sb1("RN", [128, 4, 128])
    KKn = sb1("KKn", [128, 4, 128])
    KM = sb1("KM", [128, 4, 128])
    T1 = SQK
    Bv = KQ
    RKR = RN
    E1 = sb1("E1", [128, 4, 128])
    E0 = sb1("E0", [128, 4, 128])
    EI = sb1("EI", [128, 4, 128])
    def dbl(name, shape, dt=F32):
        return [sb1("%s_%d" % (name, i), shape, dt) for i in range(2)]
    AR = dbl("AR", [128, 4, 2, 128], BF16)
    KT = dbl("KT", [128, 4, 128], BF16)
    BT = dbl("BT", [128, 4, 128], BF16)
    VTOK = dbl("VTOK", [128, 4, 128], BF16)
    A0T = dbl("A0T", [128, 4, 128], BF16)
    R0T = dbl("R0T", [128, 4, 128], BF16)
    KTT = dbl("KTT", [128, 4, 128], BF16)
    BTT = dbl("BTT", [128, 4, 128], BF16)
    G127 = dbl("G127", [128, 4])
    GOUT = dbl("GOUT", [128, 512])
    BON = dbl("BON", [128, 8])
    ZS = dbl("ZS", [128, 512])
    XSTOK = dbl("XSTOK", [128, 512])
    BTOK = dbl("BTOK", [128, 256])
    XBC = dbl("XBC", [128, 4, 128])
    DT = dbl("DT", [128, 8])
    DA = dbl("DA", [128, 8])
    CUMC = dbl("CUMC", [128, 8])
    NN = [[sb1("NN%d_%d" % (hb, i), [128, 4, 128], BF16) for i in range(2)] for hb in range(2)]
    PP = [[sb1("PP%d_%d" % (hb, i), [128, 4, 128], BF16) for i in range(2)] for hb in range(2)]
    TTb = [sb1("TTb%d" % hb, [128, 4, 128], BF16) for hb in range(2)]
    ARB = [sb1("ARB%d" % hb, [128, 4, 128], BF16) for hb in range(2)]
    KAb = [sb1("KAb%d" % hb, [128, 4, 256], BF16) for hb in range(2)]
    AXb = [sb1("AXb%d" % hb, [128, 4, 128], BF16) for hb in range(2)]
    WW = sb1("WW", [128, 4, 128], BF16)
    UU = sb1("UU", [128, 4, 128], BF16)
    QTOK = sb1("QTOK", [128, 4, 128], BF16)
    Y0 = sb1("Y0", [128, 4, 128])
    QT = sb1("QT", [128, 4, 128], BF16)
    MT = sb1("MT", [128, 4, 128], BF16)
    LL = sb1("LL", [128, 4, 64])
    HS = [sb1("HS%d" % i, [128, 4, 64]) for i in range(2)]
    HSb = [sb1("HSb%d" % i, [128, 4, 64], BF16) for i in range(2)]
    ST1 = sb1("ST1", [128, 8])
    ST2 = sb1("ST2", [128, 8])
    ST3 = sb1("ST3", [128, 8])
    MIXTOK = sb1("MIXTOK", [128, 1024])
    YTOK = MIXTOK[:, 0:512]
    YS = MIXTOK[:, 512:1024]
    MXS = sb1("MXS", [128, 8, 128], BF16)
    ECUM = sb1("ECUM", [128, 8])
    CDEC = sb1("CDEC", [128, 8])
    TE = sb1("TE", [128, 8])
    CBM = sb1("CBM", [128, 2, 128])
    TD4 = sb1("TD4", [128, 4, 128])
    SEG4 = TD4
    LD4 = sb1("LD4", [128, 4, 128])
    WT4 = LD4
    XW = sb1("XW", [128, 512])
    JUNK = sb1("JUNK", [128, 64], BF16)
    YOFF4 = sb1("YOFF4", [128, 4, 64])
    ST4 = sb1("ST4", [128, 2])
    HSS = [sb1("HSS%d" % i, [128, 8, 64]) for i in range(2)]

    kb.op('dve', lambda: nc.vector.memset(HS[0][:], 0.0), [], [HS[0][:]])
    kb.op('dve', lambda: nc.vector.memset(HSb[0][:], 0.0), [], [HSb[0][:]])
    kb.op('dve', lambda: nc.vector.memset(HSS[0][:], 0.0), [], [HSS[0][:]])
    kb.op('pool', lambda: nc.gpsimd.memset(PA[:], 0.0), [], [PA[:]])
    kb.op('pool', lambda: nc.gpsimd.memset(XB[:], 0.0), [], [XB[:]])

    def fgrp(g):
        if g < 14:
            return g * 128
        return 1792 + 512 + (g - 14) * 128

    def trb(out, in_):
        mm(out, in_, IDB[:])

    def norm_block(c):
        act(SQ[:, 0:8, :], XT[:], AF.Square)
        ps = ph()
        for kc in range(8):
            mm(ps[:, 0:128], ONES, SQ[:, kc, :], start=(kc == 0), stop=(kc == 7))
        ts('dve', RSTD[:], ps[:, 0:128], 1.0 / DM, ALU.mult, 1e-6, ALU.add)
        rsq(RSTD[:])
        tt('pool', SQ[:, 0:8, :], XT[:], P_("g_mix0", 8).unsqueeze(2).to_broadcast([128, 8, 128]), ALU.mult)
        tt('pool', XN[:], SQ[:, 0:8, :], RSTD[:].unsqueeze(1).to_broadcast([128, 8, 128]), ALU.mult)

    def stage1(c):
        own = c >= OWN0
        par = c % 2
        pa, xb = PA, XB
        R4 = PL[:, 0:4, :]
        K4 = PL[:, 4:8, :]
        V4 = PL[:, 8:12, :]
        cp('pool', pa[:, :, 0:1], pa[:, :, 128:129])
        cp('pool', xb[:, :, 0:3], xb[:, :, 128:131])
        for g0 in range(0, 22, 4):
            gs = list(range(g0, min(g0 + 4, 22)))
            ps = pb()
            for i, g in enumerate(gs):
                co = fgrp(g)
                for kc in range(8):
                    mm(ps[:, i * 128:(i + 1) * 128], WIN[:, kc, co:co + 128], XN[:, kc, :], start=(kc == 0), stop=(kc == 7))
            pag = [g for g in gs if g < 14]
            xbg = [g for g in gs if g >= 14]
            if pag:
                i0 = gs.index(pag[0])
                cp('act', pa[:, pag[0]:pag[-1] + 1, 1:129], ps[:, i0 * 128:(i0 + len(pag)) * 128].rearrange("p (g n) -> p g n", g=len(pag)))
            if xbg:
                i0 = gs.index(xbg[0])
                cp('act', xb[:, xbg[0] - 14:xbg[-1] - 13, 3:131], ps[:, i0 * 128:(i0 + len(xbg)) * 128].rearrange("p (g n) -> p g n", g=len(xbg)))
            yield
        psz = pb()
        for kc in range(8):
            mm(psz[:, :], XN[:, kc, :], WIN[:, kc, 1792:1792 + 512], start=(kc == 0), stop=(kc == 7))
        if own:
            act(ZS[par][:], psz[:, :], AF.Silu)
        psd = ph()
        for kc in range(8):
            mm(psd[:, 0:8], XN[:, kc, :], WIN[:, kc, IN0 - 8:IN0], start=(kc == 0), stop=(kc == 7))
        tt('dve', DT[par][:], psd[:, 0:8], R_("dtb", 8), ALU.add)
        if c + 1 < NCH:
            dma(XT[:], xT_d[:, :, (c + 1) * 128:(c + 2) * 128])
        yield

        def prep_rwkv():
            for (ga, gb) in ((12, 14), (4, 8), (0, 4), (8, 12)):
                tt('pool', DTMP[:, ga:gb, :], pa[:, ga:gb, 0:128], pa[:, ga:gb, 1:129], ALU.subtract)
                tt('pool', DTMP[:, ga:gb, :], DTMP[:, ga:gb, :], MUC[:, ga:gb].unsqueeze(2).to_broadcast([128, gb - ga, 128]), ALU.mult)
                tt('pool', PL[:, ga:gb, :], DTMP[:, ga:gb, :], pa[:, ga:gb, 1:129], ALU.add)
                if ga == 12:
                    act(TW[:], PL[0:64, 12, :], AF.Tanh)
                    cp('dve', ALB[64:128, :], PL[64:128, 12, :])
                    act(SGL[:], PL[:, 13, :], AF.Sigmoid)
                if ga == 4:
                    tt('pool', KQ[:], K4, P_('kk', 4).unsqueeze(2).to_broadcast([128, 4, 128]), ALU.mult)
                    tt('pool', SQK[:], KQ[:], KQ[:], ALU.mult)
                yield
            psw = pb()
            mm(psw[:, :], TW[:], WDU[:])
            tt('dve', SG[:], psw[:, :], R_("w0", 512), ALU.add)
            act(SG[:], SG[:], AF.Sigmoid)
            if own:
                psg = pb()
                mm(psg[:, :], SGL[:], WGU[:])
                cp('act', GOUT[par][:], psg[:, :])
            psi = pb()
            for p in range(4):
                mm(psi[:, p * 128:(p + 1) * 128], WIU[64:128, p * 128:(p + 1) * 128], ALB[64:128, :])
            for p in range(4):
                act(ICLR[:, p, :], psi[:, p * 128:(p + 1) * 128], AF.Sigmoid, bias=P_("a0", 1, p))
            pss = pb()
            mm(pss[:, :], BLK, SQK[:].rearrange("p a b -> p (a b)"))
            ts('dve', RN[:].rearrange("p a b -> p (a b)"), pss[:, :], 1e-12, ALU.add)
            yield
            rsq(RN[:].rearrange("p a b -> p (a b)"))
            ts('pool', T1[:], ICLR[:], -1.0, ALU.add)
            tt('pool', T1[:], T1[:], P_('ka', 4).unsqueeze(2).to_broadcast([128, 4, 128]), ALU.mult)
            tt('pool', T1[:], T1[:], K4, ALU.mult)
            for p in range(4):
                psc = ph()
                mm(psc[:, 0:256], SG[:, p * 128:(p + 1) * 128], TRI2)
                act(E1[:, p, :], psc[:, 0:128], AF.Exp, scale=-LWS)
                act(E0[:, p, :], psc[:, 128:256], AF.Exp, scale=-LWS)
                act(EI[:, p, :], psc[:, 0:128], AF.Exp, scale=LWS)
            cp('act', G127[par][:], E1[:, :, 127])
            yield
            tt('pool', KKn[:], KQ[:], RN[:], ALU.mult)
            tt('pool', KM[:], T1[:], K4, ALU.add)
            tt('pool', AR[par][:, :, 1, :], R4, E1[:], ALU.mult)
            yield
            tt('pool', Bv[:], KKn[:], ICLR[:], ALU.mult)
            stt('dve', AR[par][:, :, 0, :], KKn[:], -1.0, E0[:], ALU.mult, ALU.mult)
            tt('pool', KT[par][:], KM[:], EI[:], ALU.mult)
            tt('pool', BT[par][:], Bv[:], EI[:], ALU.mult)
            if own:
                tt('pool', RKR[:], R4, KM[:], ALU.mult)
                tt('pool', RKR[:], RKR[:], P_('rk', 4).unsqueeze(2).to_broadcast([128, 4, 128]), ALU.mult)
            yield
            for p in range(4):
                pst = ph()
                tr(pst[:, 0:128], V4[:, p, :], IDENT)
                cp('act', VTOK[par][:, p, :], pst[:, 0:128])
                for src, dst in ((AR[par][:, p, 0, :], A0T[par]), (AR[par][:, p, 1, :], R0T[par]), (KT[par][:, p, :], KTT[par]),
                                 (BT[par][:, p, :], BTT[par])):
                    pst = ph()
                    trb(pst[:, 0:128], src)
                    cp(ev(), dst[:, p, :], pst[:, 0:128])
                if p % 2 == 1:
                    yield
            if own:
                psb = ph()
                for p in range(4):
                    mm(psb[:, 2 * p:2 * p + 2], RKR[:, p, :], HSEL)
                cp('act', BON[par][:], psb[:, 0:8])

        def prep_ssd():
            CW3 = PRM[:, PC["conv_w"]:PC["conv_w"] + 32].rearrange("p (g j) -> p g j", j=4)
            for half in range(2):
                gsl = slice(4 * half, 4 * half + 4)
                c0 = XC[:, :, :] if half == 0 else XBC[par][:, :, :]
                c1 = CT2[:, :, :]
                tt('pool', c0, xb[:, gsl, 0:128], CW3[:, gsl, 0].unsqueeze(2).to_broadcast([128, 4, 128]), ALU.mult)
                for j in range(1, 4):
                    tt('pool', c1, xb[:, gsl, j:j + 128], CW3[:, gsl, j].unsqueeze(2).to_broadcast([128, 4, 128]), ALU.mult)
                    tt('pool', c0, c0, c1, ALU.add)
                yield
            for g in range(8):
                dst = XC[:, g, :] if g < 4 else XBC[par][:, g - 4, :]
                act(dst, dst, AF.Silu, bias=P_("conv_b", 1, g))
            act(DT[par][:], DT[par][:], AF.Exp)
            act(DT[par][:], DT[par][:], AF.Ln, bias=1.0)
            tt('dve', DA[par][:], DT[par][:], AREP[:], ALU.mult)
            yield
            yield
            psc = ph()
            mm(psc[:, 0:8], MUI, DA[par][:])
            cp('act', CUMC[par][:], psc[:, 0:8])
            for g in range(4):
                pst = ph()
                tr(pst[:, 0:128], XC[:, g, :], IDENT)
                cp('act', XSTOK[par][:, g * 128:(g + 1) * 128], pst[:, 0:128])
            for g in range(2):
                pst = ph()
                tr(pst[:, 0:128], XBC[par][:, g, :], IDENT)
                cp('act', BTOK[par][:, g * 128:(g + 1) * 128], pst[:, 0:128])
            yield

        for _ in parallel(prep_rwkv(), prep_ssd()):
            yield
        if c + 1 < NCH:
            norm_block(c + 1)
        yield

    def rwkv_hb(c, hb):
        own = c >= OWN0
        par = c % 2
        ar, kt, bt, vtok, a0t, r0t = AR[par], KT[par], BT[par], VTOK[par], A0T[par], R0T[par]
        heads = list(range(4 * hb, 4 * hb + 4))
        nn, pp, ttb, arb, kab, axb = NN[hb], PP[hb], TTb[hb], ARB[hb], KAb[hb], AXb[hb]

        def h8(t):
            return t[:].rearrange("p a (h d) -> p (a h) d", h=2)[:, 4 * hb:4 * hb + 4, :]
        def par_view(t, q):
            return t[:].rearrange("p (a b) n -> p a b n", b=2)[:, :, q, :]
        for q in range(2):
            bk1 = pb()
            bk2 = pb()
            bk3 = pb()
            for i in range(2):
                h = heads[2 * i + q]
                p, hr = h // 2, slice(64 * (h % 2), 64 * (h % 2) + 64)
                rhs = ar[hr, p, :, :].rearrange("p a b -> p (a b)")
                mm(bk1[:, i * 256:(i + 1) * 256], bt[hr, p, :], rhs)
                mm(bk2[:, i * 256:(i + 1) * 256], kt[hr, p, :], rhs)
                mm(bk3[:, i * 128:(i + 1) * 128], ar[hr, p, 0, :], bt[hr, p, :])
            v1 = bk1.rearrange("p (h t n) -> p h t n", h=2, t=2)
            tt('dve', par_view(pp[0], q), v1[:, :, 0, :], MU2[:, 0:128].unsqueeze(1).to_broadcast([128, 2, 128]), ALU.mult)
            tt('dve', par_view(arb, q), v1[:, :, 1, :], MU2[:, 128:256].unsqueeze(1).to_broadcast([128, 2, 128]), ALU.mult)
            tt('dve', par_view(kab, q), bk2.rearrange("p (h n) -> p h n", h=2),
               MU2.unsqueeze(1).to_broadcast([128, 2, 256]), ALU.mult)
            tt('dve', par_view(nn[0], q), bk3[:, 0:256].rearrange("p (h n) -> p h n", h=2),
               SLm.unsqueeze(1).to_broadcast([128, 2, 128]), ALU.mult)
            yield
        tt('dve', ttb[:], pp[0][:], IDENT.unsqueeze(1).to_broadcast([128, 4, 128]), ALU.add)
        cp('act', axb[:, :, 0:64], h8(a0t))
        bkx = pb()
        for i, h in enumerate(heads):
            p, hr = h // 2, slice(64 * (h % 2), 64 * (h % 2) + 64)
            mm(bkx[:, i * 64:(i + 1) * 64], kab[:, i, 0:128], vtok[:, p, hr])
        cp('act', axb[:, :, 64:128], bkx[:, 0:256].rearrange("p (h d) -> p h d", h=4))
        yield
        ci = 0
        for lvl in range(1, 7):
            ncur, pcur, nnx, pnx = nn[ci], pp[ci], nn[1 - ci], pp[1 - ci]
            bka = pb()
            for i in range(4):
                mm(bka[:, i * 128:(i + 1) * 128], pcur[:, i, :], ncur[:, i, :])
            cp('act', nnx[:], bka.rearrange("p (h n) -> p h n", h=4))
            if lvl < 6:
                bkb = pb()
                for i in range(4):
                    mm(bkb[:, i * 128:(i + 1) * 128], ncur[:, i, :], pcur[:, i, :])
                cp('act', pnx[:], bkb.rearrange("p (h n) -> p h n", h=4))
            yield
            bkc = pb()
            for i in range(4):
                mm(bkc[:, i * 128:(i + 1) * 128], nnx[:, i, :], ttb[:, i, :])
            tt('dve', ttb[:], ttb[:], bkc.rearrange("p (h n) -> p h n", h=4), ALU.add)
            yield
            ci = 1 - ci
        bkw = pb()
        for i in range(4):
            mm(bkw[:, i * 128:(i + 1) * 128], ttb[:, i, :], axb[:, i, :])
        vw = bkw.rearrange("p (h n) -> p h n", h=4)
        cp('act', h8(WW), vw[:, :, 0:64])
        cp('dve', h8(UU), vw[:, :, 64:128])
        yield
        if own:
            bkq = pb()
            for i, h in enumerate(heads):
                p, hr = h // 2, slice(64 * (h % 2), 64 * (h % 2) + 64)
                mm(bkq[:, i * 128:i * 128 + 64], arb[:, i, :], WW[:, p, hr])
                mm(bkq[:, i * 128 + 64:(i + 1) * 128], arb[:, i, :], UU[:, p, hr], start=True, stop=False)
                mm(bkq[:, i * 128 + 64:(i + 1) * 128], kab[:, i, 128:256], vtok[:, p, hr], start=False, stop=True)
            vq = bkq.rearrange("p (h n) -> p h n", h=4)
            tt('dve', h8(QTOK), vq[:, :, 0:64], h8(r0t), ALU.add)
            cp('act', h8(Y0), vq[:, :, 64:128])
            yield

    def rwkv_pairs(c):
        own = c >= OWN0
        par = c % 2
        ar, kt, bt, vtok, a0t, r0t, ktt, btt = AR[par], KT[par], BT[par], VTOK[par], A0T[par], R0T[par], KTT[par], BTT[par]
        hs, hsn = HS[par], HS[1 - par]
        hsb, hsbn = HSb[par], HSb[1 - par]
        g127 = G127[par]
        bkm = pb()
        for p in range(4):
            mm(bkm[:, p * 128:(p + 1) * 128], IDB[:], IDB[:], start=True, stop=False)
            mm(bkm[:, p * 128:(p + 1) * 128], WW[:, p, :], btt[:, p, :], start=False, stop=True)
        tt('dve', MT[:], bkm.rearrange("p (a n) -> p a n", a=4), BLK.unsqueeze(1).to_broadcast([128, 4, 128]), ALU.mult)
        bkl = pb()
        for p in range(4):
            mm(bkl[:, p * 128:(p + 1) * 128], btt[:, p, :], UU[:, p, :], start=True, stop=False)
            mm(bkl[:, p * 128:(p + 1) * 128], ktt[:, p, :], vtok[:, p, :], start=False, stop=True)
        vl = bkl.rearrange("p (a n) -> p a n", a=4)
        for hh in range(2):
            hr = slice(64 * hh, 64 * hh + 64)
            tt('dve', LL[hr, :, :], vl[hr, :, 64 * hh:64 * hh + 64], g127[hr, :].unsqueeze(2).to_broadcast([64, 4, 64]), ALU.mult)
        yield
        if own:
            bkt = pb()
            for p in range(4):
                trb(bkt[:, p * 128:(p + 1) * 128], QTOK[:, p, :])
            cp('act', QT[:], bkt.rearrange("p (a n) -> p a n", a=4))
            Y4 = YTOK.rearrange("p (a h d) -> p a h d", a=4, h=2)
            for hh in range(2):
                hr = slice(64 * hh, 64 * hh + 64)
                bky = pb()
                for p in range(4):
                    mm(bky[:, p * 64:(p + 1) * 64], QT[hr, p, :], hsb[hr, p, :])
                tt('dve', Y4[:, :, hh, :], bky[:, 0:256].rearrange("p (a d) -> p a d", a=4), Y0[:, :, 64 * hh:64 * hh + 64], ALU.add)
            yield
        bkh = pb()
        for p in range(4):
            mm(bkh[:, p * 64:(p + 1) * 64], MT[:, p, :], hsb[:, p, :])
        tt('dve', hsn[:], bkh[:, 0:256].rearrange("p (a d) -> p a d", a=4), g127[:].unsqueeze(2).to_broadcast([128, 4, 64]), ALU.mult)
        tt('dve', hsn[:], hsn[:], LL[:], ALU.add)
        yield
        if c == 15:
            ts('dve', hsn[:], hsn[:], P_("flag"), ALU.mult)
        cp('act', hsbn[:], hsn[:])
        if own:
            Y3 = YTOK.rearrange("p (h d) -> p h d", h=8)
            rsum('dve', ST1[:], Y3)
            for h in range(8):
                kb.op('act', lambda: nc.scalar.activation(out=JUNK[:], in_=YTOK[:, h * 64:(h + 1) * 64], func=AF.Square,
                                                          accum_out=ST2[:, h:h + 1]), [YTOK], [JUNK[:], ST2[:]])
            ts('dve', ST1[:], ST1[:], 1.0 / 64, ALU.mult)
            tt('dve', ST3[:], ST1[:], ST1[:], ALU.mult)
            stt('dve', ST2[:], ST2[:], 1.0 / 64, ST3[:], ALU.mult, ALU.subtract)
            ts('dve', ST2[:], ST2[:], 64e-5, ALU.add)
            rsq(ST2[:])
            tt('dve', Y3, Y3, ST1[:].unsqueeze(2).to_broadcast([128, 8, 64]), ALU.subtract)
            tt('dve', Y3, Y3, ST2[:].unsqueeze(2).to_broadcast([128, 8, 64]), ALU.mult)
            tt('pool', YTOK, YTOK, R_("gn_w", 512), ALU.mult)
            tt('pool', YTOK, YTOK, R_("gn_b", 512), ALU.add)
            yield
            for h in range(8):
                p, hr = h // 2, slice(64 * (h % 2), 64 * (h % 2) + 64)
                stt('dve', YTOK[:, h * 64:(h + 1) * 64], vtok[:, p, hr], BON[par][:, h:h + 1], YTOK[:, h * 64:(h + 1) * 64],
                    ALU.mult, ALU.add)
            tt('pool', MIXTOK[:, 0:512], YTOK, GOUT[par][:], ALU.mult)
            yield

    def stage2_ssd(c):
        own = c >= OWN0
        par = c % 2
        dt_, da, cumc, xstok, btok, xbc = DT[par], DA[par], CUMC[par], XSTOK[par], BTOK[par], XBC[par]
        if own:
            act(ECUM[:], cumc[:], AF.Exp)
            for g in range(2):
                pcb = ph()
                mm(pcb[:, 0:128], xbc[:, g, :], xbc[:, 2 + g, :])
                tt('dve', CBM[:, g, :], pcb[:, 0:128], MUI, ALU.mult)
        hss, hssn = HSS[par], HSS[1 - par]
        for g in range(2):
            hs4 = slice(4 * g, 4 * g + 4)
            xs4 = xstok[:, g * 256:(g + 1) * 256].rearrange("p (h d) -> p h d", h=4)
            tt('dve', TD4[:], MUI.unsqueeze(1).to_broadcast([128, 4, 128]), da[:, hs4].unsqueeze(2).to_broadcast([128, 4, 128]), ALU.mult)
            bkr = pb()
            mm(bkr[:, :], ONES, TD4[:].rearrange("p h n -> p (h n)"))
            vr = bkr.rearrange("p (h n) -> p h n", h=4)
            tt('dve', SEG4[:], vr, cumc[:, hs4].unsqueeze(2).to_broadcast([128, 4, 128]), ALU.subtract)
            ts('dve', SEG4[:], SEG4[:], 0.0, ALU.add, 0.0, ALU.min)
            act(LD4[:], SEG4[:], AF.Exp)
            act(CDEC[:, hs4], vr[:, :, 127], AF.Exp)
            tt('dve', TE[:, hs4], LD4[:, :, 127], dt_[:, hs4], ALU.mult)
            tt('dve', XW[:, g * 256:(g + 1) * 256].rearrange("p (h d) -> p h d", h=4), xs4,
               TE[:, hs4].unsqueeze(2).to_broadcast([128, 4, 64]), ALU.mult)
            yield
            if own:
                tt('dve', WT4[:], LD4[:], dt_[:, hs4].unsqueeze(2).to_broadcast([128, 4, 128]), ALU.mult)
                tt('dve', WT4[:], WT4[:], CBM[:, g, :].unsqueeze(1).to_broadcast([128, 4, 128]), ALU.mult)
                bky = pb()
                for i in range(4):
                    h = 4 * g + i
                    mm(bky[:, i * 128:i * 128 + 64], WT4[:, i, :], xstok[:, h * 64:(h + 1) * 64])
                    mm(bky[:, i * 128 + 64:(i + 1) * 128], xbc[:, 2 + g, :], hss[:, h, :])
                vy = bky.rearrange("p (h n) -> p h n", h=4)
                tt('dve', YOFF4[:], vy[:, :, 64:128], ECUM[:, hs4].unsqueeze(2).to_broadcast([128, 4, 64]), ALU.mult)
                tt('dve', YS[:, g * 256:(g + 1) * 256].rearrange("p (h d) -> p h d", h=4), vy[:, :, 0:64], YOFF4[:], ALU.add)
                yield
            bks = pb()
            for i in range(4):
                h = 4 * g + i
                mm(bks[:, i * 64:(i + 1) * 64], btok[:, g * 128:(g + 1) * 128], XW[:, h * 64:(h + 1) * 64])
            tt('dve', hssn[:, hs4, :], hss[:, hs4, :], CDEC[:, hs4].unsqueeze(2).to_broadcast([128, 4, 64]), ALU.mult)
            tt('dve', hssn[:, hs4, :], hssn[:, hs4, :], bks[:, 0:256].rearrange("p (h d) -> p h d", h=4), ALU.add)
            yield
        if c == 15:
            ts('dve', hssn[:], hssn[:], P_("flag"), ALU.mult)
        if own:
            for h in range(8):
                stt('dve', YS[:, h * 64:(h + 1) * 64], xstok[:, h * 64:(h + 1) * 64], P_("dskp", 1, h), YS[:, h * 64:(h + 1) * 64],
                    ALU.mult, ALU.add)
            tt('dve', YS, YS, ZS[par][:], ALU.mult)
            tt('pool', XW[:], YS, YS, ALU.mult)
            rsum('dve', ST4[:], XW[:].rearrange("p (g d) -> p g d", g=2))
            ts('dve', ST4[:], ST4[:], 1.0 / 256, ALU.mult, 1e-6, ALU.add)
            rsq(ST4[:])
            for g in range(2):
                stt('dve', MIXTOK[:, 512 + g * 256:512 + (g + 1) * 256], YS[:, g * 256:(g + 1) * 256], ST4[:, g:g + 1],
                    R_("ssdn", 256, g * 256), ALU.mult, ALU.mult)
            yield

    def stage2_out(c):
        if c >= OWN0:
            oc = c - OWN0
            for j in range(8):
                pst = ph()
                tr(pst[:, 0:128], MIXTOK[:, j * 128:(j + 1) * 128], IDENT)
                cp('act', MXS[:, j, :], pst[:, 0:128])
            dma(mix_scr[:, :, oc * 128:(oc + 1) * 128], MXS[:])
        return
        yield

    def chain(*gens):
        for g in gens:
            for _ in g:
                yield

    def parallel(*gens):
        gens = list(gens)
        while gens:
            for g in list(gens):
                try:
                    next(g)
                except StopIteration:
                    gens.remove(g)
            yield

    def weighted(items):
        st = [[g, 0, float(n) * sp] for (g, n, sp) in items]
        while st:
            st.sort(key=lambda x: (x[1] + 1) / x[2])
            x = st[0]
            try:
                next(x[0])
                x[1] += 1
            except StopIteration:
                st.remove(x)

    dma(XT[:], xT_d[:, :, 0:128])
    norm_block(0)
    for c in range(NCH + 1):
        items = []
        if c >= 1:
            items.append((chain(parallel(rwkv_hb(c - 1, 0), rwkv_hb(c - 1, 1)), rwkv_pairs(c - 1)), 23, 1.0))
            items.append((stage2_ssd(c - 1), 7, 1.0))
        if c < NCH:
            items.append((stage1(c), 19, 1.0))
        weighted(items)
        if c >= 1:
            for _ in stage2_out(c - 1):
                pass

    kb.barrier()
    for t in reversed(p1):
        t.__exit__(None, None, None)
    if debug == "mix0":
        DBb = sb("DBb", [128, 8, TOWN], BF16)
        dma(DBb[:], mix_scr)
        DB = sb("DB", [128, 8, TOWN])
        cp('dve', DB[:], DBb[:])
        dma(dbg_d, DB[:])
        OUT = sb("OUTS", [128, 8, 2048])
        kb.op('dve', lambda: nc.vector.memset(OUT[:], 0.0), [], [OUT[:]])
        dma(out_d, OUT[:])
        kb.finish('sp')
        return nc

    TILES = [(0, 512), (512, 512), (1024, 512), (1536, 512), (2048, 128)]
    MTILES = [(128, 512), (640, 512), (1152, 512), (1664, 512)]
    HT = sb("HT", [128, 8, TOWN])
    ACTA = sb("ACTA", [128, 8, TOWN], BF16)
    WA = sb("WA", [128, 8, 1024], BF16)
    WB = sb("WB", [128, 8, 1024], BF16)
    SQT = sb("SQT", [128, 8, 128])
    RS = sb("RS", [128, 128])
    REP2 = sb("REP2", [128, 144])
    for t_ in (HT, ACTA):
        kb.split[t_.name] = (8 * TOWN, TOWN, 512)
    dma(REP2[:], rep_d[:, RC["qn"]:RC["qn"] + 144])
    dma(HT[:], xT_d[:, :, OWN0 * 128:NSLOT])
    dma(ACTA[:], mix_scr)

    def dbg_dump():
        dma(dbg_d, HT[:])
        kb.finish('sp')
        print("instructions:", kb.ninst, flush=True)
        return nc

    def rmsnorm_blk(gname, n, xnf=None):
        t0 = n * 128
        act(SQT[:], HT[:, :, t0:t0 + 128], AF.Square)
        ps = pb()
        for kc in range(8):
            mm(ps[:, 0:128], ONES, SQT[:, kc, :], start=(kc == 0), stop=(kc == 7))
        ts('dve', RS[:], ps[:, 0:128], 1.0 / DM, ALU.mult, 1e-6, ALU.add)
        rsq(RS[:])
        for kc in range(8):
            if xnf is None:
                stt('dve', ACTA[:, kc, t0:t0 + 128], HT[:, kc, t0:t0 + 128], P_(gname, 1, kc), RS[:], ALU.mult, ALU.mult)
            else:
                stt('dve', xnf[:, kc, :], HT[:, kc, t0:t0 + 128], P_(gname, 1, kc), RS[:], ALU.mult, ALU.mult)
                cp('act', ACTA[:, kc, t0:t0 + 128], xnf[:, kc, :])

    def ffn_passes(passes, WD, SGa, SGb, ACTH):
        def load_gu(ps_):
            f0, nf = ps_["f0"], ps_["nf"]
            for kc in range(8):
                kb.dma('pool', WA[:, kc, 0:nf * 128], ps_["wg"][kc * 128:(kc + 1) * 128, f0 * 128:(f0 + nf) * 128])
                kb.dma('pool', WB[:, kc, 0:nf * 128], ps_["wu"][kc * 128:(kc + 1) * 128, f0 * 128:(f0 + nf) * 128])
        load_gu(passes[0])
        for i, ps_ in enumerate(passes):
            f0, nf, tiles, rb = ps_["f0"], ps_["nf"], ps_["tiles"], ps_.get("rb")
            if ps_.get("pre") is not None:
                ps_["pre"]()
            for f in range(nf):
                kb.dma('pool', WD[:, f, :], ps_["wd"][(f0 + f) * 128:(f0 + f + 1) * 128, :])
            for (t0, tn) in tiles:
                for f in range(nf):
                    pg = pb()
                    for kc in range(8):
                        mm(pg[:, 0:tn], WA[:, kc, f * 128:(f + 1) * 128], ACTA[:, kc, t0:t0 + tn], start=(kc == 0), stop=(kc == 7))
                    pu = pb()
                    for kc in range(8):
                        mm(pu[:, 0:tn], WB[:, kc, f * 128:(f + 1) * 128], ACTA[:, kc, t0:t0 + tn], start=(kc == 0), stop=(kc == 7))
                    act(SGa[:, 0:tn], pg[:, 0:tn], AF.Silu)
                    if rb is None:
                        tt('dve', ACTH[:, f, t0:t0 + tn], pu[:, 0:tn], SGa[:, 0:tn], ALU.mult)
                    else:
                        tt('dve', SGb[:, 0:tn], pu[:, 0:tn], SGa[:, 0:tn], ALU.mult)
                        tt('dve', ACTH[:, f, t0:t0 + tn], SGb[:, 0:tn], rb[:, t0 - 128:t0 - 128 + tn], ALU.mult)
            if i + 1 < len(passes):
                load_gu(passes[i + 1])
            for (t0, tn) in tiles:
                for m in range(8):
                    ps = pb()
                    for f in range(nf):
                        mm(ps[:, 0:tn], WD[:, f, m * 128:(m + 1) * 128], ACTH[:, f, t0:t0 + tn], start=(f == 0), stop=(f == nf - 1))
                    tt('dve', HT[:, m, t0:t0 + tn], HT[:, m, t0:t0 + tn], ps[:, 0:tn], ALU.add)

    def scope():
        lst = []

        def alloc(name, shape, dt=F32):
            t = nc.sbuf_tensor(name, list(shape), dt)
            h = t.__enter__()
            lst.append(t)
            return h

        def close():
            kb.barrier()
            for t in reversed(lst):
                t.__exit__(None, None, None)
        return alloc, close

    for kc in range(8):
        kb.dma('pool', WA[:, kc, :], w_out_d[kc * 128:(kc + 1) * 128, :])
    for (t0, tn) in TILES:
        for m in range(8):
            ps = pb()
            for kc in range(8):
                mm(ps[:, 0:tn], WA[:, kc, m * 128:(m + 1) * 128], ACTA[:, kc, t0:t0 + tn], start=(kc == 0), stop=(kc == 7))
            tt('dve', HT[:, m, t0:t0 + tn], HT[:, m, t0:t0 + tn], ps[:, 0:tn], ALU.add)
    if debug == "h1":
        return dbg_dump()
    for n in range(NOWN):
        rmsnorm_blk("g_ffn0", n)
    a2, close2 = scope()
    WD = a2("WD", [128, 8, 1024], BF16)
    ACTH2 = a2("ACTH2", [128, 8, TOWN], BF16)
    kb.split[ACTH2.name] = (8 * TOWN, TOWN, 512)
    SGa = a2("SGa", [128, 512])
    SGb = a2("SGb", [128, 512])
    ffn_passes([dict(wg=ffg_d, wu=ffu_d, wd=ffd_d, f0=f0, nf=nf, tiles=TILES) for (f0, nf) in ((0, 8), (8, 8), (16, 6))],
               WD, SGa, SGb, ACTH2)
    close2()
    if debug == "h2":
        return dbg_dump()

    a3, close3 = scope()
    BQ = a3("a3_BQ", [128, 1536])
    dma(BQ[:], bq_d)
    POSI = a3("a3_POSI", [128, NOWN], I32)
    dma(POSI[:], pos_d)
    POSF = a3("a3_POSF", [128, NOWN])
    TFR = a3("a3_TFR", [128, NOWN, 8])
    TFI = a3("a3_TFI", [128, NOWN, 8], I32)
    TF2 = a3("a3_TF2", [128, NOWN, 8])
    S1 = a3("a3_S1", [128, NOWN, 8])
    COS = a3("a3_COS", [128, NOWN, 8])
    SIN = a3("a3_SIN", [128, NOWN, 8])
    FRQ = CST[:, C_FRQ:C_FRQ + 8]
    cp('dve', POSF[:], POSI[:])
    tt('dve', TFR[:], POSF[:].unsqueeze(2).to_broadcast([128, NOWN, 8]), FRQ.unsqueeze(1).to_broadcast([128, NOWN, 8]), ALU.mult)
    cp('dve', TFI[:], TFR[:])
    cp('dve', TF2[:], TFI[:])
    tt('dve', TFR[:], TFR[:], TF2[:], ALU.subtract)
    act(S1[:], TFR[:], AF.Sin, scale=float(np.pi))
    act(TF2[:], TFR[:], AF.Sin, scale=float(np.pi / 2))
    tt('dve', TF2[:], TF2[:], TF2[:], ALU.mult)
    ts('dve', TF2[:], TF2[:], -2.0, ALU.mult, 1.0, ALU.add)
    tt('dve', SIN[:], S1[:], TF2[:], ALU.mult)
    ts('dve', SIN[:], SIN[:], 2.0, ALU.mult)
    tt('dve', COS[:], S1[:], S1[:], ALU.mult)
    ts('dve', COS[:], COS[:], -2.0, ALU.mult, 1.0, ALU.add)
    QN = REP2[:, 0:64]
    KN = REP2[:, 64:128]
    SINK = REP2[:, 128:144]
    NEGB = a3("a3_NEGB", [128, 1])
    TMPB = a3("a3_TMPB", [128, 64])
    MXB = a3("a3_MXB", [128, 2])
    tt('dve', TMPB[:], QN, QN, ALU.mult)
    kb.op('dve', lambda: nc.vector.reduce_max(out=MXB[:, 0:1], in_=TMPB[:], axis=AX.X), [TMPB[:]], [MXB[:]])
    tt('dve', TMPB[:], KN, KN, ALU.mult)
    kb.op('dve', lambda: nc.vector.reduce_max(out=MXB[:, 1:2], in_=TMPB[:], axis=AX.X), [TMPB[:]], [MXB[:]])
    tt('dve', NEGB[:], MXB[:, 0:1], MXB[:, 1:2], ALU.mult)
    act(NEGB[:], NEGB[:], AF.Sqrt)
    ts('dve', NEGB[:], NEGB[:], -8.0, ALU.mult)
    ESK = a3("a3_ESK", [128, 16])
    act(ESK[:], SINK, AF.Exp, bias=NEGB[:])
    KT = a3("a3_KT", [128, 2, TOWN], BF16)
    VE = a3("a3_VE", [128, NOWN, 4, 65], BF16)
    kb.op('dve', lambda: nc.vector.memset(VE[:], 1.0), [], [VE[:]])
    QKVB = a3("a3_QKVB", [128, 1536])
    QSQ = a3("a3_QSQ", [128, 1024])
    RQ = a3("a3_RQ", [128, 20])
    RT = [a3("a3_RT%d" % i, [128, 16, 8]) for i in range(4)]
    QP = a3("a3_QP", [128, 8, 128])
    QT = a3("a3_QT", [128, 8, 128], BF16)
    PTF = a3("a3_PTF", [128, 2, 512])
    PT = a3("a3_PT", [128, 2, 512], BF16)
    DEN = a3("a3_DEN", [128, 4])
    OTOK = a3("a3_OTOK", [128, 1024])
    MASK2 = CST[:, C_M2:C_M2 + 256]
    for n in range(NOWN):
        rmsnorm_blk("g_mix1", n)
    for kc in range(8):
        kb.dma('pool', WA[:, kc, :], wqkv_d[kc * 128:(kc + 1) * 128, 0:1024])
        kb.dma('pool', WB[:, kc, 0:512], wqkv_d[kc * 128:(kc + 1) * 128, 1024:1536])

    def qk_norm_rope(e, X3, nh, gain, n, rq, sq, rts):
        sq3 = sq[:, 0:nh * 64].rearrange("p (h d) -> p h d", h=nh)
        tt(e, sq3, X3, X3, ALU.mult)
        rsum('dve', rq, sq3)
        ts(e, rq, rq, 1.0 / 64, ALU.mult, 1e-6, ALU.add)
        rsq(rq)
        tt(e, X3, X3, rq.unsqueeze(2).to_broadcast([128, nh, 64]), ALU.mult)
        tt(e, X3, X3, gain.unsqueeze(1).to_broadcast([128, nh, 64]), ALU.mult)
        c = COS[:, n, :].unsqueeze(1).to_broadcast([128, nh, 8])
        sn = SIN[:, n, :].unsqueeze(1).to_broadcast([128, nh, 8])
        x1, x2 = X3[:, :, 0:8], X3[:, :, 8:16]
        r0, r1, r2, r3 = [rts[i][:, 0:nh, :] for i in range(4)]
        tt(e, r0, x1, c, ALU.mult)
        tt(e, r1, x2, sn, ALU.mult)
        tt(e, r2, x2, c, ALU.mult)
        tt(e, r3, x1, sn, ALU.mult)
        tt(e, x1, r0, r1, ALU.subtract)
        tt(e, x2, r2, r3, ALU.add)

    QTs = [QT, a3("a3_QT1", [128, 8, 128], BF16)]
    KSQ = a3("a3_KSQ", [128, 256])
    RTK = [a3("a3_RTK%d" % i, [128, 4, 8]) for i in range(4)]

    def attn_A(n):
        t0 = n * 128
        qt = QTs[n % 2]
        cgs = (2,) if n == 0 else (2, 0, 1)
        for cg in cgs:
            ps = pb()
            for kc in range(8):
                w = WA[:, kc, cg * 512:(cg + 1) * 512] if cg < 2 else WB[:, kc, 0:512]
                mm(ps[:, :], ACTA[:, kc, t0:t0 + 128], w, start=(kc == 0), stop=(kc == 7))
            tt('dve', QKVB[:, cg * 512:(cg + 1) * 512], ps[:, :], BQ[:, cg * 512:(cg + 1) * 512], ALU.add)
            yield
        K3 = QKVB[:, 1024:1280].rearrange("p (h d) -> p h d", h=4)
        qk_norm_rope('dve', K3, 4, KN, n, RQ[:, 16:20], KSQ, RTK)
        cp('act', VE[:, n, :, 0:64], QKVB[:, 1280:1536].rearrange("p (h d) -> p h d", h=4))
        yield
        for u in range(2):
            pst = ph()
            tr(pst[:, 0:128], QKVB[:, 1024 + u * 128:1024 + (u + 1) * 128], IDENT)
            cp('act', KT[:, u, t0:t0 + 128], pst[:, 0:128])
        if n == 0:
            return
        Q3 = QKVB[:, 0:1024].rearrange("p (h d) -> p h d", h=16)
        qk_norm_rope('pool', Q3, 16, QN, n, RQ[:, 0:16], QSQ, RT)
        yield
        Q4 = QKVB[:, 0:1024].rearrange("p (kv g d) -> p kv g d", kv=4, g=4)
        for u in range(2):
            cp('pool', QP[:, u * 4:(u + 1) * 4, :].rearrange("p g (j d) -> p g j d", j=2),
               Q4[:, 2 * u:2 * u + 2, :, :].rearrange("p j g d -> p g j d"))
        yield
        for t in range(8):
            pst = ph()
            tr(pst[:, 0:128], QP[:, t, :], IDENT)
            cp('act', qt[:, t, :], pst[:, 0:128])
            if t == 3:
                yield
        yield

    def attn_B(n):
        t0 = n * 128
        qt = QTs[n % 2]
        for kv in range(4):
            u, half = kv // 2, kv % 2
            hr = slice(64 * half, 64 * half + 64)
            qrhs = qt[hr, u * 4:(u + 1) * 4, :].rearrange("p g n -> p (g n)")
            bkp = pb()
            mm(bkp[:, :], KT[hr, u, t0 - 128:t0], qrhs)
            bkc = pb()
            mm(bkc[:, :], KT[hr, u, t0:t0 + 128], qrhs)
            act(PTF[:, 0, :], bkp[:, :], AF.Exp, bias=NEGB[:], scale=0.125)
            act(PTF[:, 1, :], bkc[:, :], AF.Exp, bias=NEGB[:], scale=0.125)
            tt('dve', PT[:, 0, :].rearrange("p (g n) -> p g n", g=4), PTF[:, 0, :].rearrange("p (g n) -> p g n", g=4),
               MASK2[:, 0:128].unsqueeze(1).to_broadcast([128, 4, 128]), ALU.mult)
            tt('dve', PT[:, 1, :].rearrange("p (g n) -> p g n", g=4), PTF[:, 1, :].rearrange("p (g n) -> p g n", g=4),
               MASK2[:, 128:256].unsqueeze(1).to_broadcast([128, 4, 128]), ALU.mult)
            if n == 1:
                ts('dve', PT[:, 0, :], PT[:, 0, :], P_("flag"), ALU.mult)
            yield
            pso = pb()
            for g in range(4):
                mm(pso[:, g * 65:(g + 1) * 65], PT[:, 0, g * 128:(g + 1) * 128], VE[:, n - 1, kv, :], start=True, stop=False)
                mm(pso[:, g * 65:(g + 1) * 65], PT[:, 1, g * 128:(g + 1) * 128], VE[:, n, kv, :], start=False, stop=True)
            vo = pso[:, 0:260].rearrange("p (g d) -> p g d", g=4)
            tt('dve', DEN[:], vo[:, :, 64], ESK[:, kv * 4:(kv + 1) * 4], ALU.add)
            kb.op('dve', lambda: nc.vector.reciprocal(out=DEN[:], in_=DEN[:]), [DEN[:]], [DEN[:]])
            tt('dve', OTOK[:, kv * 256:(kv + 1) * 256].rearrange("p (g d) -> p g d", g=4), vo[:, :, 0:64],
               DEN[:].unsqueeze(2).to_broadcast([128, 4, 64]), ALU.mult)
            yield
        for j in range(8):
            pst = ph()
            tr(pst[:, 0:128], OTOK[:, j * 128:(j + 1) * 128], IDENT)
            cp('act', ACTA[:, j, t0 - 128:t0], pst[:, 0:128])
            if j == 3:
                yield
        yield

    def weighted3(items):
        st = [[g, 0, float(n_)] for (g, n_) in items]
        while st:
            st.sort(key=lambda x: (x[1] + 1) / x[2])
            x = st[0]
            try:
                next(x[0])
                x[1] += 1
            except StopIteration:
                st.remove(x)

    for n in range(NOWN + 1):
        items = []
        if n - 1 >= 1:
            items.append((attn_B(n - 1), 10))
        if n < NOWN:
            items.append((attn_A(n), 9))
        weighted3(items)
    for kc in range(8):
        kb.dma('pool', WA[:, kc, :], wo_d[kc * 128:(kc + 1) * 128, :])
    for (t0, tn) in MTILES:
        for m in range(8):
            ps = pb()
            for kc in range(8):
                mm(ps[:, 0:tn], WA[:, kc, m * 128:(m + 1) * 128], ACTA[:, kc, t0 - 128:t0 - 128 + tn], start=(kc == 0), stop=(kc == 7))
            stt('dve', HT[:, m, t0:t0 + tn], ps[:, 0:tn], P_("b_o", 1, m), HT[:, m, t0:t0 + tn], ALU.add, ALU.add)
    close3()
    if debug == "h3":
        return dbg_dump()

    a4, close4 = scope()
    WD = a4("WD4", [128, 8, 1024], BF16)
    ACTH4 = a4("ACTH4", [128, 6, TOWN], BF16)
    kb.split[ACTH4.name] = (6 * TOWN, TOWN, 512)
    SGa = a4("SGa4", [128, 512])
    SGb = a4("SGb4", [128, 512])
    XNF = a4("XNF", [128, 8, 128])
    WR = a4("WR", [128, 8, 8])
    dma(WR[:], wr_d.rearrange("(kc p) e -> p kc e", p=128))
    LG = a4("LG", [128, 8])
    MX8 = a4("MX8", [128, 8])
    NV1 = a4("NV1", [128, 1])
    MSK = a4("MSK", [128, 8])
    EX = a4("EX", [128, 8])
    SME = a4("SME", [128, 1])
    COMB = a4("COMB", [128, 16, 8])
    DG = a4("DG", [128, 128])
    RBs = [a4("RB%d" % i, [128, 2048], BF16) for i in range(2)]
    for n in range(1, NOWN):
        rmsnorm_blk("g_ffn1", n, xnf=XNF)
        ps = ph()
        for kc in range(8):
            mm(ps[:, 0:8], XNF[:, kc, :], WR[:, kc, :], start=(kc == 0), stop=(kc == 7))
        cp('dve', LG[:], ps[:, 0:8])
        kb.op('dve', lambda: nc.vector.max(out=MX8[:], in_=LG[:]), [LG[:]], [MX8[:]])
        ts('dve', MSK[:], LG[:], MX8[:, 1:2], ALU.is_ge)
        ts('dve', NV1[:], MX8[:, 0:1], -1.0, ALU.mult)
        act(EX[:], LG[:], AF.Exp, bias=NV1[:])
        tt('dve', EX[:], EX[:], MSK[:], ALU.mult)
        rsum('dve', SME[:], EX[:])
        kb.op('dve', lambda: nc.vector.reciprocal(out=SME[:], in_=SME[:]), [SME[:]], [SME[:]])
        ts('dve', COMB[:, n - 1, :], EX[:], SME[:], ALU.mult)
    def build_rb(e):
        RB = RBs[e % 2]
        for q4 in range(4):
            ps = pb()
            for b in range(4):
                blk = q4 * 4 + b
                ts('dve', DG[:], IDENT, COMB[:, blk, e:e + 1], ALU.mult)
                mm(ps[:, b * 128:(b + 1) * 128], ONES, DG[:])
            cp('act', RB[:, q4 * 512:(q4 + 1) * 512], ps[:, :])

    passes = []
    for e in range(NEXP):
        passes.append(dict(wg=eg_d[e], wu=eu_d[e], wd=ed_d[e], f0=0, nf=6, tiles=MTILES, rb=RBs[e % 2]))
        passes.append(dict(wg=eg_d[e], wu=eu_d[e], wd=ed_d[e], f0=6, nf=5, tiles=MTILES, rb=RBs[e % 2],
                           pre=(lambda e=e: build_rb(e + 1)) if e + 1 < NEXP else None))
    build_rb(0)
    ffn_passes(passes, WD, SGa, SGb, ACTH4)
    if debug == "h4":
        return dbg_dump()
    dma(out_d, HT[:, :, 128:TOWN])
    kb.finish('sp')
    print("instructions:", kb.ninst, flush=True)
    return nc


def _consts():
    i = np.arange(128)
    cst = np.zeros((128, NCST), np.float32)
    cst[:, C_ID:C_ID + 128] = np.eye(128)
    mui = (i[None, :] >= i[:, None]).astype(np.float32)
    mus = (i[None, :] > i[:, None]).astype(np.float32)
    cst[:, C_MUI:C_MUI + 128] = mui
    cst[:, C_MUS:C_MUS + 128] = mus
    cst[:, C_MUI2:C_MUI2 + 128] = mui
    cst[:, C_SL:C_SL + 128] = (i[None, :] < i[:, None])
    cst[:, C_ONES:C_ONES + 128] = 1.0
    blk = (i[None, :] // 64 == i[:, None] // 64).astype(np.float32)
    cst[:, C_BLK:C_BLK + 128] = blk
    cst[:, C_HSEL] = (i < 64)
    cst[:, C_HSEL + 1] = (i >= 64)
    cst[:, C_M2:C_M2 + 128] = (i[None, :] < i[:, None])
    cst[:, C_M2 + 128:C_M2 + 256] = mui
    inv_freq = 500000.0 ** (-np.arange(0, 16, 2, dtype=np.float64) / 16.0)
    cst[:, C_FRQ:C_FRQ + 8] = (inv_freq / (2 * np.pi))[None, :]
    return cst


def _fm(v, n):
    return np.ascontiguousarray(np.asarray(v, np.float32).reshape(n, 128).T)


def _prepare(inp):
    f = lambda k: np.asarray(inp[k], np.float32)
    prm = np.zeros((128, NPRM), np.float32)
    prm[:, PC["g_mix0"]:PC["g_mix0"] + 8] = _fm(f("ev_norm_mix")[0], 8)
    prm[:, PC["g_ffn0"]:PC["g_ffn0"] + 8] = _fm(f("ev_norm_ffn")[0], 8)
    prm[:, PC["g_mix1"]:PC["g_mix1"] + 8] = _fm(f("od_norm_mix")[0], 8)
    prm[:, PC["g_ffn1"]:PC["g_ffn1"] + 8] = _fm(f("od_norm_ffn")[0], 8)
    prm[:, PC["a0"]:PC["a0"] + 4] = _fm(f("ev_a0")[0], 4)
    cw = f("ev_conv_w")[0]
    prm[:, PC["conv_w"]:PC["conv_w"] + 32] = cw.T.reshape(8, 128, 4).transpose(1, 0, 2).reshape(128, 32)
    prm[:, PC["conv_b"]:PC["conv_b"] + 8] = _fm(f("ev_conv_b")[0], 8)
    prm[:, PC["b_qkv"]:PC["b_qkv"] + 12] = _fm(f("od_b_qkv")[0], 12)
    prm[:, PC["b_o"]:PC["b_o"] + 8] = _fm(f("od_b_o")[0], 8)
    rep = np.zeros((128, NREP), np.float32)

    def setrep(n, v):
        v = np.asarray(v, np.float32).reshape(-1)
        rep[:, RC[n]:RC[n] + v.size] = v[None, :]

    setrep("w0", f("ev_w0")[0])
    setrep("gn_w", f("ev_gn_w")[0])
    setrep("gn_b", f("ev_gn_b")[0])
    setrep("ssdn", f("ev_ssd_norm")[0])
    prm[:, PC["dskp"]:PC["dskp"] + 8] = f("ev_d_skip")[0][None, :]
    setrep("dtb", f("ev_dt_bias")[0])
    setrep("alog", f("ev_a_log")[0])
    setrep("qn", f("od_q_norm")[0])
    setrep("kn", f("od_k_norm")[0])
    setrep("sink", f("od_sinks")[0])
    bc = _fm(f("ev_mu_shift")[0], 14)
    prm[:, PC["kk"]:PC["kk"] + 4] = _fm(f("ev_k_k")[0], 4)
    prm[:, PC["ka"]:PC["ka"] + 4] = _fm(f("ev_k_a")[0], 4)
    prm[:, PC["rk"]:PC["rk"] + 4] = _fm(f("ev_r_k")[0].reshape(-1), 4)
    shared = {
        "rep": rep, "cst": _consts(), "bc": bc,
        "w_in": np.ascontiguousarray(f("ev_w_in")[0]),
        "wdu": np.ascontiguousarray(f("ev_w_decay_up")[0]),
        "wiu": np.ascontiguousarray(f("ev_w_iclr_up")[0]),
        "wgu": np.ascontiguousarray(f("ev_w_gate_up")[0]),
        "w_out": np.ascontiguousarray(f("ev_w_out")[0]),
        "ffg": np.ascontiguousarray(f("ev_ffn_gate")[0]),
        "ffu": np.ascontiguousarray(f("ev_ffn_up")[0]),
        "ffd": np.ascontiguousarray(f("ev_ffn_down")[0]),
        "wqkv": np.ascontiguousarray(f("od_w_qkv")[0]),
        "wo": np.ascontiguousarray(f("od_w_o")[0]),
        "bq": np.ascontiguousarray(np.broadcast_to(f("od_b_qkv")[0][None, :], (128, 1536))),
        "wr": np.ascontiguousarray(f("od_router")[0]),
        "eg": np.ascontiguousarray(f("od_exp_gate")[0]),
        "eu": np.ascontiguousarray(f("od_exp_up")[0]),
        "ed": np.ascontiguousarray(f("od_exp_down")[0]),
    }
    positions = np.asarray(inp["positions"]).astype(np.int32)
    x = f("x")
    maps = []
    for c in range(8):
        b, half = c // 2, c % 2
        xs = np.zeros((NSLOT, DM), np.float32)
        if half == 1:
            xs[:] = x[b]
        else:
            xs[2048:] = x[b, :2048]
        xT = np.ascontiguousarray(xs.T.reshape(8, 128, NSLOT).transpose(1, 0, 2))
        p = prm.copy()
        p[:, PC["flag"]] = float(half)
        m = dict(shared)
        m["xT"] = xT
        m["prm"] = p
        ps_ = np.zeros((NSLOT,), np.int32)
        if half == 1:
            ps_[:] = positions[b]
        else:
            ps_[2048:] = positions[b, :2048]
        m["pos"] = np.ascontiguousarray(ps_[OWN0 * 128:].reshape(NOWN, 128).T)
        maps.append(m)
    return maps


def kernel(**inputs):
    debug = os.environ.get("KDEBUG", "")
    maps = _prepare(inputs)
    nc = build_nc(debug=debug)
    res = run_bass_kernel_spmd(nc, maps, core_ids=list(range(8)))
    out = np.zeros((4, 4096, DM), np.float32)
    for c in range(8):
        b, half = c // 2, c % 2
        oT = res.results[c]["outT"]
        out[b, half * 2048:(half + 1) * 2048] = oT.transpose(2, 1, 0).reshape(2048, DM)
    if debug:
        kernel.dbg = [res.results[c]["dbg"] for c in range(8)]
    return out
```

```python
import os
import numpy as np
import concourse.bass as bass
import concourse.mybir as mybir
from concourse.bass_utils import run_bass_kernel_spmd

F32 = mybir.dt.float32
BF16 = mybir.dt.bfloat16
AF = mybir.ActivationFunctionType
ALU = mybir.AluOpType
AX = mybir.AxisListType

DM = 1024
NSLOT = 4096
NCH = 32
OWN0 = 15
NOWN = NCH - OWN0
TOWN = NOWN * 128
IN0 = 3336
FF0 = 2816
FFE = 1408
NEXP = 8
LWS = 0.6065306597126334

PC = {}
_o = 0
for _n, _w in [("g_mix0", 8), ("g_ffn0", 8), ("g_mix1", 8), ("g_ffn1", 8), ("a0", 4), ("kk", 4), ("ka", 4), ("rk", 4), ("conv_w", 32), ("conv_b", 8),
               ("b_qkv", 12), ("b_o", 8), ("flag", 1), ("dskp", 8)]:
    PC[_n] = _o
    _o += _w
NPRM = _o
RC = {}
_o = 0
for _n, _w in [("w0", 512), ("gn_w", 512), ("gn_b", 512), ("ssdn", 512), ("dtb", 8), ("alog", 8),
               ("qn", 64), ("kn", 64), ("sink", 16)]:
    RC[_n] = _o
    _o += _w
NREP = _o
C_ID, C_MUI, C_MUS, C_MUI2, C_SL, C_ONES, C_BLK, C_HSEL = 0, 128, 256, 384, 512, 640, 768, 896
C_M2 = 898
C_FRQ = 1154
NCST = 1162
I32 = mybir.dt.int32


class KB:
    def __init__(self, nc, ndma=16):
        self.nc = nc
        self.E = {'pe': nc.tensor, 'dve': nc.vector, 'act': nc.scalar, 'pool': nc.gpsimd, 'sp': nc.sync}
        self.sem = {e: nc.alloc_semaphore('s_' + e) for e in self.E}
        self.cnt = {e: 0 for e in self.E}
        self.seen = {e: {} for e in self.E}
        self.lastw = {}
        self.reads = {}
        self.ndma = ndma
        self.dsem = [nc.alloc_semaphore('d_%d' % i) for i in range(ndma)]
        self.dcnt = [0] * ndma
        self.dn = 0
        self.dnq = {}
        self.ninst = 0
        self.split = {}

    def _semof(self, key):
        return self.sem[key] if isinstance(key, str) else self.dsem[key]

    def _wait(self, e, key, val):
        if val <= 0 or self.seen[e].get(key, 0) >= val:
            return
        if key == e and e == 'pe':
            return
        self.E[e].wait_ge(self._semof(key), val)
        self.seen[e][key] = val

    def _deps(self, e, ins, outs):
        for n in ins:
            w = self.lastw.get(n)
            if w is not None:
                self._wait(e, w[0], w[1])
        for n in outs:
            w = self.lastw.get(n)
            if w is not None:
                self._wait(e, w[0], w[1])
            for (k, v) in self.reads.get(n, ()):
                self._wait(e, k, v)

    def _commit(self, key, val, ins, outs):
        for n in ins:
            self.reads.setdefault(n, []).append((key, val))
        for n in outs:
            self.lastw[n] = (key, val)
            self.reads[n] = []

    def keys(self, a):
        n = a.tensor.name
        sp = self.split.get(n)
        if sp is None:
            return [n]
        row, inner, piece = sp
        col = (a.offset % row) % inner
        ext = 1
        for (st, cn) in list(a.ap)[1:]:
            if st < inner:
                ext += (cn - 1) * st
        lo, hi = col // piece, min(col + ext - 1, inner - 1) // piece
        return ["%s#%d" % (n, i) for i in range(lo, hi + 1)]

    def op(self, e, fn, ins, outs):
        xs = [k for a in ins if a.tensor.name in self.split for k in self.keys(a)]
        ins = [k for a in ins for k in self.keys(a)]
        outs = [k for a in outs for k in self.keys(a)] + xs
        self._deps(e, ins, outs)
        inst = fn()
        self.cnt[e] += 1
        inst.then_inc(self.sem[e], 1)
        self.seen[e][e] = max(self.seen[e].get(e, 0), 0)
        self._commit(e, self.cnt[e], ins, outs)
        self.ninst += 1
        return inst

    def dma(self, e, out, in_, **kw):
        half = self.ndma // 2
        base = half if e == 'pool' else 0
        k = self.dnq.get(e, 0)
        self.dnq[e] = k + 1
        slot = base + (k % half)
        self._wait(e, slot, self.dcnt[slot])
        ins = self.keys(in_)
        outs = self.keys(out)
        self._deps(e, ins, outs)
        inst = self.E[e].dma_start(out=out, in_=in_, **kw)
        self.dcnt[slot] += 16
        inst.then_inc(self.dsem[slot], 16)
        self._commit(slot, self.dcnt[slot], ins, outs)
        self.ninst += 1
        return inst

    def barrier(self):
        for e in self.E:
            for s in range(self.ndma):
                self._wait(e, s, self.dcnt[s])
            for k in self.E:
                if k != e:
                    self._wait(e, k, self.cnt[k])
        for e in ('dve', 'act', 'pool'):
            self._wait(e, e, self.cnt[e])

    def finish(self, e='sp'):
        for s in range(self.ndma):
            self._wait(e, s, self.dcnt[s])
        for k in self.E:
            if k != e:
                self._wait(e, k, self.cnt[k])


def build_nc(debug=""):
    nc = bass.Bass("TRN2", target_bir_lowering=False)
    kb = KB(nc)

    def din(name, shape, dt=F32):
        return nc.dram_tensor(name, list(shape), dt, kind="ExternalInput").ap()

    xT_d = din("xT", [128, 8, NSLOT])
    prm_d = din("prm", [128, NPRM])
    rep_d = din("rep", [128, NREP])
    cst_d = din("cst", [128, NCST])
    bc_d = din("bc", [128, 14])
    w_in_d = din("w_in", [DM, IN0])
    wdu_d = din("wdu", [64, 512])
    wiu_d = din("wiu", [64, 512])
    wgu_d = din("wgu", [128, 512])
    w_out_d = din("w_out", [DM, DM])
    ffg_d = din("ffg", [DM, FF0])
    ffu_d = din("ffu", [DM, FF0])
    ffd_d = din("ffd", [FF0, DM])
    wqkv_d = din("wqkv", [DM, 1536])
    wo_d = din("wo", [DM, DM])
    bq_d = din("bq", [128, 1536])
    pos_d = din("pos", [128, NOWN], I32)
    wr_d = din("wr", [DM, NEXP])
    eg_d = din("eg", [NEXP, DM, FFE])
    eu_d = din("eu", [NEXP, DM, FFE])
    ed_d = din("ed", [NEXP, FFE, DM])
    out_d = nc.dram_tensor("outT", [128, 8, 2048], F32, kind="ExternalOutput").ap()
    dbg_d = None
    if debug:
        dbg_d = nc.dram_tensor("dbg", [128, 8, TOWN], F32, kind="ExternalOutput").ap()

    V = {'dve': nc.vector, 'pool': nc.gpsimd}
    rr = {'el': 0, 'ev': 0, 'ph': 0, 'pb': 0, 'dq': 0}

    def el():
        rr['el'] ^= 1
        return 'dve' if rr['el'] else 'pool'

    def ev():
        rr['ev'] ^= 1
        return 'dve' if rr['ev'] else 'act'

    def aps(*xs):
        return [x for x in xs if hasattr(x, 'tensor')]

    def mm(out, lhsT, rhs, start=True, stop=True):
        kb.op('pe', lambda: nc.tensor.matmul(out, lhsT=lhsT, rhs=rhs, start=start, stop=stop), [lhsT, rhs], [out])

    def tr(out, in_, ident):
        kb.op('pe', lambda: nc.tensor.transpose(out, in_, ident), [in_, ident], [out])

    def tt(e, out, a, b, op):
        kb.op(e, lambda: V[e].tensor_tensor(out=out, in0=a, in1=b, op=op), [a, b], [out])

    def ts(e, out, a, s1, op0, s2=None, op1=None):
        if op1 is None:
            kb.op(e, lambda: V[e].tensor_scalar(out=out, in0=a, scalar1=s1, scalar2=None, op0=op0), aps(a, s1), [out])
        else:
            kb.op(e, lambda: V[e].tensor_scalar(out=out, in0=a, scalar1=s1, scalar2=s2, op0=op0, op1=op1),
                  aps(a, s1, s2), [out])

    def stt(e, out, a, s, b, op0, op1):
        e = 'dve'
        kb.op(e, lambda: V[e].scalar_tensor_tensor(out=out, in0=a, scalar=s, in1=b, op0=op0, op1=op1),
              aps(a, s, b), [out])

    def act(out, in_, func, bias=None, scale=1.0):
        if bias is None:
            kb.op('act', lambda: nc.scalar.activation(out=out, in_=in_, func=func, scale=scale), aps(in_, scale), [out])
        else:
            kb.op('act', lambda: nc.scalar.activation(out=out, in_=in_, func=func, bias=bias, scale=scale),
                  aps(in_, bias, scale), [out])

    def rsq(x):
        act(x, x, AF.Ln)
        act(x, x, AF.Exp, scale=-0.5)

    def cp(e, out, in_):
        if e == 'act':
            kb.op('act', lambda: nc.scalar.copy(out=out, in_=in_), [in_], [out])
        else:
            kb.op(e, lambda: V[e].tensor_copy(out=out, in_=in_), [in_], [out])

    def rsum(e, out, in_):
        kb.op(e, lambda: V[e].reduce_sum(out=out, in_=in_, axis=AX.X), [in_], [out])

    def dma(out, in_, q=None):
        kb.dma(q or 'sp', out, in_)

    def sb(name, shape, dt=F32):
        return nc.alloc_sbuf_tensor(name, list(shape), dt)

    BANKS = [nc.alloc_psum_tensor("PS%d" % i, [128, 512], F32) for i in range(8)]
    for b_ in BANKS:
        kb.split[b_.name] = (512, 512, 512)
    def pb():
        rr['pb'] = (rr['pb'] + 1) % 8
        return BANKS[rr['pb']][:, :]

    def ph():
        return pb()[:, 0:256]

    PRM = sb("PRM", [128, NPRM])
    CST = sb("CST", [128, NCST])
    mix_scr = nc.dram_tensor("mix_scr", [128, 8, TOWN], BF16).ap()
    dma(PRM[:], prm_d)
    dma(CST[:], cst_d)
    IDENT = CST[:, C_ID:C_ID + 128]
    MUI = CST[:, C_MUI:C_MUI + 128]
    TRI2 = CST[:, C_MUI:C_MUI + 256]
    MU2 = CST[:, C_MUS:C_MUS + 256]
    SLm = CST[:, C_SL:C_SL + 128]
    ONES = CST[:, C_ONES:C_ONES + 128]
    BLK = CST[:, C_BLK:C_BLK + 128]
    HSEL = CST[:, C_HSEL:C_HSEL + 2]

    def P_(n, w=1, o=0):
        return PRM[:, PC[n] + o:PC[n] + o + w]

    def R_(n, w, o=0):
        return REP[:, RC[n] + o:RC[n] + o + w]

    p1 = []

    def sb1(name, shape, dt=F32):
        t = nc.sbuf_tensor(name, list(shape), dt)
        h = t.__enter__()
        p1.append(t)
        return h

    REP = sb1("REP", [128, NREP])
    dma(REP[:], rep_d)
    WIN = sb1("WIN", [128, 8, IN0], BF16)
    for kc in range(8):
        kb.dma('pool', WIN[:, kc, :], w_in_d[kc * 128:(kc + 1) * 128, :])
    WDU = sb1("WDU", [64, 512], BF16)
    kb.dma('pool', WDU[:], wdu_d)
    WIU = sb1("WIU", [128, 512], BF16)
    kb.dma('pool', WIU[64:128, :], wiu_d)
    WGU = sb1("WGU", [128, 512], BF16)
    kb.dma('pool', WGU[:], wgu_d)
    MUC = sb1("MUC", [128, 14])
    dma(MUC[:], bc_d)
    IDB = sb1("IDB", [128, 128], BF16)
    cp('dve', IDB[:], IDENT)
    AREP = sb1("AREP", [128, 8])
    act(AREP[:], R_("alog", 8), AF.Exp)
    ts('dve', AREP[:], AREP[:], -1.0, ALU.mult)

    XT = sb1("XT0", [128, 8, 128])
    RSTD = sb1("RSTD", [128, 128])
    XN = sb1("XN", [128, 8, 128], BF16)
    PA = sb1("PA0", [128, 14, 129])
    XB = sb1("XB0", [128, 8, 131])
    DTMP = sb1("DTMP", [128, 14, 128])
    PL = DTMP
    SQ = DTMP
    CT2 = sb1("CT2", [128, 4, 128])
    XC = sb1("XC", [128, 4, 128])
    TW = sb1("TW", [64, 128], BF16)
    ALB = sb1("ALB", [128, 128], BF16)
    SGL = sb1("SGL", [128, 128], BF16)
    SG = sb1("SG", [128, 512])
    ICLR = sb1("ICLR", [128, 4, 128])
    KQ = sb1("KQ", [128, 4, 128])
    SQK = sb1("SQK", [128, 4, 128])
    RN = sb1("RN", [128, 4, 128])
    KKn = sb1("KKn", [128, 4, 128])
    KM = sb1("KM", [128, 4, 128])
    T1 = SQK
    Bv = KQ
    RKR = RN
    E1 = sb1("E1", [128, 4, 128])
    E0 = sb1("E0", [128, 4, 128])
    EI = sb1("EI", [128, 4, 128])
    def dbl(name, shape, dt=F32):
        return [sb1("%s_%d" % (name, i), shape, dt) for i in range(2)]
    AR = dbl("AR", [128, 4, 2, 128], BF16)
    KT = dbl("KT", [128, 4, 128], BF16)
    BT = dbl("BT", [128, 4, 128], BF16)
    VTOK = dbl("VTOK", [128, 4, 128], BF16)
    A0T = dbl("A0T", [128, 4, 128], BF16)
    R0T = dbl("R0T", [128, 4, 128], BF16)
    KTT = dbl("KTT", [128, 4, 128], BF16)
    BTT = dbl("BTT", [128, 4, 128], BF16)
    G127 = dbl("G127", [128, 4])
    GOUT = dbl("GOUT", [128, 512])
    BON = dbl("BON", [128, 8])
    ZS = dbl("ZS", [128, 512])
    XSTOK = dbl("XSTOK", [128, 512])
    BTOK = dbl("BTOK", [128, 256])
    XBC = dbl("XBC", [128, 4, 128])
    DT = dbl("DT", [128, 8])
    DA = dbl("DA", [128, 8])
    CUMC = dbl("CUMC", [128, 8])
    NN = [[sb1("NN%d_%d" % (hb, i), [128, 4, 128], BF16) for i in range(2)] for hb in range(2)]
    PP = [[sb1("PP%d_%d" % (hb, i), [128, 4, 128], BF16) for i in range(2)] for hb in range(2)]
    TTb = [sb1("TTb%d" % hb, [128, 4, 128], BF16) for hb in range(2)]
    ARB = [sb1("ARB%d" % hb, [128, 4, 128], BF16) for hb in range(2)]
    KAb = [sb1("KAb%d" % hb, [128, 4, 256], BF16) for hb in range(2)]
    AXb = [sb1("AXb%d" % hb, [128, 4, 128], BF16) for hb in range(2)]
    WW = sb1("WW", [128, 4, 128], BF16)
    UU = sb1("UU", [128, 4, 128], BF16)
    QTOK = sb1("QTOK", [128, 4, 128], BF16)
    Y0 = sb1("Y0", [128, 4, 128])
    QT = sb1("QT", [128, 4, 128], BF16)
    MT = sb1("MT", [128, 4, 128], BF16)
    LL = sb1("LL", [128, 4, 64])
    HS = [sb1("HS%d" % i, [128, 4, 64]) for i in range(2)]
    HSb = [sb1("HSb%d" % i, [128, 4, 64], BF16) for i in range(2)]
    ST1 = sb1("ST1", [128, 8])
    ST2 = sb1("ST2", [128, 8])
    ST3 = sb1("ST3", [128, 8])
    MIXTOK = sb1("MIXTOK", [128, 1024])
    YTOK = MIXTOK[:, 0:512]
    YS = MIXTOK[:, 512:1024]
    MXS = sb1("MXS", [128, 8, 128], BF16)
    ECUM = sb1("ECUM", [128, 8])
    CDEC = sb1("CDEC", [128, 8])
    TE = sb1("TE", [128, 8])
    CBM = sb1("CBM", [128, 2, 128])
    TD4 = sb1("TD4", [128, 4, 128])
    SEG4 = TD4
    LD4 = sb1("LD4", [128, 4, 128])
    WT4 = LD4
    XW = sb1("XW", [128, 512])
    JUNK = sb1("JUNK", [128, 64], BF16)
    YOFF4 = sb1("YOFF4", [128, 4, 64])
    ST4 = sb1("ST4", [128, 2])
    HSS = [sb1("HSS%d" % i, [128, 8, 64]) for i in range(2)]

    kb.op('dve', lambda: nc.vector.memset(HS[0][:], 0.0), [], [HS[0][:]])
    kb.op('dve', lambda: nc.vector.memset(HSb[0][:], 0.0), [], [HSb[0][:]])
    kb.op('dve', lambda: nc.vector.memset(HSS[0][:], 0.0), [], [HSS[0][:]])
    kb.op('pool', lambda: nc.gpsimd.memset(PA[:], 0.0), [], [PA[:]])
    kb.op('pool', lambda: nc.gpsimd.memset(XB[:], 0.0), [], [XB[:]])

    def fgrp(g):
        if g < 14:
            return g * 128
        return 1792 + 512 + (g - 14) * 128

    def trb(out, in_):
        mm(out, in_, IDB[:])

    def norm_block(c):
        act(SQ[:, 0:8, :], XT[:], AF.Square)
        ps = ph()
        for kc in range(8):
            mm(ps[:, 0:128], ONES, SQ[:, kc, :], start=(kc == 0), stop=(kc == 7))
        ts('dve', RSTD[:], ps[:, 0:128], 1.0 / DM, ALU.mult, 1e-6, ALU.add)
        rsq(RSTD[:])
        tt('pool', SQ[:, 0:8, :], XT[:], P_("g_mix0", 8).unsqueeze(2).to_broadcast([128, 8, 128]), ALU.mult)
        tt('pool', XN[:], SQ[:, 0:8, :], RSTD[:].unsqueeze(1).to_broadcast([128, 8, 128]), ALU.mult)

    def stage1(c):
        own = c >= OWN0
        par = c % 2
        pa, xb = PA, XB
        R4 = PL[:, 0:4, :]
        K4 = PL[:, 4:8, :]
        V4 = PL[:, 8:12, :]
        cp('pool', pa[:, :, 0:1], pa[:, :, 128:129])
        cp('pool', xb[:, :, 0:3], xb[:, :, 128:131])
        for g0 in range(0, 22, 4):
            gs = list(range(g0, min(g0 + 4, 22)))
            ps = pb()
            for i, g in enumerate(gs):
                co = fgrp(g)
                for kc in range(8):
                    mm(ps[:, i * 128:(i + 1) * 128], WIN[:, kc, co:co + 128], XN[:, kc, :], start=(kc == 0), stop=(kc == 7))
            pag = [g for g in gs if g < 14]
            xbg = [g for g in gs if g >= 14]
            if pag:
                i0 = gs.index(pag[0])
                cp('act', pa[:, pag[0]:pag[-1] + 1, 1:129], ps[:, i0 * 128:(i0 + len(pag)) * 128].rearrange("p (g n) -> p g n", g=len(pag)))
            if xbg:
                i0 = gs.index(xbg[0])
                cp('act', xb[:, xbg[0] - 14:xbg[-1] - 13, 3:131], ps[:, i0 * 128:(i0 + len(xbg)) * 128].rearrange("p (g n) -> p g n", g=len(xbg)))
            yield
        psz = pb()
        for kc in range(8):
            mm(psz[:, :], XN[:, kc, :], WIN[:, kc, 1792:1792 + 512], start=(kc == 0), stop=(kc == 7))
        if own:
            act(ZS[par][:], psz[:, :], AF.Silu)
        psd = ph()
        for kc in range(8):
            mm(psd[:, 0:8], XN[:, kc, :], WIN[:, kc, IN0 - 8:IN0], start=(kc == 0), stop=(kc == 7))
        tt('dve', DT[par][:], psd[:, 0:8], R_("dtb", 8), ALU.add)
        if c + 1 < NCH:
            dma(XT[:], xT_d[:, :, (c + 1) * 128:(c + 2) * 128])
        yield

        def prep_rwkv():
            for (ga, gb) in ((12, 14), (4, 8), (0, 4), (8, 12)):
                tt('pool', DTMP[:, ga:gb, :], pa[:, ga:gb, 0:128], pa[:, ga:gb, 1:129], ALU.subtract)
                tt('pool', DTMP[:, ga:gb, :], DTMP[:, ga:gb, :], MUC[:, ga:gb].unsqueeze(2).to_broadcast([128, gb - ga, 128]), ALU.mult)
                tt('pool', PL[:, ga:gb, :], DTMP[:, ga:gb, :], pa[:, ga:gb, 1:129], ALU.add)
                if ga == 12:
                    act(TW[:], PL[0:64, 12, :], AF.Tanh)
                    cp('dve', ALB[64:128, :], PL[64:128, 12, :])
                    act(SGL[:], PL[:, 13, :], AF.Sigmoid)
                if ga == 4:
                    tt('pool', KQ[:], K4, P_('kk', 4).unsqueeze(2).to_broadcast([128, 4, 128]), ALU.mult)
                    tt('pool', SQK[:], KQ[:], KQ[:], ALU.mult)
                yield
            psw = pb()
            mm(psw[:, :], TW[:], WDU[:])
            tt('dve', SG[:], psw[:, :], R_("w0", 512), ALU.add)
            act(SG[:], SG[:], AF.Sigmoid)
            if own:
                psg = pb()
                mm(psg[:, :], SGL[:], WGU[:])
                cp('act', GOUT[par][:], psg[:, :])
            psi = pb()
            for p in range(4):
                mm(psi[:, p * 128:(p + 1) * 128], WIU[64:128, p * 128:(p + 1) * 128], ALB[64:128, :])
            for p in range(4):
                act(ICLR[:, p, :], psi[:, p * 128:(p + 1) * 128], AF.Sigmoid, bias=P_("a0", 1, p))
            pss = pb()
            mm(pss[:, :], BLK, SQK[:].rearrange("p a b -> p (a b)"))
            ts('dve', RN[:].rearrange("p a b -> p (a b)"), pss[:, :], 1e-12, ALU.add)
            yield
            rsq(RN[:].rearrange("p a b -> p (a b)"))
            ts('pool', T1[:], ICLR[:], -1.0, ALU.add)
            tt('pool', T1[:], T1[:], P_('ka', 4).unsqueeze(2).to_broadcast([128, 4, 128]), ALU.mult)
            tt('pool', T1[:], T1[:], K4, ALU.mult)
            for p in range(4):
                psc = ph()
                mm(psc[:, 0:256], SG[:, p * 128:(p + 1) * 128], TRI2)
                act(E1[:, p, :], psc[:, 0:128], AF.Exp, scale=-LWS)
                act(E0[:, p, :], psc[:, 128:256], AF.Exp, scale=-LWS)
                act(EI[:, p, :], psc[:, 0:128], AF.Exp, scale=LWS)
            cp('act', G127[par][:], E1[:, :, 127])
            yield
            tt('pool', KKn[:], KQ[:], RN[:], ALU.mult)
            tt('pool', KM[:], T1[:], K4, ALU.add)
            tt('pool', AR[par][:, :, 1, :], R4, E1[:], ALU.mult)
            yield
            tt('pool', Bv[:], KKn[:], ICLR[:], ALU.mult)
            stt('dve', AR[par][:, :, 0, :], KKn[:], -1.0, E0[:], ALU.mult, ALU.mult)
            tt('pool', KT[par][:], KM[:], EI[:], ALU.mult)
            tt('pool', BT[par][:], Bv[:], EI[:], ALU.mult)
            if own:
                tt('pool', RKR[:], R4, KM[:], ALU.mult)
                tt('pool', RKR[:], RKR[:], P_('rk', 4).unsqueeze(2).to_broadcast([128, 4, 128]), ALU.mult)
            yield
            for p in range(4):
                pst = ph()
                tr(pst[:, 0:128], V4[:, p, :], IDENT)
                cp('act', VTOK[par][:, p, :], pst[:, 0:128])
                for src, dst in ((AR[par][:, p, 0, :], A0T[par]), (AR[par][:, p, 1, :], R0T[par]), (KT[par][:, p, :], KTT[par]),
                                 (BT[par][:, p, :], BTT[par])):
                    pst = ph()
                    trb(pst[:, 0:128], src)
                    cp(ev(), dst[:, p, :], pst[:, 0:128])
                if p % 2 == 1:
                    yield
            if own:
                psb = ph()
                for p in range(4):
                    mm(psb[:, 2 * p:2 * p + 2], RKR[:, p, :], HSEL)
                cp('act', BON[par][:], psb[:, 0:8])

        def prep_ssd():
            CW3 = PRM[:, PC["conv_w"]:PC["conv_w"] + 32].rearrange("p (g j) -> p g j", j=4)
            for half in range(2):
                gsl = slice(4 * half, 4 * half + 4)
                c0 = XC[:, :, :] if half == 0 else XBC[par][:, :, :]
                c1 = CT2[:, :, :]
                tt('pool', c0, xb[:, gsl, 0:128], CW3[:, gsl, 0].unsqueeze(2).to_broadcast([128, 4, 128]), ALU.mult)
                for j in range(1, 4):
                    tt('pool', c1, xb[:, gsl, j:j + 128], CW3[:, gsl, j].unsqueeze(2).to_broadcast([128, 4, 128]), ALU.mult)
                    tt('pool', c0, c0, c1, ALU.add)
                yield
            for g in range(8):
                dst = XC[:, g, :] if g < 4 else XBC[par][:, g - 4, :]
                act(dst, dst, AF.Silu, bias=P_("conv_b", 1, g))
            act(DT[par][:], DT[par][:], AF.Exp)
            act(DT[par][:], DT[par][:], AF.Ln, bias=1.0)
            tt('dve', DA[par][:], DT[par][:], AREP[:], ALU.mult)
            yield
            yield
            psc = ph()
            mm(psc[:, 0:8], MUI, DA[par][:])
            cp('act', CUMC[par][:], psc[:, 0:8])
            for g in range(4):
                pst = ph()
                tr(pst[:, 0:128], XC[:, g, :], IDENT)
                cp('act', XSTOK[par][:, g * 128:(g + 1) * 128], pst[:, 0:128])
            for g in range(2):
                pst = ph()
                tr(pst[:, 0:128], XBC[par][:, g, :], IDENT)
                cp('act', BTOK[par][:, g * 128:(g + 1) * 128], pst[:, 0:128])
            yield

        for _ in parallel(prep_rwkv(), prep_ssd()):
            yield
        if c + 1 < NCH:
            norm_block(c + 1)
        yield

    def rwkv_hb(c, hb):
        own = c >= OWN0
        par = c % 2
        ar, kt, bt, vtok, a0t, r0t = AR[par], KT[par], BT[par], VTOK[par], A0T[par], R0T[par]
        heads = list(range(4 * hb, 4 * hb + 4))
        nn, pp, ttb, arb, kab, axb = NN[hb], PP[hb], TTb[hb], ARB[hb], KAb[hb], AXb[hb]

        def h8(t):
            return t[:].rearrange("p a (h d) -> p (a h) d", h=2)[:, 4 * hb:4 * hb + 4, :]
        def par_view(t, q):
            return t[:].rearrange("p (a b) n -> p a b n", b=2)[:, :, q, :]
        for q in range(2):
            bk1 = pb()
            bk2 = pb()
            bk3 = pb()
            for i in range(2):
                h = heads[2 * i + q]
                p, hr = h // 2, slice(64 * (h % 2), 64 * (h % 2) + 64)
                rhs = ar[hr, p, :, :].rearrange("p a b -> p (a b)")
                mm(bk1[:, i * 256:(i + 1) * 256], bt[hr, p, :], rhs)
                mm(bk2[:, i * 256:(i + 1) * 256], kt[hr, p, :], rhs)
                mm(bk3[:, i * 128:(i + 1) * 128], ar[hr, p, 0, :], bt[hr, p, :])
            v1 = bk1.rearrange("p (h t n) -> p h t n", h=2, t=2)
            tt('dve', par_view(pp[0], q), v1[:, :, 0, :], MU2[:, 0:128].unsqueeze(1).to_broadcast([128, 2, 128]), ALU.mult)
            tt('dve', par_view(arb, q), v1[:, :, 1, :], MU2[:, 128:256].unsqueeze(1).to_broadcast([128, 2, 128]), ALU.mult)
            tt('dve', par_view(kab, q), bk2.rearrange("p (h n) -> p h n", h=2),
               MU2.unsqueeze(1).to_broadcast([128, 2, 256]), ALU.mult)
            tt('dve', par_view(nn[0], q), bk3[:, 0:256].rearrange("p (h n) -> p h n", h=2),
               SLm.unsqueeze(1).to_broadcast([128, 2, 128]), ALU.mult)
            yield
        tt('dve', ttb[:], pp[0][:], IDENT.unsqueeze(1).to_broadcast([128, 4, 128]), ALU.add)
        cp('act', axb[:, :, 0:64], h8(a0t))
        bkx = pb()
        for i, h in enumerate(heads):
            p, hr = h // 2, slice(64 * (h % 2), 64 * (h % 2) + 64)
            mm(bkx[:, i * 64:(i + 1) * 64], kab[:, i, 0:128], vtok[:, p, hr])
        cp('act', axb[:, :, 64:128], bkx[:, 0:256].rearrange("p (h d) -> p h d", h=4))
        yield
        ci = 0
        for lvl in range(1, 7):
            ncur, pcur, nnx, pnx = nn[ci], pp[ci], nn[1 - ci], pp[1 - ci]
            bka = pb()
            for i in range(4):
                mm(bka[:, i * 128:(i + 1) * 128], pcur[:, i, :], ncur[:, i, :])
            cp('act', nnx[:], bka.rearrange("p (h n) -> p h n", h=4))
            if lvl < 6:
                bkb = pb()
                for i in range(4):
                    mm(bkb[:, i * 128:(i + 1) * 128], ncur[:, i, :], pcur[:, i, :])
                cp('act', pnx[:], bkb.rearrange("p (h n) -> p h n", h=4))
            yield
            bkc = pb()
            for i in range(4):
                mm(bkc[:, i * 128:(i + 1) * 128], nnx[:, i, :], ttb[:, i, :])
            tt('dve', ttb[:], ttb[:], bkc.rearrange("p (h n) -> p h n", h=4), ALU.add)
            yield
            ci = 1 - ci
        bkw = pb()
        for i in range(4):
            mm(bkw[:, i * 128:(i + 1) * 128], ttb[:, i, :], axb[:, i, :])
        vw = bkw.rearrange("p (h n) -> p h n", h=4)
        cp('act', h8(WW), vw[:, :, 0:64])
        cp('dve', h8(UU), vw[:, :, 64:128])
        yield
        if own:
            bkq = pb()
            for i, h in enumerate(heads):
                p, hr = h // 2, slice(64 * (h % 2), 64 * (h % 2) + 64)
                mm(bkq[:, i * 128:i * 128 + 64], arb[:, i, :], WW[:, p, hr])
                mm(bkq[:, i * 128 + 64:(i + 1) * 128], arb[:, i, :], UU[:, p, hr], start=True, stop=False)
                mm(bkq[:, i * 128 + 64:(i + 1) * 128], kab[:, i, 128:256], vtok[:, p, hr], start=False, stop=True)
            vq = bkq.rearrange("p (h n) -> p h n", h=4)
            tt('dve', h8(QTOK), vq[:, :, 0:64], h8(r0t), ALU.add)
            cp('act', h8(Y0), vq[:, :, 64:128])
            yield

    def rwkv_pairs(c):
        own = c >= OWN0
        par = c % 2
        ar, kt, bt, vtok, a0t, r0t, ktt, btt = AR[par], KT[par], BT[par], VTOK[par], A0T[par], R0T[par], KTT[par], BTT[par]
        hs, hsn = HS[par], HS[1 - par]
        hsb, hsbn = HSb[par], HSb[1 - par]
        g127 = G127[par]
        bkm = pb()
        for p in range(4):
            mm(bkm[:, p * 128:(p + 1) * 128], IDB[:], IDB[:], start=True, stop=False)
            mm(bkm[:, p * 128:(p + 1) * 128], WW[:, p, :], btt[:, p, :], start=False, stop=True)
        tt('dve', MT[:], bkm.rearrange("p (a n) -> p a n", a=4), BLK.unsqueeze(1).to_broadcast([128, 4, 128]), ALU.mult)
        bkl = pb()
        for p in range(4):
            mm(bkl[:, p * 128:(p + 1) * 128], btt[:, p, :], UU[:, p, :], start=True, stop=False)
            mm(bkl[:, p * 128:(p + 1) * 128], ktt[:, p, :], vtok[:, p, :], start=False, stop=True)
        vl = bkl.rearrange("p (a n) -> p a n", a=4)
        for hh in range(2):
            hr = slice(64 * hh, 64 * hh + 64)
            tt('dve', LL[hr, :, :], vl[hr, :, 64 * hh:64 * hh + 64], g127[hr, :].unsqueeze(2).to_broadcast([64, 4, 64]), ALU.mult)
        yield
        if own:
            bkt = pb()
            for p in range(4):
                trb(bkt[:, p * 128:(p + 1) * 128], QTOK[:, p, :])
            cp('act', QT[:], bkt.rearrange("p (a n) -> p a n", a=4))
            Y4 = YTOK.rearrange("p (a h d) -> p a h d", a=4, h=2)
            for hh in range(2):
                hr = slice(64 * hh, 64 * hh + 64)
                bky = pb()
                for p in range(4):
                    mm(bky[:, p * 64:(p + 1) * 64], QT[hr, p, :], hsb[hr, p, :])
                tt('dve', Y4[:, :, hh, :], bky[:, 0:256].rearrange("p (a d) -> p a d", a=4), Y0[:, :, 64 * hh:64 * hh + 64], ALU.add)
            yield
        bkh = pb()
        for p in range(4):
            mm(bkh[:, p * 64:(p + 1) * 64], MT[:, p, :], hsb[:, p, :])
        tt('dve', hsn[:], bkh[:, 0:256].rearrange("p (a d) -> p a d", a=4), g127[:].unsqueeze(2).to_broadcast([128, 4, 64]), ALU.mult)
        tt('dve', hsn[:], hsn[:], LL[:], ALU.add)
        yield
        if c == 15:
            ts('dve', hsn[:], hsn[:], P_("flag"), ALU.mult)
        cp('act', hsbn[:], hsn[:])
        if own:
            Y3 = YTOK.rearrange("p (h d) -> p h d", h=8)
            rsum('dve', ST1[:], Y3)
            for h in range(8):
                kb.op('act', lambda: nc.scalar.activation(out=JUNK[:], in_=YTOK[:, h * 64:(h + 1) * 64], func=AF.Square,
                                                          accum_out=ST2[:, h:h + 1]), [YTOK], [JUNK[:], ST2[:]])
            ts('dve', ST1[:], ST1[:], 1.0 / 64, ALU.mult)
            tt('dve', ST3[:], ST1[:], ST1[:], ALU.mult)
            stt('dve', ST2[:], ST2[:], 1.0 / 64, ST3[:], ALU.mult, ALU.subtract)
            ts('dve', ST2[:], ST2[:], 64e-5, ALU.add)
            rsq(ST2[:])
            tt('dve', Y3, Y3, ST1[:].unsqueeze(2).to_broadcast([128, 8, 64]), ALU.subtract)
            tt('dve', Y3, Y3, ST2[:].unsqueeze(2).to_broadcast([128, 8, 64]), ALU.mult)
            tt('pool', YTOK, YTOK, R_("gn_w", 512), ALU.mult)
            tt('pool', YTOK, YTOK, R_("gn_b", 512), ALU.add)
            yield
            for h in range(8):
                p, hr = h // 2, slice(64 * (h % 2), 64 * (h % 2) + 64)
                stt('dve', YTOK[:, h * 64:(h + 1) * 64], vtok[:, p, hr], BON[par][:, h:h + 1], YTOK[:, h * 64:(h + 1) * 64],
                    ALU.mult, ALU.add)
            tt('pool', MIXTOK[:, 0:512], YTOK, GOUT[par][:], ALU.mult)
            yield

    def stage2_ssd(c):
        own = c >= OWN0
        par = c % 2
        dt_, da, cumc, xstok, btok, xbc = DT[par], DA[par], CUMC[par], XSTOK[par], BTOK[par], XBC[par]
        if own:
            act(ECUM[:], cumc[:], AF.Exp)
            for g in range(2):
                pcb = ph()
                mm(pcb[:, 0:128], xbc[:, g, :], xbc[:, 2 + g, :])
                tt('dve', CBM[:, g, :], pcb[:, 0:128], MUI, ALU.mult)
        hss, hssn = HSS[par], HSS[1 - par]
        for g in range(2):
            hs4 = slice(4 * g, 4 * g + 4)
            xs4 = xstok[:, g * 256:(g + 1) * 256].rearrange("p (h d) -> p h d", h=4)
            tt('dve', TD4[:], MUI.unsqueeze(1).to_broadcast([128, 4, 128]), da[:, hs4].unsqueeze(2).to_broadcast([128, 4, 128]), ALU.mult)
            bkr = pb()
            mm(bkr[:, :], ONES, TD4[:].rearrange("p h n -> p (h n)"))
            vr = bkr.rearrange("p (h n) -> p h n", h=4)
            tt('dve', SEG4[:], vr, cumc[:, hs4].unsqueeze(2).to_broadcast([128, 4, 128]), ALU.subtract)
            ts('dve', SEG4[:], SEG4[:], 0.0, ALU.add, 0.0, ALU.min)
            act(LD4[:], SEG4[:], AF.Exp)
            act(CDEC[:, hs4], vr[:, :, 127], AF.Exp)
            tt('dve', TE[:, hs4], LD4[:, :, 127], dt_[:, hs4], ALU.mult)
            tt('dve', XW[:, g * 256:(g + 1) * 256].rearrange("p (h d) -> p h d", h=4), xs4,
               TE[:, hs4].unsqueeze(2).to_broadcast([128, 4, 64]), ALU.mult)
            yield
            if own:
                tt('dve', WT4[:], LD4[:], dt_[:, hs4].unsqueeze(2).to_broadcast([128, 4, 128]), ALU.mult)
                tt('dve', WT4[:], WT4[:], CBM[:, g, :].unsqueeze(1).to_broadcast([128, 4, 128]), ALU.mult)
                bky = pb()
                for i in range(4):
                    h = 4 * g + i
                    mm(bky[:, i * 128:i * 128 + 64], WT4[:, i, :], xstok[:, h * 64:(h + 1) * 64])
                    mm(bky[:, i * 128 + 64:(i + 1) * 128], xbc[:, 2 + g, :], hss[:, h, :])
                vy = bky.rearrange("p (h n) -> p h n", h=4)
                tt('dve', YOFF4[:], vy[:, :, 64:128], ECUM[:, hs4].unsqueeze(2).to_broadcast([128, 4, 64]), ALU.mult)
                tt('dve', YS[:, g * 256:(g + 1) * 256].rearrange("p (h d) -> p h d", h=4), vy[:, :, 0:64], YOFF4[:], ALU.add)
                yield
            bks = pb()
            for i in range(4):
                h = 4 * g + i
                mm(bks[:, i * 64:(i + 1) * 64], btok[:, g * 128:(g + 1) * 128], XW[:, h * 64:(h + 1) * 64])
            tt('dve', hssn[:, hs4, :], hss[:, hs4, :], CDEC[:, hs4].unsqueeze(2).to_broadcast([128, 4, 64]), ALU.mult)
            tt('dve', hssn[:, hs4, :], hssn[:, hs4, :], bks[:, 0:256].rearrange("p (h d) -> p h d", h=4), ALU.add)
            yield
        if c == 15:
            ts('dve', hssn[:], hssn[:], P_("flag"), ALU.mult)
        if own:
            for h in range(8):
                stt('dve', YS[:, h * 64:(h + 1) * 64], xstok[:, h * 64:(h + 1) * 64], P_("dskp", 1, h), YS[:, h * 64:(h + 1) * 64],
                    ALU.mult, ALU.add)
            tt('dve', YS, YS, ZS[par][:], ALU.mult)
            tt('pool', XW[:], YS, YS, ALU.mult)
            rsum('dve', ST4[:], XW[:].rearrange("p (g d) -> p g d", g=2))
            ts('dve', ST4[:], ST4[:], 1.0 / 256, ALU.mult, 1e-6, ALU.add)
            rsq(ST4[:])
            for g in range(2):
                stt('dve', MIXTOK[:, 512 + g * 256:512 + (g + 1) * 256], YS[:, g * 256:(g + 1) * 256], ST4[:, g:g + 1],
                    R_("ssdn", 256, g * 256), ALU.mult, ALU.mult)
            yield

    def stage2_out(c):
        if c >= OWN0:
            oc = c - OWN0
            for j in range(8):
                pst = ph()
                tr(pst[:, 0:128], MIXTOK[:, j * 128:(j + 1) * 128], IDENT)
                cp('act', MXS[:, j, :], pst[:, 0:128])
            dma(mix_scr[:, :, oc * 128:(oc + 1) * 128], MXS[:])
        return
        yield

    def chain(*gens):
        for g in gens:
            for _ in g:
                yield

    def parallel(*gens):
        gens = list(gens)
        while gens:
            for g in list(gens):
                try:
                    next(g)
                except StopIteration:
                    gens.remove(g)
            yield

    def weighted(items):
        st = [[g, 0, float(n) * sp] for (g, n, sp) in items]
        while st:
            st.sort(key=lambda x: (x[1] + 1) / x[2])
            x = st[0]
            try:
                next(x[0])
                x[1] += 1
            except StopIteration:
                st.remove(x)

    dma(XT[:], xT_d[:, :, 0:128])
    norm_block(0)
    for c in range(NCH + 1):
        items = []
        if c >= 1:
            items.append((chain(parallel(rwkv_hb(c - 1, 0), rwkv_hb(c - 1, 1)), rwkv_pairs(c - 1)), 23, 1.0))
            items.append((stage2_ssd(c - 1), 7, 1.0))
        if c < NCH:
            items.append((stage1(c), 19, 1.0))
        weighted(items)
        if c >= 1:
            for _ in stage2_out(c - 1):
                pass

    kb.barrier()
    for t in reversed(p1):
        t.__exit__(None, None, None)
    if debug == "mix0":
        DBb = sb("DBb", [128, 8, TOWN], BF16)
        dma(DBb[:], mix_scr)
        DB = sb("DB", [128, 8, TOWN])
        cp('dve', DB[:], DBb[:])
        dma(dbg_d, DB[:])
        OUT = sb("OUTS", [128, 8, 2048])
        kb.op('dve', lambda: nc.vector.memset(OUT[:], 0.0), [], [OUT[:]])
        dma(out_d, OUT[:])
        kb.finish('sp')
        return nc

    TILES = [(0, 512), (512, 512), (1024, 512), (1536, 512), (2048, 128)]
    MTILES = [(128, 512), (640, 512), (1152, 512), (1664, 512)]
    HT = sb("HT", [128, 8, TOWN])
    ACTA = sb("ACTA", [128, 8, TOWN], BF16)
    WA = sb("WA", [128, 8, 1024], BF16)
    WB = sb("WB", [128, 8, 1024], BF16)
    SQT = sb("SQT", [128, 8, 128])
    RS = sb("RS", [128, 128])
    REP2 = sb("REP2", [128, 144])
    for t_ in (HT, ACTA):
        kb.split[t_.name] = (8 * TOWN, TOWN, 512)
    dma(REP2[:], rep_d[:, RC["qn"]:RC["qn"] + 144])
    dma(HT[:], xT_d[:, :, OWN0 * 128:NSLOT])
    dma(ACTA[:], mix_scr)

    def dbg_dump():
        dma(dbg_d, HT[:])
        kb.finish('sp')
        print("instructions:", kb.ninst, flush=True)
        return nc

    def rmsnorm_blk(gname, n, xnf=None):
        t0 = n * 128
        act(SQT[:], HT[:, :, t0:t0 + 128], AF.Square)
        ps = pb()
        for kc in range(8):
            mm(ps[:, 0:128], ONES, SQT[:, kc, :], start=(kc == 0), stop=(kc == 7))
        ts('dve', RS[:], ps[:, 0:128], 1.0 / DM, ALU.mult, 1e-6, ALU.add)
        rsq(RS[:])
        for kc in range(8):
            if xnf is None:
                stt('dve', ACTA[:, kc, t0:t0 + 128], HT[:, kc, t0:t0 + 128], P_(gname, 1, kc), RS[:], ALU.mult, ALU.mult)
            else:
                stt('dve', xnf[:, kc, :], HT[:, kc, t0:t0 + 128], P_(gname, 1, kc), RS[:], ALU.mult, ALU.mult)
                cp('act', ACTA[:, kc, t0:t0 + 128], xnf[:, kc, :])

    def ffn_passes(passes, WD, SGa, SGb, ACTH):
        def load_gu(ps_):
            f0, nf = ps_["f0"], ps_["nf"]
            for kc in range(8):
                kb.dma('pool', WA[:, kc, 0:nf * 128], ps_["wg"][kc * 128:(kc + 1) * 128, f0 * 128:(f0 + nf) * 128])
                kb.dma('pool', WB[:, kc, 0:nf * 128], ps_["wu"][kc * 128:(kc + 1) * 128, f0 * 128:(f0 + nf) * 128])
        load_gu(passes[0])
        for i, ps_ in enumerate(passes):
            f0, nf, tiles, rb = ps_["f0"], ps_["nf"], ps_["tiles"], ps_.get("rb")
            if ps_.get("pre") is not None:
                ps_["pre"]()
            for f in range(nf):
                kb.dma('pool', WD[:, f, :], ps_["wd"][(f0 + f) * 128:(f0 + f + 1) * 128, :])
            for (t0, tn) in tiles:
                for f in range(nf):
                    pg = pb()
                    for kc in range(8):
                        mm(pg[:, 0:tn], WA[:, kc, f * 128:(f + 1) * 128], ACTA[:, kc, t0:t0 + tn], start=(kc == 0), stop=(kc == 7))
                    pu = pb()
                    for kc in range(8):
                        mm(pu[:, 0:tn], WB[:, kc, f * 128:(f + 1) * 128], ACTA[:, kc, t0:t0 + tn], start=(kc == 0), stop=(kc == 7))
                    act(SGa[:, 0:tn], pg[:, 0:tn], AF.Silu)
                    if rb is None:
                        tt('dve', ACTH[:, f, t0:t0 + tn], pu[:, 0:tn], SGa[:, 0:tn], ALU.mult)
                    else:
                        tt('dve', SGb[:, 0:tn], pu[:, 0:tn], SGa[:, 0:tn], ALU.mult)
                        tt('dve', ACTH[:, f, t0:t0 + tn], SGb[:, 0:tn], rb[:, t0 - 128:t0 - 128 + tn], ALU.mult)
            if i + 1 < len(passes):
                load_gu(passes[i + 1])
            for (t0, tn) in tiles:
                for m in range(8):
                    ps = pb()
                    for f in range(nf):
                        mm(ps[:, 0:tn], WD[:, f, m * 128:(m + 1) * 128], ACTH[:, f, t0:t0 + tn], start=(f == 0), stop=(f == nf - 1))
                    tt('dve', HT[:, m, t0:t0 + tn], HT[:, m, t0:t0 + tn], ps[:, 0:tn], ALU.add)
                if ps_.get("post_tile") is not None:
                    ps_["post_tile"](t0, tn)

    def scope():
        lst = []

        def alloc(name, shape, dt=F32):
            t = nc.sbuf_tensor(name, list(shape), dt)
            h = t.__enter__()
            lst.append(t)
            return h

        def close():
            kb.barrier()
            for t in reversed(lst):
                t.__exit__(None, None, None)
        return alloc, close

    for kc in range(8):
        kb.dma('pool', WA[:, kc, :], w_out_d[kc * 128:(kc + 1) * 128, :])
    for (t0, tn) in TILES:
        for m in range(8):
            ps = pb()
            for kc in range(8):
                mm(ps[:, 0:tn], WA[:, kc, m * 128:(m + 1) * 128], ACTA[:, kc, t0:t0 + tn], start=(kc == 0), stop=(kc == 7))
            tt('dve', HT[:, m, t0:t0 + tn], HT[:, m, t0:t0 + tn], ps[:, 0:tn], ALU.add)
        if debug != "h1":
            for n in range(t0 // 128, (t0 + tn) // 128):
                rmsnorm_blk("g_ffn0", n)
    if debug == "h1":
        return dbg_dump()
    a2, close2 = scope()
    WD = a2("WD", [128, 8, 1024], BF16)
    ACTH2 = a2("ACTH2", [128, 8, TOWN], BF16)
    kb.split[ACTH2.name] = (8 * TOWN, TOWN, 512)
    SGa = a2("SGa", [128, 512])
    SGb = a2("SGb", [128, 512])
    def norm_mix1_tile(t0, tn):
        for n in range(t0 // 128, (t0 + tn) // 128):
            rmsnorm_blk("g_mix1", n)
    p2passes = [dict(wg=ffg_d, wu=ffu_d, wd=ffd_d, f0=f0, nf=nf, tiles=TILES) for (f0, nf) in ((0, 8), (8, 8), (16, 6))]
    if debug != "h2":
        p2passes[-1]["post_tile"] = norm_mix1_tile
    ffn_passes(p2passes, WD, SGa, SGb, ACTH2)
    close2()
    if debug == "h2":
        return dbg_dump()

    a3, close3 = scope()
    BQ = a3("a3_BQ", [128, 1536])
    dma(BQ[:], bq_d)
    POSI = a3("a3_POSI", [128, NOWN], I32)
    dma(POSI[:], pos_d)
    POSF = a3("a3_POSF", [128, NOWN])
    TFR = a3("a3_TFR", [128, NOWN, 8])
    TFI = a3("a3_TFI", [128, NOWN, 8], I32)
    TF2 = a3("a3_TF2", [128, NOWN, 8])
    S1 = a3("a3_S1", [128, NOWN, 8])
    COS = a3("a3_COS", [128, NOWN, 8])
    SIN = a3("a3_SIN", [128, NOWN, 8])
    FRQ = CST[:, C_FRQ:C_FRQ + 8]
    cp('dve', POSF[:], POSI[:])
    tt('dve', TFR[:], POSF[:].unsqueeze(2).to_broadcast([128, NOWN, 8]), FRQ.unsqueeze(1).to_broadcast([128, NOWN, 8]), ALU.mult)
    cp('dve', TFI[:], TFR[:])
    cp('dve', TF2[:], TFI[:])
    tt('dve', TFR[:], TFR[:], TF2[:], ALU.subtract)
    act(S1[:], TFR[:], AF.Sin, scale=float(np.pi))
    act(TF2[:], TFR[:], AF.Sin, scale=float(np.pi / 2))
    tt('dve', TF2[:], TF2[:], TF2[:], ALU.mult)
    ts('dve', TF2[:], TF2[:], -2.0, ALU.mult, 1.0, ALU.add)
    tt('dve', SIN[:], S1[:], TF2[:], ALU.mult)
    ts('dve', SIN[:], SIN[:], 2.0, ALU.mult)
    tt('dve', COS[:], S1[:], S1[:], ALU.mult)
    ts('dve', COS[:], COS[:], -2.0, ALU.mult, 1.0, ALU.add)
    QN = REP2[:, 0:64]
    KN = REP2[:, 64:128]
    SINK = REP2[:, 128:144]
    NEGB = a3("a3_NEGB", [128, 1])
    TMPB = a3("a3_TMPB", [128, 64])
    MXB = a3("a3_MXB", [128, 2])
    tt('dve', TMPB[:], QN, QN, ALU.mult)
    kb.op('dve', lambda: nc.vector.reduce_max(out=MXB[:, 0:1], in_=TMPB[:], axis=AX.X), [TMPB[:]], [MXB[:]])
    tt('dve', TMPB[:], KN, KN, ALU.mult)
    kb.op('dve', lambda: nc.vector.reduce_max(out=MXB[:, 1:2], in_=TMPB[:], axis=AX.X), [TMPB[:]], [MXB[:]])
    tt('dve', NEGB[:], MXB[:, 0:1], MXB[:, 1:2], ALU.mult)
    act(NEGB[:], NEGB[:], AF.Sqrt)
    ts('dve', NEGB[:], NEGB[:], -8.0, ALU.mult)
    ESK = a3("a3_ESK", [128, 16])
    act(ESK[:], SINK, AF.Exp, bias=NEGB[:])
    KT = a3("a3_KT", [128, 2, TOWN], BF16)
    VE = a3("a3_VE", [128, NOWN, 4, 65], BF16)
    kb.op('dve', lambda: nc.vector.memset(VE[:], 1.0), [], [VE[:]])
    QKVB = a3("a3_QKVB", [128, 1536])
    QSQ = a3("a3_QSQ", [128, 1024])
    RQ = a3("a3_RQ", [128, 20])
    RT = [a3("a3_RT%d" % i, [128, 16, 8]) for i in range(4)]
    QP = a3("a3_QP", [128, 8, 128])
    QT = a3("a3_QT", [128, 8, 128], BF16)
    PTF = a3("a3_PTF", [128, 2, 512])
    PT = a3("a3_PT", [128, 2, 512], BF16)
    DEN = a3("a3_DEN", [128, 4])
    OTOK = a3("a3_OTOK", [128, 1024])
    MASK2 = CST[:, C_M2:C_M2 + 256]
    for kc in range(8):
        kb.dma('pool', WA[:, kc, :], wqkv_d[kc * 128:(kc + 1) * 128, 0:1024])
        kb.dma('pool', WB[:, kc, 0:512], wqkv_d[kc * 128:(kc + 1) * 128, 1024:1536])

    def qk_norm_rope(e, X3, nh, gain, n, rq, sq, rts):
        sq3 = sq[:, 0:nh * 64].rearrange("p (h d) -> p h d", h=nh)
        tt(e, sq3, X3, X3, ALU.mult)
        rsum('dve', rq, sq3)
        ts(e, rq, rq, 1.0 / 64, ALU.mult, 1e-6, ALU.add)
        rsq(rq)
        tt(e, X3, X3, rq.unsqueeze(2).to_broadcast([128, nh, 64]), ALU.mult)
        tt(e, X3, X3, gain.unsqueeze(1).to_broadcast([128, nh, 64]), ALU.mult)
        c = COS[:, n, :].unsqueeze(1).to_broadcast([128, nh, 8])
        sn = SIN[:, n, :].unsqueeze(1).to_broadcast([128, nh, 8])
        x1, x2 = X3[:, :, 0:8], X3[:, :, 8:16]
        r0, r1, r2, r3 = [rts[i][:, 0:nh, :] for i in range(4)]
        tt(e, r0, x1, c, ALU.mult)
        tt(e, r1, x2, sn, ALU.mult)
        tt(e, r2, x2, c, ALU.mult)
        tt(e, r3, x1, sn, ALU.mult)
        tt(e, x1, r0, r1, ALU.subtract)
        tt(e, x2, r2, r3, ALU.add)

    QTs = [QT, a3("a3_QT1", [128, 8, 128], BF16)]
    KSQ = a3("a3_KSQ", [128, 256])
    RTK = [a3("a3_RTK%d" % i, [128, 4, 8]) for i in range(4)]

    def attn_A(n):
        t0 = n * 128
        qt = QTs[n % 2]
        cgs = (2,) if n == 0 else (2, 0, 1)
        for cg in cgs:
            ps = pb()
            for kc in range(8):
                w = WA[:, kc, cg * 512:(cg + 1) * 512] if cg < 2 else WB[:, kc, 0:512]
                mm(ps[:, :], ACTA[:, kc, t0:t0 + 128], w, start=(kc == 0), stop=(kc == 7))
            tt('dve', QKVB[:, cg * 512:(cg + 1) * 512], ps[:, :], BQ[:, cg * 512:(cg + 1) * 512], ALU.add)
            yield
        K3 = QKVB[:, 1024:1280].rearrange("p (h d) -> p h d", h=4)
        qk_norm_rope('dve', K3, 4, KN, n, RQ[:, 16:20], KSQ, RTK)
        cp('act', VE[:, n, :, 0:64], QKVB[:, 1280:1536].rearrange("p (h d) -> p h d", h=4))
        yield
        for u in range(2):
            pst = ph()
            tr(pst[:, 0:128], QKVB[:, 1024 + u * 128:1024 + (u + 1) * 128], IDENT)
            cp('act', KT[:, u, t0:t0 + 128], pst[:, 0:128])
        if n == 0:
            return
        Q3 = QKVB[:, 0:1024].rearrange("p (h d) -> p h d", h=16)
        qk_norm_rope('pool', Q3, 16, QN, n, RQ[:, 0:16], QSQ, RT)
        yield
        Q4 = QKVB[:, 0:1024].rearrange("p (kv g d) -> p kv g d", kv=4, g=4)
        for u in range(2):
            cp('pool', QP[:, u * 4:(u + 1) * 4, :].rearrange("p g (j d) -> p g j d", j=2),
               Q4[:, 2 * u:2 * u + 2, :, :].rearrange("p j g d -> p g j d"))
        yield
        for t in range(8):
            pst = ph()
            tr(pst[:, 0:128], QP[:, t, :], IDENT)
            cp('act', qt[:, t, :], pst[:, 0:128])
            if t == 3:
                yield
        yield

    def attn_B(n):
        t0 = n * 128
        qt = QTs[n % 2]
        for kv in range(4):
            u, half = kv // 2, kv % 2
            hr = slice(64 * half, 64 * half + 64)
            qrhs = qt[hr, u * 4:(u + 1) * 4, :].rearrange("p g n -> p (g n)")
            bkp = pb()
            mm(bkp[:, :], KT[hr, u, t0 - 128:t0], qrhs)
            bkc = pb()
            mm(bkc[:, :], KT[hr, u, t0:t0 + 128], qrhs)
            act(PTF[:, 0, :], bkp[:, :], AF.Exp, bias=NEGB[:], scale=0.125)
            act(PTF[:, 1, :], bkc[:, :], AF.Exp, bias=NEGB[:], scale=0.125)
            tt('dve', PT[:, 0, :].rearrange("p (g n) -> p g n", g=4), PTF[:, 0, :].rearrange("p (g n) -> p g n", g=4),
               MASK2[:, 0:128].unsqueeze(1).to_broadcast([128, 4, 128]), ALU.mult)
            tt('dve', PT[:, 1, :].rearrange("p (g n) -> p g n", g=4), PTF[:, 1, :].rearrange("p (g n) -> p g n", g=4),
               MASK2[:, 128:256].unsqueeze(1).to_broadcast([128, 4, 128]), ALU.mult)
            if n == 1:
                ts('dve', PT[:, 0, :], PT[:, 0, :], P_("flag"), ALU.mult)
            yield
            pso = pb()
            for g in range(4):
                mm(pso[:, g * 65:(g + 1) * 65], PT[:, 0, g * 128:(g + 1) * 128], VE[:, n - 1, kv, :], start=True, stop=False)
                mm(pso[:, g * 65:(g + 1) * 65], PT[:, 1, g * 128:(g + 1) * 128], VE[:, n, kv, :], start=False, stop=True)
            vo = pso[:, 0:260].rearrange("p (g d) -> p g d", g=4)
            tt('dve', DEN[:], vo[:, :, 64], ESK[:, kv * 4:(kv + 1) * 4], ALU.add)
            kb.op('dve', lambda: nc.vector.reciprocal(out=DEN[:], in_=DEN[:]), [DEN[:]], [DEN[:]])
            tt('dve', OTOK[:, kv * 256:(kv + 1) * 256].rearrange("p (g d) -> p g d", g=4), vo[:, :, 0:64],
               DEN[:].unsqueeze(2).to_broadcast([128, 4, 64]), ALU.mult)
            yield
        for j in range(8):
            pst = ph()
            tr(pst[:, 0:128], OTOK[:, j * 128:(j + 1) * 128], IDENT)
            cp('act', ACTA[:, j, t0 - 128:t0], pst[:, 0:128])
            if j == 3:
                yield
        yield

    def weighted3(items):
        st = [[g, 0, float(n_)] for (g, n_) in items]
        while st:
            st.sort(key=lambda x: (x[1] + 1) / x[2])
            x = st[0]
            try:
                next(x[0])
                x[1] += 1
            except StopIteration:
                st.remove(x)

    for n in range(NOWN + 1):
        items = []
        if n - 1 >= 1:
            items.append((attn_B(n - 1), 10))
        if n < NOWN:
            items.append((attn_A(n), 9))
        weighted3(items)
    for kc in range(8):
        kb.dma('pool', WA[:, kc, :], wo_d[kc * 128:(kc + 1) * 128, :])
    for (t0, tn) in MTILES:
        for m in range(8):
            ps = pb()
            for kc in range(8):
                mm(ps[:, 0:tn], WA[:, kc, m * 128:(m + 1) * 128], ACTA[:, kc, t0 - 128:t0 - 128 + tn], start=(kc == 0), stop=(kc == 7))
            stt('dve', HT[:, m, t0:t0 + tn], ps[:, 0:tn], P_("b_o", 1, m), HT[:, m, t0:t0 + tn], ALU.add, ALU.add)
    close3()
    if debug == "h3":
        return dbg_dump()

    a4, close4 = scope()
    WD = a4("WD4", [128, 8, 1024], BF16)
    ACTH4 = a4("ACTH4", [128, 6, TOWN], BF16)
    kb.split[ACTH4.name] = (6 * TOWN, TOWN, 512)
    SGa = a4("SGa4", [128, 512])
    SGb = a4("SGb4", [128, 512])
    XNF = a4("XNF", [128, 8, 128])
    WR = a4("WR", [128, 8, 8])
    dma(WR[:], wr_d.rearrange("(kc p) e -> p kc e", p=128))
    LG = a4("LG", [128, 8])
    MX8 = a4("MX8", [128, 8])
    NV1 = a4("NV1", [128, 1])
    MSK = a4("MSK", [128, 8])
    EX = a4("EX", [128, 8])
    SME = a4("SME", [128, 1])
    COMB = a4("COMB", [128, 16, 8])
    DG = a4("DG", [128, 128])
    RBs = [a4("RB%d" % i, [128, 2048], BF16) for i in range(2)]
    for n in range(1, NOWN):
        rmsnorm_blk("g_ffn1", n, xnf=XNF)
        ps = ph()
        for kc in range(8):
            mm(ps[:, 0:8], XNF[:, kc, :], WR[:, kc, :], start=(kc == 0), stop=(kc == 7))
        cp('dve', LG[:], ps[:, 0:8])
        kb.op('dve', lambda: nc.vector.max(out=MX8[:], in_=LG[:]), [LG[:]], [MX8[:]])
        ts('dve', MSK[:], LG[:], MX8[:, 1:2], ALU.is_ge)
        ts('dve', NV1[:], MX8[:, 0:1], -1.0, ALU.mult)
        act(EX[:], LG[:], AF.Exp, bias=NV1[:])
        tt('dve', EX[:], EX[:], MSK[:], ALU.mult)
        rsum('dve', SME[:], EX[:])
        kb.op('dve', lambda: nc.vector.reciprocal(out=SME[:], in_=SME[:]), [SME[:]], [SME[:]])
        ts('dve', COMB[:, n - 1, :], EX[:], SME[:], ALU.mult)
    def build_rb(e):
        RB = RBs[e % 2]
        for q4 in range(4):
            ps = pb()
            for b in range(4):
                blk = q4 * 4 + b
                ts('dve', DG[:], IDENT, COMB[:, blk, e:e + 1], ALU.mult)
                mm(ps[:, b * 128:(b + 1) * 128], ONES, DG[:])
            cp('act', RB[:, q4 * 512:(q4 + 1) * 512], ps[:, :])

    passes = []
    for e in range(NEXP):
        passes.append(dict(wg=eg_d[e], wu=eu_d[e], wd=ed_d[e], f0=0, nf=6, tiles=MTILES, rb=RBs[e % 2]))
        passes.append(dict(wg=eg_d[e], wu=eu_d[e], wd=ed_d[e], f0=6, nf=5, tiles=MTILES, rb=RBs[e % 2],
                           pre=(lambda e=e: build_rb(e + 1)) if e + 1 < NEXP else None))
    build_rb(0)
    ffn_passes(passes, WD, SGa, SGb, ACTH4)
    if debug == "h4":
        return dbg_dump()
    dma(out_d, HT[:, :, 128:TOWN])
    kb.finish('sp')
    print("instructions:", kb.ninst, flush=True)
    return nc


def _consts():
    i = np.arange(128)
    cst = np.zeros((128, NCST), np.float32)
    cst[:, C_ID:C_ID + 128] = np.eye(128)
    mui = (i[None, :] >= i[:, None]).astype(np.float32)
    mus = (i[None, :] > i[:, None]).astype(np.float32)
    cst[:, C_MUI:C_MUI + 128] = mui
    cst[:, C_MUS:C_MUS + 128] = mus
    cst[:, C_MUI2:C_MUI2 + 128] = mui
    cst[:, C_SL:C_SL + 128] = (i[None, :] < i[:, None])
    cst[:, C_ONES:C_ONES + 128] = 1.0
    blk = (i[None, :] // 64 == i[:, None] // 64).astype(np.float32)
    cst[:, C_BLK:C_BLK + 128] = blk
    cst[:, C_HSEL] = (i < 64)
    cst[:, C_HSEL + 1] = (i >= 64)
    cst[:, C_M2:C_M2 + 128] = (i[None, :] < i[:, None])
    cst[:, C_M2 + 128:C_M2 + 256] = mui
    inv_freq = 500000.0 ** (-np.arange(0, 16, 2, dtype=np.float64) / 16.0)
    cst[:, C_FRQ:C_FRQ + 8] = (inv_freq / (2 * np.pi))[None, :]
    return cst


def _fm(v, n):
    return np.ascontiguousarray(np.asarray(v, np.float32).reshape(n, 128).T)


def _prepare(inp):
    f = lambda k: np.asarray(inp[k], np.float32)
    prm = np.zeros((128, NPRM), np.float32)
    prm[:, PC["g_mix0"]:PC["g_mix0"] + 8] = _fm(f("ev_norm_mix")[0], 8)
    prm[:, PC["g_ffn0"]:PC["g_ffn0"] + 8] = _fm(f("ev_norm_ffn")[0], 8)
    prm[:, PC["g_mix1"]:PC["g_mix1"] + 8] = _fm(f("od_norm_mix")[0], 8)
    prm[:, PC["g_ffn1"]:PC["g_ffn1"] + 8] = _fm(f("od_norm_ffn")[0], 8)
    prm[:, PC["a0"]:PC["a0"] + 4] = _fm(f("ev_a0")[0], 4)
    cw = f("ev_conv_w")[0]
    prm[:, PC["conv_w"]:PC["conv_w"] + 32] = cw.T.reshape(8, 128, 4).transpose(1, 0, 2).reshape(128, 32)
    prm[:, PC["conv_b"]:PC["conv_b"] + 8] = _fm(f("ev_conv_b")[0], 8)
    prm[:, PC["b_qkv"]:PC["b_qkv"] + 12] = _fm(f("od_b_qkv")[0], 12)
    prm[:, PC["b_o"]:PC["b_o"] + 8] = _fm(f("od_b_o")[0], 8)
    rep = np.zeros((128, NREP), np.float32)

    def setrep(n, v):
        v = np.asarray(v, np.float32).reshape(-1)
        rep[:, RC[n]:RC[n] + v.size] = v[None, :]

    setrep("w0", f("ev_w0")[0])
    setrep("gn_w", f("ev_gn_w")[0])
    setrep("gn_b", f("ev_gn_b")[0])
    setrep("ssdn", f("ev_ssd_norm")[0])
    prm[:, PC["dskp"]:PC["dskp"] + 8] = f("ev_d_skip")[0][None, :]
    setrep("dtb", f("ev_dt_bias")[0])
    setrep("alog", f("ev_a_log")[0])
    setrep("qn", f("od_q_norm")[0])
    setrep("kn", f("od_k_norm")[0])
    setrep("sink", f("od_sinks")[0])
    bc = _fm(f("ev_mu_shift")[0], 14)
    prm[:, PC["kk"]:PC["kk"] + 4] = _fm(f("ev_k_k")[0], 4)
    prm[:, PC["ka"]:PC["ka"] + 4] = _fm(f("ev_k_a")[0], 4)
    prm[:, PC["rk"]:PC["rk"] + 4] = _fm(f("ev_r_k")[0].reshape(-1), 4)
    shared = {
        "rep": rep, "cst": _consts(), "bc": bc,
        "w_in": np.ascontiguousarray(f("ev_w_in")[0]),
        "wdu": np.ascontiguousarray(f("ev_w_decay_up")[0]),
        "wiu": np.ascontiguousarray(f("ev_w_iclr_up")[0]),
        "wgu": np.ascontiguousarray(f("ev_w_gate_up")[0]),
        "w_out": np.ascontiguousarray(f("ev_w_out")[0]),
        "ffg": np.ascontiguousarray(f("ev_ffn_gate")[0]),
        "ffu": np.ascontiguousarray(f("ev_ffn_up")[0]),
        "ffd": np.ascontiguousarray(f("ev_ffn_down")[0]),
        "wqkv": np.ascontiguousarray(f("od_w_qkv")[0]),
        "wo": np.ascontiguousarray(f("od_w_o")[0]),
        "bq": np.ascontiguousarray(np.broadcast_to(f("od_b_qkv")[0][None, :], (128, 1536))),
        "wr": np.ascontiguousarray(f("od_router")[0]),
        "eg": np.ascontiguousarray(f("od_exp_gate")[0]),
        "eu": np.ascontiguousarray(f("od_exp_up")[0]),
        "ed": np.ascontiguousarray(f("od_exp_down")[0]),
    }
    positions = np.asarray(inp["positions"]).astype(np.int32)
    x = f("x")
    maps = []
    for c in range(8):
        b, half = c // 2, c % 2
        xs = np.zeros((NSLOT, DM), np.float32)
        if half == 1:
            xs[:] = x[b]
        else:
            xs[2048:] = x[b, :2048]
        xT = np.ascontiguousarray(xs.T.reshape(8, 128, NSLOT).transpose(1, 0, 2))
        p = prm.copy()
        p[:, PC["flag"]] = float(half)
        m = dict(shared)
        m["xT"] = xT
        m["prm"] = p
        ps_ = np.zeros((NSLOT,), np.int32)
        if half == 1:
            ps_[:] = positions[b]
        else:
            ps_[2048:] = positions[b, :2048]
        m["pos"] = np.ascontiguousarray(ps_[OWN0 * 128:].reshape(NOWN, 128).T)
        maps.append(m)
    return maps


def kernel(**inputs):
    debug = os.environ.get("KDEBUG", "")
    maps = _prepare(inputs)
    nc = build_nc(debug=debug)
    res = run_bass_kernel_spmd(nc, maps, core_ids=list(range(8)))
    out = np.zeros((4, 4096, DM), np.float32)
    for c in range(8):
        b, half = c // 2, c % 2
        oT = res.results[c]["outT"]
        out[b, half * 2048:(half + 1) * 2048] = oT.transpose(2, 1, 0).reshape(2048, DM)
    if debug:
        kernel.dbg = [res.results[c]["dbg"] for c in range(8)]
    return out
```

```python
import os
import numpy as np
import concourse.bass as bass
import concourse.mybir as mybir
from concourse.bass_utils import run_bass_kernel_spmd

F32 = mybir.dt.float32
BF16 = mybir.dt.bfloat16
AF = mybir.ActivationFunctionType
ALU = mybir.AluOpType
AX = mybir.AxisListType

DM = 1024
NSLOT = 4096
NCH = 32
OWN0 = 15
NOWN = NCH - OWN0
TOWN = NOWN * 128
IN0 = 3336
FF0 = 2816
FFE = 1408
NEXP = 8
LWS = 0.6065306597126334

PC = {}
_o = 0
for _n, _w in [("g_mix0", 8), ("g_ffn0", 8), ("g_mix1", 8), ("g_ffn1", 8), ("a0", 4), ("kk", 4), ("ka", 4), ("rk", 4), ("conv_w", 32), ("conv_b", 8),
               ("b_qkv", 12), ("b_o", 8), ("flag", 1), ("dskp", 8)]:
    PC[_n] = _o
    _o += _w
NPRM = _o
RC = {}
_o = 0
for _n, _w in [("w0", 512), ("gn_w", 512), ("gn_b", 512), ("ssdn", 512), ("dtb", 8), ("alog", 8),
               ("qn", 64), ("kn", 64), ("sink", 16)]:
    RC[_n] = _o
    _o += _w
NREP = _o
C_ID, C_MUI, C_MUS, C_MUI2, C_SL, C_ONES, C_BLK, C_HSEL = 0, 128, 256, 384, 512, 640, 768, 896
C_M2 = 898
C_FRQ = 1154
NCST = 1162
I32 = mybir.dt.int32


class KB:
    def __init__(self, nc, ndma=16):
        self.nc = nc
        self.E = {'pe': nc.tensor, 'dve': nc.vector, 'act': nc.scalar, 'pool': nc.gpsimd, 'sp': nc.sync}
        self.sem = {e: nc.alloc_semaphore('s_' + e) for e in self.E}
        self.cnt = {e: 0 for e in self.E}
        self.seen = {e: {} for e in self.E}
        self.lastw = {}
        self.reads = {}
        self.ndma = ndma
        self.dsem = [nc.alloc_semaphore('d_%d' % i) for i in range(ndma)]
        self.dcnt = [0] * ndma
        self.dn = 0
        self.dnq = {}
        self.ninst = 0
        self.split = {}

    def _semof(self, key):
        return self.sem[key] if isinstance(key, str) else self.dsem[key]

    def _wait(self, e, key, val):
        if val <= 0 or self.seen[e].get(key, 0) >= val:
            return
        if key == e and e == 'pe':
            return
        self.E[e].wait_ge(self._semof(key), val)
        self.seen[e][key] = val

    def _deps(self, e, ins, outs):
        for n in ins:
            w = self.lastw.get(n)
            if w is not None:
                self._wait(e, w[0], w[1])
        for n in outs:
            w = self.lastw.get(n)
            if w is not None:
                self._wait(e, w[0], w[1])
            for (k, v) in self.reads.get(n, ()):
                self._wait(e, k, v)

    def _commit(self, key, val, ins, outs):
        for n in ins:
            self.reads.setdefault(n, []).append((key, val))
        for n in outs:
            self.lastw[n] = (key, val)
            self.reads[n] = []

    def keys(self, a):
        n = a.tensor.name
        sp = self.split.get(n)
        if sp is None:
            return [n]
        row, inner, piece = sp
        col = (a.offset % row) % inner
        ext = 1
        for (st, cn) in list(a.ap)[1:]:
            if st < inner:
                ext += (cn - 1) * st
        lo, hi = col // piece, min(col + ext - 1, inner - 1) // piece
        return ["%s#%d" % (n, i) for i in range(lo, hi + 1)]

    def op(self, e, fn, ins, outs, inc=True):
        xs = [k for a in ins if a.tensor.name in self.split for k in self.keys(a)]
        ins = [k for a in ins for k in self.keys(a)]
        outs = [k for a in outs for k in self.keys(a)] + xs
        self._deps(e, ins, outs)
        inst = fn()
        if inc:
            self.cnt[e] += 1
            inst.then_inc(self.sem[e], 1)
            val = self.cnt[e]
        else:
            val = self.cnt[e] + 1
        self._commit(e, val, ins, outs)
        self.ninst += 1
        return inst

    def dma(self, e, out, in_, **kw):
        half = self.ndma // 2
        base = half if e == 'pool' else 0
        k = self.dnq.get(e, 0)
        self.dnq[e] = k + 1
        slot = base + (k % half)
        self._wait(e, slot, self.dcnt[slot])
        ins = self.keys(in_)
        outs = self.keys(out)
        self._deps(e, ins, outs)
        inst = self.E[e].dma_start(out=out, in_=in_, **kw)
        self.dcnt[slot] += 16
        inst.then_inc(self.dsem[slot], 16)
        self._commit(slot, self.dcnt[slot], ins, outs)
        self.ninst += 1
        return inst

    def barrier(self):
        for e in self.E:
            for s in range(self.ndma):
                self._wait(e, s, self.dcnt[s])
            for k in self.E:
                if k != e:
                    self._wait(e, k, self.cnt[k])
        for e in ('dve', 'act', 'pool'):
            self._wait(e, e, self.cnt[e])

    def finish(self, e='sp'):
        for s in range(self.ndma):
            self._wait(e, s, self.dcnt[s])
        for k in self.E:
            if k != e:
                self._wait(e, k, self.cnt[k])


def build_nc(debug=""):
    nc = bass.Bass("TRN2", target_bir_lowering=False)
    kb = KB(nc)

    def din(name, shape, dt=F32):
        return nc.dram_tensor(name, list(shape), dt, kind="ExternalInput").ap()

    xT_d = din("xT", [128, 8, NSLOT])
    prm_d = din("prm", [128, NPRM])
    rep_d = din("rep", [128, NREP])
    cst_d = din("cst", [128, NCST])
    bc_d = din("bc", [128, 14])
    w_in_d = din("w_in", [DM, IN0])
    wdu_d = din("wdu", [64, 512])
    wiu_d = din("wiu", [64, 512])
    wgu_d = din("wgu", [128, 512])
    w_out_d = din("w_out", [DM, DM])
    ffg_d = din("ffg", [DM, FF0])
    ffu_d = din("ffu", [DM, FF0])
    ffd_d = din("ffd", [FF0, DM])
    wqkv_d = din("wqkv", [DM, 1536])
    wo_d = din("wo", [DM, DM])
    bq_d = din("bq", [128, 1536])
    pos_d = din("pos", [128, NOWN], I32)
    wr_d = din("wr", [DM, NEXP])
    eg_d = din("eg", [NEXP, DM, FFE])
    eu_d = din("eu", [NEXP, DM, FFE])
    ed_d = din("ed", [NEXP, FFE, DM])
    out_d = nc.dram_tensor("outT", [128, 8, 2048], F32, kind="ExternalOutput").ap()
    dbg_d = None
    if debug:
        dbg_d = nc.dram_tensor("dbg", [128, 8, TOWN], F32, kind="ExternalOutput").ap()

    V = {'dve': nc.vector, 'pool': nc.gpsimd}
    rr = {'el': 0, 'ev': 0, 'ph': 0, 'pb': 0, 'dq': 0}

    def el():
        rr['el'] ^= 1
        return 'dve' if rr['el'] else 'pool'

    def ev():
        rr['ev'] ^= 1
        return 'dve' if rr['ev'] else 'act'

    def aps(*xs):
        return [x for x in xs if hasattr(x, 'tensor')]

    def mm(out, lhsT, rhs, start=True, stop=True):
        kb.op('pe', lambda: nc.tensor.matmul(out, lhsT=lhsT, rhs=rhs, start=start, stop=stop), [lhsT, rhs], [out], inc=bool(stop))

    def tr(out, in_, ident):
        kb.op('pe', lambda: nc.tensor.transpose(out, in_, ident), [in_, ident], [out])

    def tt(e, out, a, b, op):
        kb.op(e, lambda: V[e].tensor_tensor(out=out, in0=a, in1=b, op=op), [a, b], [out])

    def ts(e, out, a, s1, op0, s2=None, op1=None):
        if op1 is None:
            kb.op(e, lambda: V[e].tensor_scalar(out=out, in0=a, scalar1=s1, scalar2=None, op0=op0), aps(a, s1), [out])
        else:
            kb.op(e, lambda: V[e].tensor_scalar(out=out, in0=a, scalar1=s1, scalar2=s2, op0=op0, op1=op1),
                  aps(a, s1, s2), [out])

    def stt(e, out, a, s, b, op0, op1):
        e = 'dve'
        kb.op(e, lambda: V[e].scalar_tensor_tensor(out=out, in0=a, scalar=s, in1=b, op0=op0, op1=op1),
              aps(a, s, b), [out])

    def act(out, in_, func, bias=None, scale=1.0):
        if bias is None:
            kb.op('act', lambda: nc.scalar.activation(out=out, in_=in_, func=func, scale=scale), aps(in_, scale), [out])
        else:
            kb.op('act', lambda: nc.scalar.activation(out=out, in_=in_, func=func, bias=bias, scale=scale),
                  aps(in_, bias, scale), [out])

    def rsq(x):
        act(x, x, AF.Ln)
        act(x, x, AF.Exp, scale=-0.5)

    def cp(e, out, in_):
        if e == 'act':
            kb.op('act', lambda: nc.scalar.copy(out=out, in_=in_), [in_], [out])
        else:
            kb.op(e, lambda: V[e].tensor_copy(out=out, in_=in_), [in_], [out])

    def rsum(e, out, in_):
        kb.op(e, lambda: V[e].reduce_sum(out=out, in_=in_, axis=AX.X), [in_], [out])

    def dma(out, in_, q=None):
        kb.dma(q or 'sp', out, in_)

    def sb(name, shape, dt=F32):
        return nc.alloc_sbuf_tensor(name, list(shape), dt)

    BANKS = [nc.alloc_psum_tensor("PS%d" % i, [128, 512], F32) for i in range(8)]
    for b_ in BANKS:
        kb.split[b_.name] = (512, 512, 512)
    def pb():
        rr['pb'] = (rr['pb'] + 1) % 8
        return BANKS[rr['pb']][:, :]

    def ph():
        return pb()[:, 0:256]

    PRM = sb("PRM", [128, NPRM])
    CST = sb("CST", [128, NCST])
    mix_scr = nc.dram_tensor("mix_scr", [128, 8, TOWN], BF16).ap()
    dma(PRM[:], prm_d)
    dma(CST[:], cst_d)
    IDENT = CST[:, C_ID:C_ID + 128]
    MUI = CST[:, C_MUI:C_MUI + 128]
    TRI2 = CST[:, C_MUI:C_MUI + 256]
    MU2 = CST[:, C_MUS:C_MUS + 256]
    SLm = CST[:, C_SL:C_SL + 128]
    ONES = CST[:, C_ONES:C_ONES + 128]
    BLK = CST[:, C_BLK:C_BLK + 128]
    HSEL = CST[:, C_HSEL:C_HSEL + 2]

    def P_(n, w=1, o=0):
        return PRM[:, PC[n] + o:PC[n] + o + w]

    def R_(n, w, o=0):
        return REP[:, RC[n] + o:RC[n] + o + w]

    p1 = []

    def sb1(name, shape, dt=F32):
        t = nc.sbuf_tensor(name, list(shape), dt)
        h = t.__enter__()
        p1.append(t)
        return h

    REP = sb1("REP", [128, NREP])
    dma(REP[:], rep_d)
    WIN = sb1("WIN", [128, 8, IN0], BF16)
    for kc in range(8):
        kb.dma('pool', WIN[:, kc, :], w_in_d[kc * 128:(kc + 1) * 128, :])
    WDU = sb1("WDU", [64, 512], BF16)
    kb.dma('pool', WDU[:], wdu_d)
    WIU = sb1("WIU", [128, 512], BF16)
    kb.dma('pool', WIU[64:128, :], wiu_d)
    WGU = sb1("WGU", [128, 512], BF16)
    kb.dma('pool', WGU[:], wgu_d)
    MUC = sb1("MUC", [128, 14])
    dma(MUC[:], bc_d)
    IDB = sb1("IDB", [128, 128], BF16)
    cp('dve', IDB[:], IDENT)
    AREP = sb1("AREP", [128, 8])
    act(AREP[:], R_("alog", 8), AF.Exp)
    ts('dve', AREP[:], AREP[:], -1.0, ALU.mult)

    XT = sb1("XT0", [128, 8, 128])
    RSTD = sb1("RSTD", [128, 128])
    XN = sb1("XN", [128, 8, 128], BF16)
    PA = sb1("PA0", [128, 14, 129])
    XB = sb1("XB0", [128, 8, 131])
    DTMP = sb1("DTMP", [128, 14, 128])
    PL = DTMP
    SQ = DTMP
    CT2 = sb1("CT2", [128, 4, 128])
    XC = sb1("XC", [128, 4, 128])
    TW = sb1("TW", [64, 128], BF16)
    ALB = sb1("ALB", [128, 128], BF16)
    SGL = sb1("SGL", [128, 128], BF16)
    SG = sb1("SG", [128, 512])
    ICLR = sb1("ICLR", [128, 4, 128])
    KQ = sb1("KQ", [128, 4, 128])
    SQK = sb1("SQK", [128, 4, 128])
    RN = sb1("RN", [128, 4, 128])
    KKn = sb1("KKn", [128, 4, 128])
    KM = sb1("KM", [128, 4, 128])
    T1 = SQK
    Bv = KQ
    RKR = RN
    E1 = sb1("E1", [128, 4, 128])
    E0 = sb1("E0", [128, 4, 128])
    EI = sb1("EI", [128, 4, 128])
    def dbl(name, shape, dt=F32):
        return [sb1("%s_%d" % (name, i), shape, dt) for i in range(2)]
    AR = dbl("AR", [128, 4, 2, 128], BF16)
    KT = dbl("KT", [128, 4, 128], BF16)
    BT = dbl("BT", [128, 4, 128], BF16)
    VTOK = dbl("VTOK", [128, 4, 128], BF16)
    A0T = dbl("A0T", [128, 4, 128], BF16)
    R0T = dbl("R0T", [128, 4, 128], BF16)
    KTT = dbl("KTT", [128, 4, 128], BF16)
    BTT = dbl("BTT", [128, 4, 128], BF16)
    G127 = dbl("G127", [128, 4])
    GOUT = dbl("GOUT", [128, 512])
    BON = dbl("BON", [128, 8])
    ZS = dbl("ZS", [128, 512])
    XSTOK = dbl("XSTOK", [128, 512])
    BTOK = dbl("BTOK", [128, 256])
    XBC = dbl("XBC", [128, 4, 128])
    DT = dbl("DT", [128, 8])
    DA = dbl("DA", [128, 8])
    CUMC = dbl("CUMC", [128, 8])
    NN = [[sb1("NN%d_%d" % (hb, i), [128, 4, 128], BF16) for i in range(2)] for hb in range(2)]
    PP = [[sb1("PP%d_%d" % (hb, i), [128, 4, 128], BF16) for i in range(2)] for hb in range(2)]
    TTb = [sb1("TTb%d" % hb, [128, 4, 128], BF16) for hb in range(2)]
    ARB = [sb1("ARB%d" % hb, [128, 4, 128], BF16) for hb in range(2)]
    KAb = [sb1("KAb%d" % hb, [128, 4, 256], BF16) for hb in range(2)]
    AXb = [sb1("AXb%d" % hb, [128, 4, 128], BF16) for hb in range(2)]
    WW = sb1("WW", [128, 4, 128], BF16)
    UU = sb1("UU", [128, 4, 128], BF16)
    QTOK = sb1("QTOK", [128, 4, 128], BF16)
    Y0 = sb1("Y0", [128, 4, 128])
    QT = sb1("QT", [128, 4, 128], BF16)
    MT = sb1("MT", [128, 4, 128], BF16)
    LL = sb1("LL", [128, 4, 64])
    HS = [sb1("HS%d" % i, [128, 4, 64]) for i in range(2)]
    HSb = [sb1("HSb%d" % i, [128, 4, 64], BF16) for i in range(2)]
    ST1 = sb1("ST1", [128, 8])
    ST2 = sb1("ST2", [128, 8])
    ST3 = sb1("ST3", [128, 8])
    MIXTOK = sb1("MIXTOK", [128, 1024])
    YTOK = MIXTOK[:, 0:512]
    YS = MIXTOK[:, 512:1024]
    MXS = sb1("MXS", [128, 8, 128], BF16)
    ECUM = sb1("ECUM", [128, 8])
    CDEC = sb1("CDEC", [128, 8])
    TE = sb1("TE", [128, 8])
    CBM = sb1("CBM", [128, 2, 128])
    TD4 = sb1("TD4", [128, 4, 128])
    SEG4 = TD4
    LD4 = sb1("LD4", [128, 4, 128])
    WT4 = LD4
    XW = sb1("XW", [128, 512])
    JUNK = sb1("JUNK", [128, 64], BF16)
    YOFF4 = sb1("YOFF4", [128, 4, 64])
    ST4 = sb1("ST4", [128, 2])
    HSS = [sb1("HSS%d" % i, [128, 8, 64]) for i in range(2)]

    kb.op('dve', lambda: nc.vector.memset(HS[0][:], 0.0), [], [HS[0][:]])
    kb.op('dve', lambda: nc.vector.memset(HSb[0][:], 0.0), [], [HSb[0][:]])
    kb.op('dve', lambda: nc.vector.memset(HSS[0][:], 0.0), [], [HSS[0][:]])
    kb.op('pool', lambda: nc.gpsimd.memset(PA[:], 0.0), [], [PA[:]])
    kb.op('pool', lambda: nc.gpsimd.memset(XB[:], 0.0), [], [XB[:]])

    def fgrp(g):
        if g < 14:
            return g * 128
        return 1792 + 512 + (g - 14) * 128

    def trb(out, in_):
        mm(out, in_, IDB[:])

    def norm_block(c):
        act(SQ[:, 0:8, :], XT[:], AF.Square)
        ps = ph()
        for kc in range(8):
            mm(ps[:, 0:128], ONES, SQ[:, kc, :], start=(kc == 0), stop=(kc == 7))
        ts('dve', RSTD[:], ps[:, 0:128], 1.0 / DM, ALU.mult, 1e-6, ALU.add)
        rsq(RSTD[:])
        tt('pool', SQ[:, 0:8, :], XT[:], P_("g_mix0", 8).unsqueeze(2).to_broadcast([128, 8, 128]), ALU.mult)
        tt('pool', XN[:], SQ[:, 0:8, :], RSTD[:].unsqueeze(1).to_broadcast([128, 8, 128]), ALU.mult)

    def stage1(c):
        own = c >= OWN0
        par = c % 2
        pa, xb = PA, XB
        R4 = PL[:, 0:4, :]
        K4 = PL[:, 4:8, :]
        V4 = PL[:, 8:12, :]
        cp('pool', pa[:, :, 0:1], pa[:, :, 128:129])
        cp('pool', xb[:, :, 0:3], xb[:, :, 128:131])
        for g0 in range(0, 22, 4):
            gs = list(range(g0, min(g0 + 4, 22)))
            ps = pb()
            for i, g in enumerate(gs):
                co = fgrp(g)
                for kc in range(8):
                    mm(ps[:, i * 128:(i + 1) * 128], WIN[:, kc, co:co + 128], XN[:, kc, :], start=(kc == 0), stop=(kc == 7))
            pag = [g for g in gs if g < 14]
            xbg = [g for g in gs if g >= 14]
            if pag:
                i0 = gs.index(pag[0])
                cp('act', pa[:, pag[0]:pag[-1] + 1, 1:129], ps[:, i0 * 128:(i0 + len(pag)) * 128].rearrange("p (g n) -> p g n", g=len(pag)))
            if xbg:
                i0 = gs.index(xbg[0])
                cp('act', xb[:, xbg[0] - 14:xbg[-1] - 13, 3:131], ps[:, i0 * 128:(i0 + len(xbg)) * 128].rearrange("p (g n) -> p g n", g=len(xbg)))
            yield
        psz = pb()
        for kc in range(8):
            mm(psz[:, :], XN[:, kc, :], WIN[:, kc, 1792:1792 + 512], start=(kc == 0), stop=(kc == 7))
        if own:
            act(ZS[par][:], psz[:, :], AF.Silu)
        psd = ph()
        for kc in range(8):
            mm(psd[:, 0:8], XN[:, kc, :], WIN[:, kc, IN0 - 8:IN0], start=(kc == 0), stop=(kc == 7))
        tt('dve', DT[par][:], psd[:, 0:8], R_("dtb", 8), ALU.add)
        if c + 1 < NCH:
            dma(XT[:], xT_d[:, :, (c + 1) * 128:(c + 2) * 128])
        yield

        def prep_rwkv():
            for (ga, gb) in ((12, 14), (4, 8), (0, 4), (8, 12)):
                tt('pool', DTMP[:, ga:gb, :], pa[:, ga:gb, 0:128], pa[:, ga:gb, 1:129], ALU.subtract)
                tt('pool', DTMP[:, ga:gb, :], DTMP[:, ga:gb, :], MUC[:, ga:gb].unsqueeze(2).to_broadcast([128, gb - ga, 128]), ALU.mult)
                tt('pool', PL[:, ga:gb, :], DTMP[:, ga:gb, :], pa[:, ga:gb, 1:129], ALU.add)
                if ga == 12:
                    act(TW[:], PL[0:64, 12, :], AF.Tanh)
                    cp('dve', ALB[64:128, :], PL[64:128, 12, :])
                    act(SGL[:], PL[:, 13, :], AF.Sigmoid)
                if ga == 4:
                    tt('pool', KQ[:], K4, P_('kk', 4).unsqueeze(2).to_broadcast([128, 4, 128]), ALU.mult)
                    tt('pool', SQK[:], KQ[:], KQ[:], ALU.mult)
                yield
            psw = pb()
            mm(psw[:, :], TW[:], WDU[:])
            tt('dve', SG[:], psw[:, :], R_("w0", 512), ALU.add)
            act(SG[:], SG[:], AF.Sigmoid)
            if own:
                psg = pb()
                mm(psg[:, :], SGL[:], WGU[:])
                cp('act', GOUT[par][:], psg[:, :])
            psi = pb()
            for p in range(4):
                mm(psi[:, p * 128:(p + 1) * 128], WIU[64:128, p * 128:(p + 1) * 128], ALB[64:128, :])
            for p in range(4):
                act(ICLR[:, p, :], psi[:, p * 128:(p + 1) * 128], AF.Sigmoid, bias=P_("a0", 1, p))
            pss = pb()
            mm(pss[:, :], BLK, SQK[:].rearrange("p a b -> p (a b)"))
            ts('dve', RN[:].rearrange("p a b -> p (a b)"), pss[:, :], 1e-12, ALU.add)
            yield
            rsq(RN[:].rearrange("p a b -> p (a b)"))
            ts('pool', T1[:], ICLR[:], -1.0, ALU.add)
            tt('pool', T1[:], T1[:], P_('ka', 4).unsqueeze(2).to_broadcast([128, 4, 128]), ALU.mult)
            tt('pool', T1[:], T1[:], K4, ALU.mult)
            for p in range(4):
                psc = ph()
                mm(psc[:, 0:256], SG[:, p * 128:(p + 1) * 128], TRI2)
                act(E1[:, p, :], psc[:, 0:128], AF.Exp, scale=-LWS)
                act(E0[:, p, :], psc[:, 128:256], AF.Exp, scale=-LWS)
                act(EI[:, p, :], psc[:, 0:128], AF.Exp, scale=LWS)
            cp('act', G127[par][:], E1[:, :, 127])
            yield
            tt('pool', KKn[:], KQ[:], RN[:], ALU.mult)
            tt('pool', KM[:], T1[:], K4, ALU.add)
            tt('pool', AR[par][:, :, 1, :], R4, E1[:], ALU.mult)
            yield
            tt('pool', Bv[:], KKn[:], ICLR[:], ALU.mult)
            stt('dve', AR[par][:, :, 0, :], KKn[:], -1.0, E0[:], ALU.mult, ALU.mult)
            tt('pool', KT[par][:], KM[:], EI[:], ALU.mult)
            tt('pool', BT[par][:], Bv[:], EI[:], ALU.mult)
            if own:
                tt('pool', RKR[:], R4, KM[:], ALU.mult)
                tt('pool', RKR[:], RKR[:], P_('rk', 4).unsqueeze(2).to_broadcast([128, 4, 128]), ALU.mult)
            yield
            for p in range(4):
                pst = ph()
                tr(pst[:, 0:128], V4[:, p, :], IDENT)
                cp('act', VTOK[par][:, p, :], pst[:, 0:128])
                for src, dst in ((AR[par][:, p, 0, :], A0T[par]), (AR[par][:, p, 1, :], R0T[par]), (KT[par][:, p, :], KTT[par]),
                                 (BT[par][:, p, :], BTT[par])):
                    pst = ph()
                    trb(pst[:, 0:128], src)
                    cp(ev(), dst[:, p, :], pst[:, 0:128])
                if p % 2 == 1:
                    yield
            if own:
                psb = ph()
                for p in range(4):
                    mm(psb[:, 2 * p:2 * p + 2], RKR[:, p, :], HSEL)
                cp('act', BON[par][:], psb[:, 0:8])

        def prep_ssd():
            CW3 = PRM[:, PC["conv_w"]:PC["conv_w"] + 32].rearrange("p (g j) -> p g j", j=4)
            for half in range(2):
                gsl = slice(4 * half, 4 * half + 4)
                c0 = XC[:, :, :] if half == 0 else XBC[par][:, :, :]
                c1 = CT2[:, :, :]
                tt('pool', c0, xb[:, gsl, 0:128], CW3[:, gsl, 0].unsqueeze(2).to_broadcast([128, 4, 128]), ALU.mult)
                for j in range(1, 4):
                    tt('pool', c1, xb[:, gsl, j:j + 128], CW3[:, gsl, j].unsqueeze(2).to_broadcast([128, 4, 128]), ALU.mult)
                    tt('pool', c0, c0, c1, ALU.add)
                yield
            for g in range(8):
                dst = XC[:, g, :] if g < 4 else XBC[par][:, g - 4, :]
                act(dst, dst, AF.Silu, bias=P_("conv_b", 1, g))
            act(DT[par][:], DT[par][:], AF.Exp)
            act(DT[par][:], DT[par][:], AF.Ln, bias=1.0)
            tt('dve', DA[par][:], DT[par][:], AREP[:], ALU.mult)
            yield
            yield
            psc = ph()
            mm(psc[:, 0:8], MUI, DA[par][:])
            cp('act', CUMC[par][:], psc[:, 0:8])
            for g in range(4):
                pst = ph()
                tr(pst[:, 0:128], XC[:, g, :], IDENT)
                cp('act', XSTOK[par][:, g * 128:(g + 1) * 128], pst[:, 0:128])
            for g in range(2):
                pst = ph()
                tr(pst[:, 0:128], XBC[par][:, g, :], IDENT)
                cp('act', BTOK[par][:, g * 128:(g + 1) * 128], pst[:, 0:128])
            yield

        for _ in parallel(prep_rwkv(), prep_ssd()):
            yield
        if c + 1 < NCH:
            norm_block(c + 1)
        yield

    def rwkv_hb(c, hb):
        own = c >= OWN0
        par = c % 2
        ar, kt, bt, vtok, a0t, r0t = AR[par], KT[par], BT[par], VTOK[par], A0T[par], R0T[par]
        heads = list(range(4 * hb, 4 * hb + 4))
        nn, pp, ttb, arb, kab, axb = NN[hb], PP[hb], TTb[hb], ARB[hb], KAb[hb], AXb[hb]

        def h8(t):
            return t[:].rearrange("p a (h d) -> p (a h) d", h=2)[:, 4 * hb:4 * hb + 4, :]
        def par_view(t, q):
            return t[:].rearrange("p (a b) n -> p a b n", b=2)[:, :, q, :]
        for q in range(2):
            bk1 = pb()
            bk2 = pb()
            bk3 = pb()
            for i in range(2):
                h = heads[2 * i + q]
                p, hr = h // 2, slice(64 * (h % 2), 64 * (h % 2) + 64)
                rhs = ar[hr, p, :, :].rearrange("p a b -> p (a b)")
                mm(bk1[:, i * 256:(i + 1) * 256], bt[hr, p, :], rhs)
                mm(bk2[:, i * 256:(i + 1) * 256], kt[hr, p, :], rhs)
                mm(bk3[:, i * 128:(i + 1) * 128], ar[hr, p, 0, :], bt[hr, p, :])
            v1 = bk1.rearrange("p (h t n) -> p h t n", h=2, t=2)
            tt('dve', par_view(pp[0], q), v1[:, :, 0, :], MU2[:, 0:128].unsqueeze(1).to_broadcast([128, 2, 128]), ALU.mult)
            tt('dve', par_view(arb, q), v1[:, :, 1, :], MU2[:, 128:256].unsqueeze(1).to_broadcast([128, 2, 128]), ALU.mult)
            tt('dve', par_view(kab, q), bk2.rearrange("p (h n) -> p h n", h=2),
               MU2.unsqueeze(1).to_broadcast([128, 2, 256]), ALU.mult)
            tt('dve', par_view(nn[0], q), bk3[:, 0:256].rearrange("p (h n) -> p h n", h=2),
               SLm.unsqueeze(1).to_broadcast([128, 2, 128]), ALU.mult)
            yield
        tt('dve', ttb[:], pp[0][:], IDENT.unsqueeze(1).to_broadcast([128, 4, 128]), ALU.add)
        cp('act', axb[:, :, 0:64], h8(a0t))
        bkx = pb()
        for i, h in enumerate(heads):
            p, hr = h // 2, slice(64 * (h % 2), 64 * (h % 2) + 64)
            mm(bkx[:, i * 64:(i + 1) * 64], kab[:, i, 0:128], vtok[:, p, hr])
        cp('act', axb[:, :, 64:128], bkx[:, 0:256].rearrange("p (h d) -> p h d", h=4))
        yield
        ci = 0
        for lvl in range(1, 7):
            ncur, pcur, nnx, pnx = nn[ci], pp[ci], nn[1 - ci], pp[1 - ci]
            bka = pb()
            for i in range(4):
                mm(bka[:, i * 128:(i + 1) * 128], pcur[:, i, :], ncur[:, i, :])
            cp('act', nnx[:], bka.rearrange("p (h n) -> p h n", h=4))
            if lvl < 6:
                bkb = pb()
                for i in range(4):
                    mm(bkb[:, i * 128:(i + 1) * 128], ncur[:, i, :], pcur[:, i, :])
                cp('act', pnx[:], bkb.rearrange("p (h n) -> p h n", h=4))
            yield
            bkc = pb()
            for i in range(4):
                mm(bkc[:, i * 128:(i + 1) * 128], nnx[:, i, :], ttb[:, i, :])
            tt('dve', ttb[:], ttb[:], bkc.rearrange("p (h n) -> p h n", h=4), ALU.add)
            yield
            ci = 1 - ci
        bkw = pb()
        for i in range(4):
            mm(bkw[:, i * 128:(i + 1) * 128], ttb[:, i, :], axb[:, i, :])
        vw = bkw.rearrange("p (h n) -> p h n", h=4)
        cp('act', h8(WW), vw[:, :, 0:64])
        cp('dve', h8(UU), vw[:, :, 64:128])
        yield
        if own:
            bkq = pb()
            for i, h in enumerate(heads):
                p, hr = h // 2, slice(64 * (h % 2), 64 * (h % 2) + 64)
                mm(bkq[:, i * 128:i * 128 + 64], arb[:, i, :], WW[:, p, hr])
                mm(bkq[:, i * 128 + 64:(i + 1) * 128], arb[:, i, :], UU[:, p, hr], start=True, stop=False)
                mm(bkq[:, i * 128 + 64:(i + 1) * 128], kab[:, i, 128:256], vtok[:, p, hr], start=False, stop=True)
            vq = bkq.rearrange("p (h n) -> p h n", h=4)
            tt('dve', h8(QTOK), vq[:, :, 0:64], h8(r0t), ALU.add)
            cp('act', h8(Y0), vq[:, :, 64:128])
            yield

    def rwkv_pairs(c):
        own = c >= OWN0
        par = c % 2
        ar, kt, bt, vtok, a0t, r0t, ktt, btt = AR[par], KT[par], BT[par], VTOK[par], A0T[par], R0T[par], KTT[par], BTT[par]
        hs, hsn = HS[par], HS[1 - par]
        hsb, hsbn = HSb[par], HSb[1 - par]
        g127 = G127[par]
        bkm = pb()
        for p in range(4):
            mm(bkm[:, p * 128:(p + 1) * 128], IDB[:], IDB[:], start=True, stop=False)
            mm(bkm[:, p * 128:(p + 1) * 128], WW[:, p, :], btt[:, p, :], start=False, stop=True)
        tt('dve', MT[:], bkm.rearrange("p (a n) -> p a n", a=4), BLK.unsqueeze(1).to_broadcast([128, 4, 128]), ALU.mult)
        bkl = pb()
        for p in range(4):
            mm(bkl[:, p * 128:(p + 1) * 128], btt[:, p, :], UU[:, p, :], start=True, stop=False)
            mm(bkl[:, p * 128:(p + 1) * 128], ktt[:, p, :], vtok[:, p, :], start=False, stop=True)
        vl = bkl.rearrange("p (a n) -> p a n", a=4)
        for hh in range(2):
            hr = slice(64 * hh, 64 * hh + 64)
            tt('dve', LL[hr, :, :], vl[hr, :, 64 * hh:64 * hh + 64], g127[hr, :].unsqueeze(2).to_broadcast([64, 4, 64]), ALU.mult)
        yield
        if own:
            bkt = pb()
            for p in range(4):
                trb(bkt[:, p * 128:(p + 1) * 128], QTOK[:, p, :])
            cp('act', QT[:], bkt.rearrange("p (a n) -> p a n", a=4))
            Y4 = YTOK.rearrange("p (a h d) -> p a h d", a=4, h=2)
            for hh in range(2):
                hr = slice(64 * hh, 64 * hh + 64)
                bky = pb()
                for p in range(4):
                    mm(bky[:, p * 64:(p + 1) * 64], QT[hr, p, :], hsb[hr, p, :])
                tt('dve', Y4[:, :, hh, :], bky[:, 0:256].rearrange("p (a d) -> p a d", a=4), Y0[:, :, 64 * hh:64 * hh + 64], ALU.add)
            yield
        bkh = pb()
        for p in range(4):
            mm(bkh[:, p * 64:(p + 1) * 64], MT[:, p, :], hsb[:, p, :])
        tt('dve', hsn[:], bkh[:, 0:256].rearrange("p (a d) -> p a d", a=4), g127[:].unsqueeze(2).to_broadcast([128, 4, 64]), ALU.mult)
        tt('dve', hsn[:], hsn[:], LL[:], ALU.add)
        yield
        if c == 15:
            ts('dve', hsn[:], hsn[:], P_("flag"), ALU.mult)
        cp('act', hsbn[:], hsn[:])
        if own:
            Y3 = YTOK.rearrange("p (h d) -> p h d", h=8)
            rsum('dve', ST1[:], Y3)
            for h in range(8):
                kb.op('act', lambda: nc.scalar.activation(out=JUNK[:], in_=YTOK[:, h * 64:(h + 1) * 64], func=AF.Square,
                                                          accum_out=ST2[:, h:h + 1]), [YTOK], [JUNK[:], ST2[:]])
            ts('dve', ST1[:], ST1[:], 1.0 / 64, ALU.mult)
            tt('dve', ST3[:], ST1[:], ST1[:], ALU.mult)
            stt('dve', ST2[:], ST2[:], 1.0 / 64, ST3[:], ALU.mult, ALU.subtract)
            ts('dve', ST2[:], ST2[:], 64e-5, ALU.add)
            rsq(ST2[:])
            tt('dve', Y3, Y3, ST1[:].unsqueeze(2).to_broadcast([128, 8, 64]), ALU.subtract)
            tt('dve', Y3, Y3, ST2[:].unsqueeze(2).to_broadcast([128, 8, 64]), ALU.mult)
            tt('pool', YTOK, YTOK, R_("gn_w", 512), ALU.mult)
            tt('pool', YTOK, YTOK, R_("gn_b", 512), ALU.add)
            yield
            for h in range(8):
                p, hr = h // 2, slice(64 * (h % 2), 64 * (h % 2) + 64)
                stt('dve', YTOK[:, h * 64:(h + 1) * 64], vtok[:, p, hr], BON[par][:, h:h + 1], YTOK[:, h * 64:(h + 1) * 64],
                    ALU.mult, ALU.add)
            tt('pool', MIXTOK[:, 0:512], YTOK, GOUT[par][:], ALU.mult)
            yield

    def stage2_ssd(c):
        own = c >= OWN0
        par = c % 2
        dt_, da, cumc, xstok, btok, xbc = DT[par], DA[par], CUMC[par], XSTOK[par], BTOK[par], XBC[par]
        if own:
            act(ECUM[:], cumc[:], AF.Exp)
            for g in range(2):
                pcb = ph()
                mm(pcb[:, 0:128], xbc[:, g, :], xbc[:, 2 + g, :])
                tt('dve', CBM[:, g, :], pcb[:, 0:128], MUI, ALU.mult)
        hss, hssn = HSS[par], HSS[1 - par]
        for g in range(2):
            hs4 = slice(4 * g, 4 * g + 4)
            xs4 = xstok[:, g * 256:(g + 1) * 256].rearrange("p (h d) -> p h d", h=4)
            tt('dve', TD4[:], MUI.unsqueeze(1).to_broadcast([128, 4, 128]), da[:, hs4].unsqueeze(2).to_broadcast([128, 4, 128]), ALU.mult)
            bkr = pb()
            mm(bkr[:, :], ONES, TD4[:].rearrange("p h n -> p (h n)"))
            vr = bkr.rearrange("p (h n) -> p h n", h=4)
            tt('dve', SEG4[:], vr, cumc[:, hs4].unsqueeze(2).to_broadcast([128, 4, 128]), ALU.subtract)
            ts('dve', SEG4[:], SEG4[:], 0.0, ALU.add, 0.0, ALU.min)
            act(LD4[:], SEG4[:], AF.Exp)
            act(CDEC[:, hs4], vr[:, :, 127], AF.Exp)
            tt('dve', TE[:, hs4], LD4[:, :, 127], dt_[:, hs4], ALU.mult)
            tt('dve', XW[:, g * 256:(g + 1) * 256].rearrange("p (h d) -> p h d", h=4), xs4,
               TE[:, hs4].unsqueeze(2).to_broadcast([128, 4, 64]), ALU.mult)
            yield
            if own:
                tt('dve', WT4[:], LD4[:], dt_[:, hs4].unsqueeze(2).to_broadcast([128, 4, 128]), ALU.mult)
                tt('dve', WT4[:], WT4[:], CBM[:, g, :].unsqueeze(1).to_broadcast([128, 4, 128]), ALU.mult)
                bky = pb()
                for i in range(4):
                    h = 4 * g + i
                    mm(bky[:, i * 128:i * 128 + 64], WT4[:, i, :], xstok[:, h * 64:(h + 1) * 64])
                    mm(bky[:, i * 128 + 64:(i + 1) * 128], xbc[:, 2 + g, :], hss[:, h, :])
                vy = bky.rearrange("p (h n) -> p h n", h=4)
                tt('dve', YOFF4[:], vy[:, :, 64:128], ECUM[:, hs4].unsqueeze(2).to_broadcast([128, 4, 64]), ALU.mult)
                tt('dve', YS[:, g * 256:(g + 1) * 256].rearrange("p (h d) -> p h d", h=4), vy[:, :, 0:64], YOFF4[:], ALU.add)
                yield
            bks = pb()
            for i in range(4):
                h = 4 * g + i
                mm(bks[:, i * 64:(i + 1) * 64], btok[:, g * 128:(g + 1) * 128], XW[:, h * 64:(h + 1) * 64])
            tt('dve', hssn[:, hs4, :], hss[:, hs4, :], CDEC[:, hs4].unsqueeze(2).to_broadcast([128, 4, 64]), ALU.mult)
            tt('dve', hssn[:, hs4, :], hssn[:, hs4, :], bks[:, 0:256].rearrange("p (h d) -> p h d", h=4), ALU.add)
            yield
        if c == 15:
            ts('dve', hssn[:], hssn[:], P_("flag"), ALU.mult)
        if own:
            for h in range(8):
                stt('dve', YS[:, h * 64:(h + 1) * 64], xstok[:, h * 64:(h + 1) * 64], P_("dskp", 1, h), YS[:, h * 64:(h + 1) * 64],
                    ALU.mult, ALU.add)
            tt('dve', YS, YS, ZS[par][:], ALU.mult)
            tt('pool', XW[:], YS, YS, ALU.mult)
            rsum('dve', ST4[:], XW[:].rearrange("p (g d) -> p g d", g=2))
            ts('dve', ST4[:], ST4[:], 1.0 / 256, ALU.mult, 1e-6, ALU.add)
            rsq(ST4[:])
            for g in range(2):
                stt('dve', MIXTOK[:, 512 + g * 256:512 + (g + 1) * 256], YS[:, g * 256:(g + 1) * 256], ST4[:, g:g + 1],
                    R_("ssdn", 256, g * 256), ALU.mult, ALU.mult)
            yield

    def stage2_out(c):
        if c >= OWN0:
            oc = c - OWN0
            for j in range(8):
                pst = ph()
                tr(pst[:, 0:128], MIXTOK[:, j * 128:(j + 1) * 128], IDENT)
                cp('act', MXS[:, j, :], pst[:, 0:128])
            dma(mix_scr[:, :, oc * 128:(oc + 1) * 128], MXS[:])
        return
        yield

    def chain(*gens):
        for g in gens:
            for _ in g:
                yield

    def parallel(*gens):
        gens = list(gens)
        while gens:
            for g in list(gens):
                try:
                    next(g)
                except StopIteration:
                    gens.remove(g)
            yield

    def weighted(items):
        st = [[g, 0, float(n) * sp] for (g, n, sp) in items]
        while st:
            st.sort(key=lambda x: (x[1] + 1) / x[2])
            x = st[0]
            try:
                next(x[0])
                x[1] += 1
            except StopIteration:
                st.remove(x)

    dma(XT[:], xT_d[:, :, 0:128])
    norm_block(0)
    for c in range(NCH + 1):
        items = []
        if c >= 1:
            items.append((chain(parallel(rwkv_hb(c - 1, 0), rwkv_hb(c - 1, 1)), rwkv_pairs(c - 1)), 23, 1.0))
            items.append((stage2_ssd(c - 1), 7, 1.0))
        if c < NCH:
            items.append((stage1(c), 19, 1.0))
        weighted(items)
        if c >= 1:
            for _ in stage2_out(c - 1):
                pass

    kb.barrier()
    for t in reversed(p1):
        t.__exit__(None, None, None)
    if debug == "mix0":
        DBb = sb("DBb", [128, 8, TOWN], BF16)
        dma(DBb[:], mix_scr)
        DB = sb("DB", [128, 8, TOWN])
        cp('dve', DB[:], DBb[:])
        dma(dbg_d, DB[:])
        OUT = sb("OUTS", [128, 8, 2048])
        kb.op('dve', lambda: nc.vector.memset(OUT[:], 0.0), [], [OUT[:]])
        dma(out_d, OUT[:])
        kb.finish('sp')
        return nc

    TILES = [(0, 512), (512, 512), (1024, 512), (1536, 512), (2048, 128)]
    MTILES = [(128, 512), (640, 512), (1152, 512), (1664, 512)]
    HT = sb("HT", [128, 8, TOWN])
    ACTA = sb("ACTA", [128, 8, TOWN], BF16)
    WA = sb("WA", [128, 8, 1024], BF16)
    WB = sb("WB", [128, 8, 1024], BF16)
    SQT = sb("SQT", [128, 8, 128])
    RS = sb("RS", [128, 128])
    REP2 = sb("REP2", [128, 144])
    for t_ in (HT, ACTA):
        kb.split[t_.name] = (8 * TOWN, TOWN, 512)
    dma(REP2[:], rep_d[:, RC["qn"]:RC["qn"] + 144])
    dma(HT[:], xT_d[:, :, OWN0 * 128:NSLOT])
    dma(ACTA[:], mix_scr)

    def dbg_dump():
        dma(dbg_d, HT[:])
        kb.finish('sp')
        print("instructions:", kb.ninst, flush=True)
        return nc

    def rmsnorm_blk(gname, n, xnf=None):
        t0 = n * 128
        act(SQT[:], HT[:, :, t0:t0 + 128], AF.Square)
        ps = pb()
        for kc in range(8):
            mm(ps[:, 0:128], ONES, SQT[:, kc, :], start=(kc == 0), stop=(kc == 7))
        ts('dve', RS[:], ps[:, 0:128], 1.0 / DM, ALU.mult, 1e-6, ALU.add)
        rsq(RS[:])
        for kc in range(8):
            if xnf is None:
                stt('dve', ACTA[:, kc, t0:t0 + 128], HT[:, kc, t0:t0 + 128], P_(gname, 1, kc), RS[:], ALU.mult, ALU.mult)
            else:
                stt('dve', xnf[:, kc, :], HT[:, kc, t0:t0 + 128], P_(gname, 1, kc), RS[:], ALU.mult, ALU.mult)
                cp('act', ACTA[:, kc, t0:t0 + 128], xnf[:, kc, :])

    def ffn_passes(passes, WD, SGa, SGb, ACTH):
        def load_gu(ps_):
            f0, nf = ps_["f0"], ps_["nf"]
            for kc in range(8):
                kb.dma('pool', WA[:, kc, 0:nf * 128], ps_["wg"][kc * 128:(kc + 1) * 128, f0 * 128:(f0 + nf) * 128])
                kb.dma('pool', WB[:, kc, 0:nf * 128], ps_["wu"][kc * 128:(kc + 1) * 128, f0 * 128:(f0 + nf) * 128])
        load_gu(passes[0])
        for i, ps_ in enumerate(passes):
            f0, nf, tiles, rb = ps_["f0"], ps_["nf"], ps_["tiles"], ps_.get("rb")
            if ps_.get("pre") is not None:
                ps_["pre"]()
            for f in range(nf):
                kb.dma('pool', WD[:, f, :], ps_["wd"][(f0 + f) * 128:(f0 + f + 1) * 128, :])
            for (t0, tn) in tiles:
                for f in range(nf):
                    pg = pb()
                    for kc in range(8):
                        mm(pg[:, 0:tn], WA[:, kc, f * 128:(f + 1) * 128], ACTA[:, kc, t0:t0 + tn], start=(kc == 0), stop=(kc == 7))
                    pu = pb()
                    for kc in range(8):
                        mm(pu[:, 0:tn], WB[:, kc, f * 128:(f + 1) * 128], ACTA[:, kc, t0:t0 + tn], start=(kc == 0), stop=(kc == 7))
                    act(SGa[:, 0:tn], pg[:, 0:tn], AF.Silu)
                    if rb is None:
                        tt('dve', ACTH[:, f, t0:t0 + tn], pu[:, 0:tn], SGa[:, 0:tn], ALU.mult)
                    else:
                        tt('dve', SGb[:, 0:tn], pu[:, 0:tn], SGa[:, 0:tn], ALU.mult)
                        tt('dve', ACTH[:, f, t0:t0 + tn], SGb[:, 0:tn], rb[:, t0 - 128:t0 - 128 + tn], ALU.mult)
            if i + 1 < len(passes):
                load_gu(passes[i + 1])
            for (t0, tn) in tiles:
                for m in range(8):
                    ps = pb()
                    for f in range(nf):
                        mm(ps[:, 0:tn], WD[:, f, m * 128:(m + 1) * 128], ACTH[:, f, t0:t0 + tn], start=(f == 0), stop=(f == nf - 1))
                    tt('dve', HT[:, m, t0:t0 + tn], HT[:, m, t0:t0 + tn], ps[:, 0:tn], ALU.add)

    def scope():
        lst = []

        def alloc(name, shape, dt=F32):
            t = nc.sbuf_tensor(name, list(shape), dt)
            h = t.__enter__()
            lst.append(t)
            return h

        def close():
            kb.barrier()
            for t in reversed(lst):
                t.__exit__(None, None, None)
        return alloc, close

    for kc in range(8):
        kb.dma('pool', WA[:, kc, :], w_out_d[kc * 128:(kc + 1) * 128, :])
    for (t0, tn) in TILES:
        for m in range(8):
            ps = pb()
            for kc in range(8):
                mm(ps[:, 0:tn], WA[:, kc, m * 128:(m + 1) * 128], ACTA[:, kc, t0:t0 + tn], start=(kc == 0), stop=(kc == 7))
            tt('dve', HT[:, m, t0:t0 + tn], HT[:, m, t0:t0 + tn], ps[:, 0:tn], ALU.add)
    if debug == "h1":
        return dbg_dump()
    for n in range(NOWN):
        rmsnorm_blk("g_ffn0", n)
    a2, close2 = scope()
    WD = a2("WD", [128, 8, 1024], BF16)
    ACTH2 = a2("ACTH2", [128, 8, TOWN], BF16)
    kb.split[ACTH2.name] = (8 * TOWN, TOWN, 512)
    SGa = a2("SGa", [128, 512])
    SGb = a2("SGb", [128, 512])
    ffn_passes([dict(wg=ffg_d, wu=ffu_d, wd=ffd_d, f0=f0, nf=nf, tiles=TILES) for (f0, nf) in ((0, 8), (8, 8), (16, 6))],
               WD, SGa, SGb, ACTH2)
    close2()
    if debug == "h2":
        return dbg_dump()

    a3, close3 = scope()
    BQ = a3("a3_BQ", [128, 1536])
    dma(BQ[:], bq_d)
    POSI = a3("a3_POSI", [128, NOWN], I32)
    dma(POSI[:], pos_d)
    POSF = a3("a3_POSF", [128, NOWN])
    TFR = a3("a3_TFR", [128, NOWN, 8])
    TFI = a3("a3_TFI", [128, NOWN, 8], I32)
    TF2 = a3("a3_TF2", [128, NOWN, 8])
    S1 = a3("a3_S1", [128, NOWN, 8])
    COS = a3("a3_COS", [128, NOWN, 8])
    SIN = a3("a3_SIN", [128, NOWN, 8])
    FRQ = CST[:, C_FRQ:C_FRQ + 8]
    cp('dve', POSF[:], POSI[:])
    tt('dve', TFR[:], POSF[:].unsqueeze(2).to_broadcast([128, NOWN, 8]), FRQ.unsqueeze(1).to_broadcast([128, NOWN, 8]), ALU.mult)
    cp('dve', TFI[:], TFR[:])
    cp('dve', TF2[:], TFI[:])
    tt('dve', TFR[:], TFR[:], TF2[:], ALU.subtract)
    act(S1[:], TFR[:], AF.Sin, scale=float(np.pi))
    act(TF2[:], TFR[:], AF.Sin, scale=float(np.pi / 2))
    tt('dve', TF2[:], TF2[:], TF2[:], ALU.mult)
    ts('dve', TF2[:], TF2[:], -2.0, ALU.mult, 1.0, ALU.add)
    tt('dve', SIN[:], S1[:], TF2[:], ALU.mult)
    ts('dve', SIN[:], SIN[:], 2.0, ALU.mult)
    tt('dve', COS[:], S1[:], S1[:], ALU.mult)
    ts('dve', COS[:], COS[:], -2.0, ALU.mult, 1.0, ALU.add)
    QN = REP2[:, 0:64]
    KN = REP2[:, 64:128]
    SINK = REP2[:, 128:144]
    NEGB = a3("a3_NEGB", [128, 1])
    TMPB = a3("a3_TMPB", [128, 64])
    MXB = a3("a3_MXB", [128, 2])
    tt('dve', TMPB[:], QN, QN, ALU.mult)
    kb.op('dve', lambda: nc.vector.reduce_max(out=MXB[:, 0:1], in_=TMPB[:], axis=AX.X), [TMPB[:]], [MXB[:]])
    tt('dve', TMPB[:], KN, KN, ALU.mult)
    kb.op('dve', lambda: nc.vector.reduce_max(out=MXB[:, 1:2], in_=TMPB[:], axis=AX.X), [TMPB[:]], [MXB[:]])
    tt('dve', NEGB[:], MXB[:, 0:1], MXB[:, 1:2], ALU.mult)
    act(NEGB[:], NEGB[:], AF.Sqrt)
    ts('dve', NEGB[:], NEGB[:], -8.0, ALU.mult)
    ESK = a3("a3_ESK", [128, 16])
    act(ESK[:], SINK, AF.Exp, bias=NEGB[:])
    KT = a3("a3_KT", [128, 2, TOWN], BF16)
    VE = a3("a3_VE", [128, NOWN, 4, 65], BF16)
    kb.op('dve', lambda: nc.vector.memset(VE[:], 1.0), [], [VE[:]])
    QKVB = a3("a3_QKVB", [128, 1536])
    QSQ = a3("a3_QSQ", [128, 1024])
    RQ = a3("a3_RQ", [128, 20])
    RT = [a3("a3_RT%d" % i, [128, 16, 8]) for i in range(4)]
    QP = a3("a3_QP", [128, 8, 128])
    QT = a3("a3_QT", [128, 8, 128], BF16)
    PTF = a3("a3_PTF", [128, 2, 512])
    PT = a3("a3_PT", [128, 2, 512], BF16)
    DEN = a3("a3_DEN", [128, 4])
    OTOK = a3("a3_OTOK", [128, 1024])
    MASK2 = CST[:, C_M2:C_M2 + 256]
    for n in range(NOWN):
        rmsnorm_blk("g_mix1", n)
    for kc in range(8):
        kb.dma('pool', WA[:, kc, :], wqkv_d[kc * 128:(kc + 1) * 128, 0:1024])
        kb.dma('pool', WB[:, kc, 0:512], wqkv_d[kc * 128:(kc + 1) * 128, 1024:1536])

    def qk_norm_rope(e, X3, nh, gain, n, rq, sq, rts):
        sq3 = sq[:, 0:nh * 64].rearrange("p (h d) -> p h d", h=nh)
        tt(e, sq3, X3, X3, ALU.mult)
        rsum('dve', rq, sq3)
        ts(e, rq, rq, 1.0 / 64, ALU.mult, 1e-6, ALU.add)
        rsq(rq)
        tt(e, X3, X3, rq.unsqueeze(2).to_broadcast([128, nh, 64]), ALU.mult)
        tt(e, X3, X3, gain.unsqueeze(1).to_broadcast([128, nh, 64]), ALU.mult)
        c = COS[:, n, :].unsqueeze(1).to_broadcast([128, nh, 8])
        sn = SIN[:, n, :].unsqueeze(1).to_broadcast([128, nh, 8])
        x1, x2 = X3[:, :, 0:8], X3[:, :, 8:16]
        r0, r1, r2, r3 = [rts[i][:, 0:nh, :] for i in range(4)]
        tt(e, r0, x1, c, ALU.mult)
        tt(e, r1, x2, sn, ALU.mult)
        tt(e, r2, x2, c, ALU.mult)
        tt(e, r3, x1, sn, ALU.mult)
        tt(e, x1, r0, r1, ALU.subtract)
        tt(e, x2, r2, r3, ALU.add)

    QTs = [QT, a3("a3_QT1", [128, 8, 128], BF16)]
    KSQ = a3("a3_KSQ", [128, 256])
    RTK = [a3("a3_RTK%d" % i, [128, 4, 8]) for i in range(4)]

    def attn_A(n):
        t0 = n * 128
        qt = QTs[n % 2]
        cgs = (2,) if n == 0 else (2, 0, 1)
        for cg in cgs:
            ps = pb()
            for kc in range(8):
                w = WA[:, kc, cg * 512:(cg + 1) * 512] if cg < 2 else WB[:, kc, 0:512]
                mm(ps[:, :], ACTA[:, kc, t0:t0 + 128], w, start=(kc == 0), stop=(kc == 7))
            tt('dve', QKVB[:, cg * 512:(cg + 1) * 512], ps[:, :], BQ[:, cg * 512:(cg + 1) * 512], ALU.add)
            yield
        K3 = QKVB[:, 1024:1280].rearrange("p (h d) -> p h d", h=4)
        qk_norm_rope('dve', K3, 4, KN, n, RQ[:, 16:20], KSQ, RTK)
        cp('act', VE[:, n, :, 0:64], QKVB[:, 1280:1536].rearrange("p (h d) -> p h d", h=4))
        yield
        for u in range(2):
            pst = ph()
            tr(pst[:, 0:128], QKVB[:, 1024 + u * 128:1024 + (u + 1) * 128], IDENT)
            cp('act', KT[:, u, t0:t0 + 128], pst[:, 0:128])
        if n == 0:
            return
        Q3 = QKVB[:, 0:1024].rearrange("p (h d) -> p h d", h=16)
        qk_norm_rope('pool', Q3, 16, QN, n, RQ[:, 0:16], QSQ, RT)
        yield
        Q4 = QKVB[:, 0:1024].rearrange("p (kv g d) -> p kv g d", kv=4, g=4)
        for u in range(2):
            cp('pool', QP[:, u * 4:(u + 1) * 4, :].rearrange("p g (j d) -> p g j d", j=2),
               Q4[:, 2 * u:2 * u + 2, :, :].rearrange("p j g d -> p g j d"))
        yield
        for t in range(8):
            pst = ph()
            tr(pst[:, 0:128], QP[:, t, :], IDENT)
            cp('act', qt[:, t, :], pst[:, 0:128])
            if t == 3:
                yield
        yield

    def attn_B(n):
        t0 = n * 128
        qt = QTs[n % 2]
        for kv in range(4):
            u, half = kv // 2, kv % 2
            hr = slice(64 * half, 64 * half + 64)
            qrhs = qt[hr, u * 4:(u + 1) * 4, :].rearrange("p g n -> p (g n)")
            bkp = pb()
            mm(bkp[:, :], KT[hr, u, t0 - 128:t0], qrhs)
            bkc = pb()
            mm(bkc[:, :], KT[hr, u, t0:t0 + 128], qrhs)
            act(PTF[:, 0, :], bkp[:, :], AF.Exp, bias=NEGB[:], scale=0.125)
            act(PTF[:, 1, :], bkc[:, :], AF.Exp, bias=NEGB[:], scale=0.125)
            tt('dve', PT[:, 0, :].rearrange("p (g n) -> p g n", g=4), PTF[:, 0, :].rearrange("p (g n) -> p g n", g=4),
               MASK2[:, 0:128].unsqueeze(1).to_broadcast([128, 4, 128]), ALU.mult)
            tt('dve', PT[:, 1, :].rearrange("p (g n) -> p g n", g=4), PTF[:, 1, :].rearrange("p (g n) -> p g n", g=4),
               MASK2[:, 128:256].unsqueeze(1).to_broadcast([128, 4, 128]), ALU.mult)
            if n == 1:
                ts('dve', PT[:, 0, :], PT[:, 0, :], P_("flag"), ALU.mult)
            yield
            pso = pb()
            for g in range(4):
                mm(pso[:, g * 65:(g + 1) * 65], PT[:, 0, g * 128:(g + 1) * 128], VE[:, n - 1, kv, :], start=True, stop=False)
                mm(pso[:, g * 65:(g + 1) * 65], PT[:, 1, g * 128:(g + 1) * 128], VE[:, n, kv, :], start=False, stop=True)
            vo = pso[:, 0:260].rearrange("p (g d) -> p g d", g=4)
            tt('dve', DEN[:], vo[:, :, 64], ESK[:, kv * 4:(kv + 1) * 4], ALU.add)
            kb.op('dve', lambda: nc.vector.reciprocal(out=DEN[:], in_=DEN[:]), [DEN[:]], [DEN[:]])
            tt('dve', OTOK[:, kv * 256:(kv + 1) * 256].rearrange("p (g d) -> p g d", g=4), vo[:, :, 0:64],
               DEN[:].unsqueeze(2).to_broadcast([128, 4, 64]), ALU.mult)
            yield
        for j in range(8):
            pst = ph()
            tr(pst[:, 0:128], OTOK[:, j * 128:(j + 1) * 128], IDENT)
            cp('act', ACTA[:, j, t0 - 128:t0], pst[:, 0:128])
            if j == 3:
                yield
        yield

    def weighted3(items):
        st = [[g, 0, float(n_)] for (g, n_) in items]
        while st:
            st.sort(key=lambda x: (x[1] + 1) / x[2])
            x = st[0]
            try:
                next(x[0])
                x[1] += 1
            except StopIteration:
                st.remove(x)

    for n in range(NOWN + 1):
        items = []
        if n - 1 >= 1:
            items.append((attn_B(n - 1), 10))
        if n < NOWN:
            items.append((attn_A(n), 9))
        weighted3(items)
    for kc in range(8):
        kb.dma('pool', WA[:, kc, :], wo_d[kc * 128:(kc + 1) * 128, :])
    for (t0, tn) in MTILES:
        for m in range(8):
            ps = pb()
            for kc in range(8):
                mm(ps[:, 0:tn], WA[:, kc, m * 128:(m + 1) * 128], ACTA[:, kc, t0 - 128:t0 - 128 + tn], start=(kc == 0), stop=(kc == 7))
            stt('dve', HT[:, m, t0:t0 + tn], ps[:, 0:tn], P_("b_o", 1, m), HT[:, m, t0:t0 + tn], ALU.add, ALU.add)
    close3()
    if debug == "h3":
        return dbg_dump()

    a4, close4 = scope()
    WD = a4("WD4", [128, 8, 1024], BF16)
    ACTH4 = a4("ACTH4", [128, 6, TOWN], BF16)
    kb.split[ACTH4.name] = (6 * TOWN, TOWN, 512)
    SGa = a4("SGa4", [128, 512])
    SGb = a4("SGb4", [128, 512])
    XNF = a4("XNF", [128, 8, 128])
    WR = a4("WR", [128, 8, 8])
    dma(WR[:], wr_d.rearrange("(kc p) e -> p kc e", p=128))
    LG = a4("LG", [128, 8])
    MX8 = a4("MX8", [128, 8])
    NV1 = a4("NV1", [128, 1])
    MSK = a4("MSK", [128, 8])
    EX = a4("EX", [128, 8])
    SME = a4("SME", [128, 1])
    COMB = a4("COMB", [128, 16, 8])
    DG = a4("DG", [128, 128])
    RBs = [a4("RB%d" % i, [128, 2048], BF16) for i in range(2)]
    for n in range(1, NOWN):
        rmsnorm_blk("g_ffn1", n, xnf=XNF)
        ps = ph()
        for kc in range(8):
            mm(ps[:, 0:8], XNF[:, kc, :], WR[:, kc, :], start=(kc == 0), stop=(kc == 7))
        cp('dve', LG[:], ps[:, 0:8])
        kb.op('dve', lambda: nc.vector.max(out=MX8[:], in_=LG[:]), [LG[:]], [MX8[:]])
        ts('dve', MSK[:], LG[:], MX8[:, 1:2], ALU.is_ge)
        ts('dve', NV1[:], MX8[:, 0:1], -1.0, ALU.mult)
        act(EX[:], LG[:], AF.Exp, bias=NV1[:])
        tt('dve', EX[:], EX[:], MSK[:], ALU.mult)
        rsum('dve', SME[:], EX[:])
        kb.op('dve', lambda: nc.vector.reciprocal(out=SME[:], in_=SME[:]), [SME[:]], [SME[:]])
        ts('dve', COMB[:, n - 1, :], EX[:], SME[:], ALU.mult)
    def build_rb(e):
        RB = RBs[e % 2]
        for q4 in range(4):
            ps = pb()
            for b in range(4):
                blk = q4 * 4 + b
                ts('dve', DG[:], IDENT, COMB[:, blk, e:e + 1], ALU.mult)
                mm(ps[:, b * 128:(b + 1) * 128], ONES, DG[:])
            cp('act', RB[:, q4 * 512:(q4 + 1) * 512], ps[:, :])

    passes = []
    for e in range(NEXP):
        passes.append(dict(wg=eg_d[e], wu=eu_d[e], wd=ed_d[e], f0=0, nf=6, tiles=MTILES, rb=RBs[e % 2]))
        passes.append(dict(wg=eg_d[e], wu=eu_d[e], wd=ed_d[e], f0=6, nf=5, tiles=MTILES, rb=RBs[e % 2],
                           pre=(lambda e=e: build_rb(e + 1)) if e + 1 < NEXP else None))
    build_rb(0)
    ffn_passes(passes, WD, SGa, SGb, ACTH4)
    if debug == "h4":
        return dbg_dump()
    dma(out_d, HT[:, :, 128:TOWN])
    kb.finish('sp')
    print("instructions:", kb.ninst, flush=True)
    return nc


def _consts():
    i = np.arange(128)
    cst = np.zeros((128, NCST), np.float32)
    cst[:, C_ID:C_ID + 128] = np.eye(128)
    mui = (i[None, :] >= i[:, None]).astype(np.float32)
    mus = (i[None, :] > i[:, None]).astype(np.float32)
    cst[:, C_MUI:C_MUI + 128] = mui
    cst[:, C_MUS:C_MUS + 128] = mus
    cst[:, C_MUI2:C_MUI2 + 128] = mui
    cst[:, C_SL:C_SL + 128] = (i[None, :] < i[:, None])
    cst[:, C_ONES:C_ONES + 128] = 1.0
    blk = (i[None, :] // 64 == i[:, None] // 64).astype(np.float32)
    cst[:, C_BLK:C_BLK + 128] = blk
    cst[:, C_HSEL] = (i < 64)
    cst[:, C_HSEL + 1] = (i >= 64)
    cst[:, C_M2:C_M2 + 128] = (i[None, :] < i[:, None])
    cst[:, C_M2 + 128:C_M2 + 256] = mui
    inv_freq = 500000.0 ** (-np.arange(0, 16, 2, dtype=np.float64) / 16.0)
    cst[:, C_FRQ:C_FRQ + 8] = (inv_freq / (2 * np.pi))[None, :]
    return cst


def _fm(v, n):
    return np.ascontiguousarray(np.asarray(v, np.float32).reshape(n, 128).T)


def _prepare(inp):
    f = lambda k: np.asarray(inp[k], np.float32)
    prm = np.zeros((128, NPRM), np.float32)
    prm[:, PC["g_mix0"]:PC["g_mix0"] + 8] = _fm(f("ev_norm_mix")[0], 8)
    prm[:, PC["g_ffn0"]:PC["g_ffn0"] + 8] = _fm(f("ev_norm_ffn")[0], 8)
    prm[:, PC["g_mix1"]:PC["g_mix1"] + 8] = _fm(f("od_norm_mix")[0], 8)
    prm[:, PC["g_ffn1"]:PC["g_ffn1"] + 8] = _fm(f("od_norm_ffn")[0], 8)
    prm[:, PC["a0"]:PC["a0"] + 4] = _fm(f("ev_a0")[0], 4)
    cw = f("ev_conv_w")[0]
    prm[:, PC["conv_w"]:PC["conv_w"] + 32] = cw.T.reshape(8, 128, 4).transpose(1, 0, 2).reshape(128, 32)
    prm[:, PC["conv_b"]:PC["conv_b"] + 8] = _fm(f("ev_conv_b")[0], 8)
    prm[:, PC["b_qkv"]:PC["b_qkv"] + 12] = _fm(f("od_b_qkv")[0], 12)
    prm[:, PC["b_o"]:PC["b_o"] + 8] = _fm(f("od_b_o")[0], 8)
    rep = np.zeros((128, NREP), np.float32)

    def setrep(n, v):
        v = np.asarray(v, np.float32).reshape(-1)
        rep[:, RC[n]:RC[n] + v.size] = v[None, :]

    setrep("w0", f("ev_w0")[0])
    setrep("gn_w", f("ev_gn_w")[0])
    setrep("gn_b", f("ev_gn_b")[0])
    setrep("ssdn", f("ev_ssd_norm")[0])
    prm[:, PC["dskp"]:PC["dskp"] + 8] = f("ev_d_skip")[0][None, :]
    setrep("dtb", f("ev_dt_bias")[0])
    setrep("alog", f("ev_a_log")[0])
    setrep("qn", f("od_q_norm")[0])
    setrep("kn", f("od_k_norm")[0])
    setrep("sink", f("od_sinks")[0])
    bc = _fm(f("ev_mu_shift")[0], 14)
    prm[:, PC["kk"]:PC["kk"] + 4] = _fm(f("ev_k_k")[0], 4)
    prm[:, PC["ka"]:PC["ka"] + 4] = _fm(f("ev_k_a")[0], 4)
    prm[:, PC["rk"]:PC["rk"] + 4] = _fm(f("ev_r_k")[0].reshape(-1), 4)
    shared = {
        "rep": rep, "cst": _consts(), "bc": bc,
        "w_in": np.ascontiguousarray(f("ev_w_in")[0]),
        "wdu": np.ascontiguousarray(f("ev_w_decay_up")[0]),
        "wiu": np.ascontiguousarray(f("ev_w_iclr_up")[0]),
        "wgu": np.ascontiguousarray(f("ev_w_gate_up")[0]),
        "w_out": np.ascontiguousarray(f("ev_w_out")[0]),
        "ffg": np.ascontiguousarray(f("ev_ffn_gate")[0]),
        "ffu": np.ascontiguousarray(f("ev_ffn_up")[0]),
        "ffd": np.ascontiguousarray(f("ev_ffn_down")[0]),
        "wqkv": np.ascontiguousarray(f("od_w_qkv")[0]),
        "wo": np.ascontiguousarray(f("od_w_o")[0]),
        "bq": np.ascontiguousarray(np.broadcast_to(f("od_b_qkv")[0][None, :], (128, 1536))),
        "wr": np.ascontiguousarray(f("od_router")[0]),
        "eg": np.ascontiguousarray(f("od_exp_gate")[0]),
        "eu": np.ascontiguousarray(f("od_exp_up")[0]),
        "ed": np.ascontiguousarray(f("od_exp_down")[0]),
    }
    positions = np.asarray(inp["positions"]).astype(np.int32)
    x = f("x")
    maps = []
    for c in range(8):
        b, half = c // 2, c % 2
        xs = np.zeros((NSLOT, DM), np.float32)
        if half == 1:
            xs[:] = x[b]
        else:
            xs[2048:] = x[b, :2048]
        xT = np.ascontiguousarray(xs.T.reshape(8, 128, NSLOT).transpose(1, 0, 2))
        p = prm.copy()
        p[:, PC["flag"]] = float(half)
        m = dict(shared)
        m["xT"] = xT
        m["prm"] = p
        ps_ = np.zeros((NSLOT,), np.int32)
        if half == 1:
            ps_[:] = positions[b]
        else:
            ps_[2048:] = positions[b, :2048]
        m["pos"] = np.ascontiguousarray(ps_[OWN0 * 128:].reshape(NOWN, 128).T)
        maps.append(m)
    return maps


def kernel(**inputs):
    debug = os.environ.get("KDEBUG", "")
    maps = _prepare(inputs)
    nc = build_nc(debug=debug)
    res = run_bass_kernel_spmd(nc, maps, core_ids=list(range(8)))
    out = np.zeros((4, 4096, DM), np.float32)
    for c in range(8):
        b, half = c // 2, c % 2
        oT = res.results[c]["outT"]
        out[b, half * 2048:(half + 1) * 2048] = oT.transpose(2, 1, 0).reshape(2048, DM)
    if debug:
        kernel.dbg = [res.results[c]["dbg"] for c in range(8)]
    return out
```

```python
import os
import numpy as np
import concourse.bass as bass
import concourse.mybir as mybir
from concourse.bass_utils import run_bass_kernel_spmd

F32 = mybir.dt.float32
BF16 = mybir.dt.bfloat16
AF = mybir.ActivationFunctionType
ALU = mybir.AluOpType
AX = mybir.AxisListType

DM = 1024
NSLOT = 4096
NCH = 32
OWN0 = 15
NOWN = NCH - OWN0
TOWN = NOWN * 128
IN0 = 3336
FF0 = 2816
FFE = 1408
NEXP = 8
LWS = 0.6065306597126334

PC = {}
_o = 0
for _n, _w in [("g_mix0", 8), ("g_ffn0", 8), ("g_mix1", 8), ("g_ffn1", 8), ("a0", 4), ("kk", 4), ("ka", 4), ("rk", 4), ("conv_w", 32), ("conv_b", 8),
               ("b_qkv", 12), ("b_o", 8), ("flag", 1), ("dskp", 8)]:
    PC[_n] = _o
    _o += _w
NPRM = _o
RC = {}
_o = 0
for _n, _w in [("w0", 512), ("gn_w", 512), ("gn_b", 512), ("ssdn", 512), ("dtb", 8), ("alog", 8),
               ("qn", 64), ("kn", 64), ("sink", 16)]:
    RC[_n] = _o
    _o += _w
NREP = _o
C_ID, C_MUI, C_MUS, C_MUI2, C_SL, C_ONES, C_BLK, C_HSEL = 0, 128, 256, 384, 512, 640, 768, 896
C_M2 = 898
C_FRQ = 1154
NCST = 1162
I32 = mybir.dt.int32


class KB:
    def __init__(self, nc, ndma=16):
        self.nc = nc
        self.E = {'pe': nc.tensor, 'dve': nc.vector, 'act': nc.scalar, 'pool': nc.gpsimd, 'sp': nc.sync}
        self.sem = {e: nc.alloc_semaphore('s_' + e) for e in self.E}
        self.cnt = {e: 0 for e in self.E}
        self.seen = {e: {} for e in self.E}
        self.lastw = {}
        self.reads = {}
        self.ndma = ndma
        self.dsem = [nc.alloc_semaphore('d_%d' % i) for i in range(ndma)]
        self.dcnt = [0] * ndma
        self.dn = 0
        self.dnq = {}
        self.ninst = 0
        self.split = {}

    def _semof(self, key):
        return self.sem[key] if isinstance(key, str) else self.dsem[key]

    def _wait(self, e, key, val):
        if val <= 0 or self.seen[e].get(key, 0) >= val:
            return
        if key == e and e == 'pe':
            return
        self.E[e].wait_ge(self._semof(key), val)
        self.seen[e][key] = val

    def _deps(self, e, ins, outs):
        for n in ins:
            w = self.lastw.get(n)
            if w is not None:
                self._wait(e, w[0], w[1])
        for n in outs:
            w = self.lastw.get(n)
            if w is not None:
                self._wait(e, w[0], w[1])
            for (k, v) in self.reads.get(n, ()):
                self._wait(e, k, v)

    def _commit(self, key, val, ins, outs):
        for n in ins:
            self.reads.setdefault(n, []).append((key, val))
        for n in outs:
            self.lastw[n] = (key, val)
            self.reads[n] = []

    def keys(self, a):
        n = a.tensor.name
        sp = self.split.get(n)
        if sp is None:
            return [n]
        row, inner, piece = sp
        col = (a.offset % row) % inner
        ext = 1
        for (st, cn) in list(a.ap)[1:]:
            if st < inner:
                ext += (cn - 1) * st
        lo, hi = col // piece, min(col + ext - 1, inner - 1) // piece
        return ["%s#%d" % (n, i) for i in range(lo, hi + 1)]

    def op(self, e, fn, ins, outs, inc=True):
        xs = [k for a in ins if a.tensor.name in self.split for k in self.keys(a)]
        ins = [k for a in ins for k in self.keys(a)]
        outs = [k for a in outs for k in self.keys(a)] + xs
        self._deps(e, ins, outs)
        inst = fn()
        if inc:
            self.cnt[e] += 1
            inst.then_inc(self.sem[e], 1)
            val = self.cnt[e]
        else:
            val = self.cnt[e] + 1
        self._commit(e, val, ins, outs)
        self.ninst += 1
        return inst

    def dma(self, e, out, in_, **kw):
        half = self.ndma // 2
        base = half if e == 'pool' else 0
        k = self.dnq.get(e, 0)
        self.dnq[e] = k + 1
        slot = base + (k % half)
        self._wait(e, slot, self.dcnt[slot])
        ins = self.keys(in_)
        outs = self.keys(out)
        self._deps(e, ins, outs)
        inst = self.E[e].dma_start(out=out, in_=in_, **kw)
        self.dcnt[slot] += 16
        inst.then_inc(self.dsem[slot], 16)
        self._commit(slot, self.dcnt[slot], ins, outs)
        self.ninst += 1
        return inst

    def barrier(self):
        for e in self.E:
            for s in range(self.ndma):
                self._wait(e, s, self.dcnt[s])
            for k in self.E:
                if k != e:
                    self._wait(e, k, self.cnt[k])
        for e in ('dve', 'act', 'pool'):
            self._wait(e, e, self.cnt[e])

    def finish(self, e='sp'):
        for s in range(self.ndma):
            self._wait(e, s, self.dcnt[s])
        for k in self.E:
            if k != e:
                self._wait(e, k, self.cnt[k])


def build_nc(debug=""):
    nc = bass.Bass("TRN2", target_bir_lowering=False)
    kb = KB(nc)

    def din(name, shape, dt=F32):
        return nc.dram_tensor(name, list(shape), dt, kind="ExternalInput").ap()

    xT_d = din("xT", [128, 8, NSLOT])
    prm_d = din("prm", [128, NPRM])
    rep_d = din("rep", [128, NREP])
    cst_d = din("cst", [128, NCST])
    bc_d = din("bc", [128, 14])
    w_in_d = din("w_in", [DM, IN0])
    wdu_d = din("wdu", [64, 512])
    wiu_d = din("wiu", [64, 512])
    wgu_d = din("wgu", [128, 512])
    w_out_d = din("w_out", [DM, DM])
    ffg_d = din("ffg", [DM, FF0])
    ffu_d = din("ffu", [DM, FF0])
    ffd_d = din("ffd", [FF0, DM])
    wqkv_d = din("wqkv", [DM, 1536])
    wo_d = din("wo", [DM, DM])
    bq_d = din("bq", [128, 1536])
    pos_d = din("pos", [128, NOWN], I32)
    wr_d = din("wr", [DM, NEXP])
    eg_d = din("eg", [NEXP, DM, FFE])
    eu_d = din("eu", [NEXP, DM, FFE])
    ed_d = din("ed", [NEXP, FFE, DM])
    out_d = nc.dram_tensor("outT", [128, 8, 2048], F32, kind="ExternalOutput").ap()
    dbg_d = None
    if debug:
        dbg_d = nc.dram_tensor("dbg", [128, 8, TOWN], F32, kind="ExternalOutput").ap()

    V = {'dve': nc.vector, 'pool': nc.gpsimd}
    rr = {'el': 0, 'ev': 0, 'ph': 0, 'pb': 0, 'dq': 0}

    def el():
        rr['el'] ^= 1
        return 'dve' if rr['el'] else 'pool'

    def ev():
        rr['ev'] ^= 1
        return 'dve' if rr['ev'] else 'act'

    def aps(*xs):
        return [x for x in xs if hasattr(x, 'tensor')]

    def mm(out, lhsT, rhs, start=True, stop=True):
        kb.op('pe', lambda: nc.tensor.matmul(out, lhsT=lhsT, rhs=rhs, start=start, stop=stop), [lhsT, rhs], [out], inc=bool(stop))

    def tr(out, in_, ident):
        kb.op('pe', lambda: nc.tensor.transpose(out, in_, ident), [in_, ident], [out])

    def tt(e, out, a, b, op):
        kb.op(e, lambda: V[e].tensor_tensor(out=out, in0=a, in1=b, op=op), [a, b], [out])

    def ts(e, out, a, s1, op0, s2=None, op1=None):
        if op1 is None:
            kb.op(e, lambda: V[e].tensor_scalar(out=out, in0=a, scalar1=s1, scalar2=None, op0=op0), aps(a, s1), [out])
        else:
            kb.op(e, lambda: V[e].tensor_scalar(out=out, in0=a, scalar1=s1, scalar2=s2, op0=op0, op1=op1),
                  aps(a, s1, s2), [out])

    def stt(e, out, a, s, b, op0, op1):
        e = 'dve'
        kb.op(e, lambda: V[e].scalar_tensor_tensor(out=out, in0=a, scalar=s, in1=b, op0=op0, op1=op1),
              aps(a, s, b), [out])

    def act(out, in_, func, bias=None, scale=1.0):
        if bias is None:
            kb.op('act', lambda: nc.scalar.activation(out=out, in_=in_, func=func, scale=scale), aps(in_, scale), [out])
        else:
            kb.op('act', lambda: nc.scalar.activation(out=out, in_=in_, func=func, bias=bias, scale=scale),
                  aps(in_, bias, scale), [out])

    def rsq(x):
        act(x, x, AF.Ln)
        act(x, x, AF.Exp, scale=-0.5)

    def cp(e, out, in_):
        if e == 'act':
            kb.op('act', lambda: nc.scalar.copy(out=out, in_=in_), [in_], [out])
        else:
            kb.op(e, lambda: V[e].tensor_copy(out=out, in_=in_), [in_], [out])

    def rsum(e, out, in_):
        kb.op(e, lambda: V[e].reduce_sum(out=out, in_=in_, axis=AX.X), [in_], [out])

    def dma(out, in_, q=None):
        kb.dma(q or 'sp', out, in_)

    def sb(name, shape, dt=F32):
        return nc.alloc_sbuf_tensor(name, list(shape), dt)

    BANKS = [nc.alloc_psum_tensor("PS%d" % i, [128, 512], F32) for i in range(8)]
    for b_ in BANKS:
        kb.split[b_.name] = (512, 512, 512)
    def pb():
        rr['pb'] = (rr['pb'] + 1) % 8
        return BANKS[rr['pb']][:, :]

    def ph():
        return pb()[:, 0:256]

    PRM = sb("PRM", [128, NPRM])
    CST = sb("CST", [128, NCST])
    mix_scr = nc.dram_tensor("mix_scr", [128, 8, TOWN], BF16).ap()
    dma(PRM[:], prm_d)
    dma(CST[:], cst_d)
    IDENT = CST[:, C_ID:C_ID + 128]
    MUI = CST[:, C_MUI:C_MUI + 128]
    TRI2 = CST[:, C_MUI:C_MUI + 256]
    MU2 = CST[:, C_MUS:C_MUS + 256]
    SLm = CST[:, C_SL:C_SL + 128]
    ONES = CST[:, C_ONES:C_ONES + 128]
    BLK = CST[:, C_BLK:C_BLK + 128]
    HSEL = CST[:, C_HSEL:C_HSEL + 2]

    def P_(n, w=1, o=0):
        return PRM[:, PC[n] + o:PC[n] + o + w]

    def R_(n, w, o=0):
        return REP[:, RC[n] + o:RC[n] + o + w]

    p1 = []

    def sb1(name, shape, dt=F32):
        t = nc.sbuf_tensor(name, list(shape), dt)
        h = t.__enter__()
        p1.append(t)
        return h

    REP = sb1("REP", [128, NREP])
    dma(REP[:], rep_d)
    WIN = sb1("WIN", [128, 8, IN0], BF16)
    for kc in range(8):
        kb.dma('pool', WIN[:, kc, :], w_in_d[kc * 128:(kc + 1) * 128, :])
    WDU = sb1("WDU", [64, 512], BF16)
    kb.dma('pool', WDU[:], wdu_d)
    WIU = sb1("WIU", [128, 512], BF16)
    kb.dma('pool', WIU[64:128, :], wiu_d)
    WGU = sb1("WGU", [128, 512], BF16)
    kb.dma('pool', WGU[:], wgu_d)
    MUC = sb1("MUC", [128, 14])
    dma(MUC[:], bc_d)
    IDB = sb1("IDB", [128, 128], BF16)
    cp('dve', IDB[:], IDENT)
    AREP = sb1("AREP", [128, 8])
    act(AREP[:], R_("alog", 8), AF.Exp)
    ts('dve', AREP[:], AREP[:], -1.0, ALU.mult)

    XT = sb1("XT0", [128, 8, 128])
    RSTD = sb1("RSTD", [128, 128])
    XN = sb1("XN", [128, 8, 128], BF16)
    PA = sb1("PA0", [128, 14, 129])
    XB = sb1("XB0", [128, 8, 131])
    DTMP = sb1("DTMP", [128, 14, 128])
    PL = DTMP
    SQ = DTMP
    CT2 = sb1("CT2", [128, 4, 128])
    XC = sb1("XC", [128, 4, 128])
    TW = sb1("TW", [64, 128], BF16)
    ALB = sb1("ALB", [128, 128], BF16)
    SGL = sb1("SGL", [128, 128], BF16)
    SG = sb1("SG", [128, 512])
    ICLR = sb1("ICLR", [128, 4, 128])
    KQ = sb1("KQ", [128, 4, 128])
    SQK = sb1("SQK", [128, 4, 128])
    RN = sb1("RN", [128, 4, 128])
    KKn = sb1("KKn", [128, 4, 128])
    KM = sb1("KM", [128, 4, 128])
    T1 = SQK
    Bv = KQ
    RKR = RN
    E1 = sb1("E1", [128, 4, 128])
    E0 = sb1("E0", [128, 4, 128])
    EI = sb1("EI", [128, 4, 128])
    def dbl(name, shape, dt=F32):
        return [sb1("%s_%d" % (name, i), shape, dt) for i in range(2)]
    AR = dbl("AR", [128, 4, 2, 128], BF16)
    KT = dbl("KT", [128, 4, 128], BF16)
    BT = dbl("BT", [128, 4, 128], BF16)
    VTOK = dbl("VTOK", [128, 4, 128], BF16)
    A0T = dbl("A0T", [128, 4, 128], BF16)
    R0T = dbl("R0T", [128, 4, 128], BF16)
    KTT = dbl("KTT", [128, 4, 128], BF16)
    BTT = dbl("BTT", [128, 4, 128], BF16)
    G127 = dbl("G127", [128, 4])
    GOUT = dbl("GOUT", [128, 512])
    BON = dbl("BON", [128, 8])
    ZS = dbl("ZS", [128, 512])
    XSTOK = dbl("XSTOK", [128, 512])
    BTOK = dbl("BTOK", [128, 256])
    XBC = dbl("XBC", [128, 4, 128])
    DT = dbl("DT", [128, 8])
    DA = dbl("DA", [128, 8])
    CUMC = dbl("CUMC", [128, 8])
    NN = [[sb1("NN%d_%d" % (hb, i), [128, 4, 128], BF16) for i in range(2)] for hb in range(2)]
    PP = [[sb1("PP%d_%d" % (hb, i), [128, 4, 128], BF16) for i in range(2)] for hb in range(2)]
    TTb = [sb1("TTb%d" % hb, [128, 4, 128], BF16) for hb in range(2)]
    ARB = [sb1("ARB%d" % hb, [128, 4, 128], BF16) for hb in range(2)]
    KAb = [sb1("KAb%d" % hb, [128, 4, 256], BF16) for hb in range(2)]
    AXb = [sb1("AXb%d" % hb, [128, 4, 128], BF16) for hb in range(2)]
    WW = sb1("WW", [128, 4, 128], BF16)
    UU = sb1("UU", [128, 4, 128], BF16)
    QTOK = sb1("QTOK", [128, 4, 128], BF16)
    Y0 = sb1("Y0", [128, 4, 128])
    QT = sb1("QT", [128, 4, 128], BF16)
    MT = sb1("MT", [128, 4, 128], BF16)
    LL = sb1("LL", [128, 4, 64])
    HS = [sb1("HS%d" % i, [128, 4, 64]) for i in range(2)]
    HSb = [sb1("HSb%d" % i, [128, 4, 64], BF16) for i in range(2)]
    ST1 = sb1("ST1", [128, 8])
    ST2 = sb1("ST2", [128, 8])
    ST3 = sb1("ST3", [128, 8])
    MIXTOK = sb1("MIXTOK", [128, 1024])
    YTOK = MIXTOK[:, 0:512]
    YS = MIXTOK[:, 512:1024]
    MXS = sb1("MXS", [128, 8, 128], BF16)
    ECUM = sb1("ECUM", [128, 8])
    CDEC = sb1("CDEC", [128, 8])
    TE = sb1("TE", [128, 8])
    CBM = sb1("CBM", [128, 2, 128])
    TD4 = sb1("TD4", [128, 4, 128])
    SEG4 = TD4
    LD4 = sb1("LD4", [128, 4, 128])
    WT4 = LD4
    XW = sb1("XW", [128, 512])
    JUNK = sb1("JUNK", [128, 64], BF16)
    YOFF4 = sb1("YOFF4", [128, 4, 64])
    ST4 = sb1("ST4", [128, 2])
    HSS = [sb1("HSS%d" % i, [128, 8, 64]) for i in range(2)]

    kb.op('dve', lambda: nc.vector.memset(HS[0][:], 0.0), [], [HS[0][:]])
    kb.op('dve', lambda: nc.vector.memset(HSb[0][:], 0.0), [], [HSb[0][:]])
    kb.op('dve', lambda: nc.vector.memset(HSS[0][:], 0.0), [], [HSS[0][:]])
    kb.op('pool', lambda: nc.gpsimd.memset(PA[:], 0.0), [], [PA[:]])
    kb.op('pool', lambda: nc.gpsimd.memset(XB[:], 0.0), [], [XB[:]])

    def fgrp(g):
        if g < 14:
            return g * 128
        return 1792 + 512 + (g - 14) * 128

    def trb(out, in_):
        mm(out, in_, IDB[:])

    def norm_block(c):
        act(SQ[:, 0:8, :], XT[:], AF.Square)
        ps = ph()
        for kc in range(8):
            mm(ps[:, 0:128], ONES, SQ[:, kc, :], start=(kc == 0), stop=(kc == 7))
        ts('dve', RSTD[:], ps[:, 0:128], 1.0 / DM, ALU.mult, 1e-6, ALU.add)
        rsq(RSTD[:])
        tt('pool', SQ[:, 0:8, :], XT[:], P_("g_mix0", 8).unsqueeze(2).to_broadcast([128, 8, 128]), ALU.mult)
        tt('pool', XN[:], SQ[:, 0:8, :], RSTD[:].unsqueeze(1).to_broadcast([128, 8, 128]), ALU.mult)

    def stage1(c):
        own = c >= OWN0
        par = c % 2
        pa, xb = PA, XB
        R4 = PL[:, 0:4, :]
        K4 = PL[:, 4:8, :]
        V4 = PL[:, 8:12, :]
        cp('pool', pa[:, :, 0:1], pa[:, :, 128:129])
        cp('pool', xb[:, :, 0:3], xb[:, :, 128:131])
        for g0 in range(0, 22, 4):
            gs = list(range(g0, min(g0 + 4, 22)))
            dead = {0, 1, 2, 3, 13} if c <= OWN0 - 2 else set()
            if all(g in dead for g in gs):
                continue
            ps = pb()
            for i, g in enumerate(gs):
                if g in dead:
                    continue
                co = fgrp(g)
                for kc in range(8):
                    mm(ps[:, i * 128:(i + 1) * 128], WIN[:, kc, co:co + 128], XN[:, kc, :], start=(kc == 0), stop=(kc == 7))
            pag = [g for g in gs if g < 14 and g not in dead]
            xbg = [g for g in gs if g >= 14]
            if pag:
                i0 = gs.index(pag[0])
                cp('act', pa[:, pag[0]:pag[-1] + 1, 1:129], ps[:, i0 * 128:(i0 + len(pag)) * 128].rearrange("p (g n) -> p g n", g=len(pag)))
            if xbg:
                i0 = gs.index(xbg[0])
                cp('act', xb[:, xbg[0] - 14:xbg[-1] - 13, 3:131], ps[:, i0 * 128:(i0 + len(xbg)) * 128].rearrange("p (g n) -> p g n", g=len(xbg)))
            yield
        if own:
            psz = pb()
            for kc in range(8):
                mm(psz[:, :], XN[:, kc, :], WIN[:, kc, 1792:1792 + 512], start=(kc == 0), stop=(kc == 7))
            act(ZS[par][:], psz[:, :], AF.Silu)
        psd = ph()
        for kc in range(8):
            mm(psd[:, 0:8], XN[:, kc, :], WIN[:, kc, IN0 - 8:IN0], start=(kc == 0), stop=(kc == 7))
        tt('dve', DT[par][:], psd[:, 0:8], R_("dtb", 8), ALU.add)
        if c + 1 < NCH:
            dma(XT[:], xT_d[:, :, (c + 1) * 128:(c + 2) * 128])
        yield

        def prep_rwkv():
            for (ga, gb) in ((12, 14), (4, 8), (0, 4), (8, 12)):
                tt('pool', DTMP[:, ga:gb, :], pa[:, ga:gb, 0:128], pa[:, ga:gb, 1:129], ALU.subtract)
                tt('pool', DTMP[:, ga:gb, :], DTMP[:, ga:gb, :], MUC[:, ga:gb].unsqueeze(2).to_broadcast([128, gb - ga, 128]), ALU.mult)
                tt('pool', PL[:, ga:gb, :], DTMP[:, ga:gb, :], pa[:, ga:gb, 1:129], ALU.add)
                if ga == 12:
                    act(TW[:], PL[0:64, 12, :], AF.Tanh)
                    cp('dve', ALB[64:128, :], PL[64:128, 12, :])
                    act(SGL[:], PL[:, 13, :], AF.Sigmoid)
                if ga == 4:
                    tt('pool', KQ[:], K4, P_('kk', 4).unsqueeze(2).to_broadcast([128, 4, 128]), ALU.mult)
                    tt('pool', SQK[:], KQ[:], KQ[:], ALU.mult)
                yield
            psw = pb()
            mm(psw[:, :], TW[:], WDU[:])
            tt('dve', SG[:], psw[:, :], R_("w0", 512), ALU.add)
            act(SG[:], SG[:], AF.Sigmoid)
            if own:
                psg = pb()
                mm(psg[:, :], SGL[:], WGU[:])
                cp('act', GOUT[par][:], psg[:, :])
            psi = pb()
            for p in range(4):
                mm(psi[:, p * 128:(p + 1) * 128], WIU[64:128, p * 128:(p + 1) * 128], ALB[64:128, :])
            for p in range(4):
                act(ICLR[:, p, :], psi[:, p * 128:(p + 1) * 128], AF.Sigmoid, bias=P_("a0", 1, p))
            pss = pb()
            mm(pss[:, :], BLK, SQK[:].rearrange("p a b -> p (a b)"))
            ts('dve', RN[:].rearrange("p a b -> p (a b)"), pss[:, :], 1e-12, ALU.add)
            yield
            rsq(RN[:].rearrange("p a b -> p (a b)"))
            ts('pool', T1[:], ICLR[:], -1.0, ALU.add)
            tt('pool', T1[:], T1[:], P_('ka', 4).unsqueeze(2).to_broadcast([128, 4, 128]), ALU.mult)
            tt('pool', T1[:], T1[:], K4, ALU.mult)
            for p in range(4):
                psc = ph()
                mm(psc[:, 0:256], SG[:, p * 128:(p + 1) * 128], TRI2)
                act(E1[:, p, :], psc[:, 0:128], AF.Exp, scale=-LWS)
                act(E0[:, p, :], psc[:, 128:256], AF.Exp, scale=-LWS)
                act(EI[:, p, :], psc[:, 0:128], AF.Exp, scale=LWS)
            cp('act', G127[par][:], E1[:, :, 127])
            yield
            tt('pool', KKn[:], KQ[:], RN[:], ALU.mult)
            tt('pool', KM[:], T1[:], K4, ALU.add)
            tt('pool', AR[par][:, :, 1, :], R4, E1[:], ALU.mult)
            yield
            tt('pool', Bv[:], KKn[:], ICLR[:], ALU.mult)
            stt('dve', AR[par][:, :, 0, :], KKn[:], -1.0, E0[:], ALU.mult, ALU.mult)
            tt('pool', KT[par][:], KM[:], EI[:], ALU.mult)
            tt('pool', BT[par][:], Bv[:], EI[:], ALU.mult)
            if own:
                tt('pool', RKR[:], R4, KM[:], ALU.mult)
                tt('pool', RKR[:], RKR[:], P_('rk', 4).unsqueeze(2).to_broadcast([128, 4, 128]), ALU.mult)
            yield
            for p in range(4):
                pst = ph()
                tr(pst[:, 0:128], V4[:, p, :], IDENT)
                cp('act', VTOK[par][:, p, :], pst[:, 0:128])
                for src, dst in ((AR[par][:, p, 0, :], A0T[par]), (AR[par][:, p, 1, :], R0T[par]), (KT[par][:, p, :], KTT[par]),
                                 (BT[par][:, p, :], BTT[par])):
                    pst = ph()
                    trb(pst[:, 0:128], src)
                    cp(ev(), dst[:, p, :], pst[:, 0:128])
                if p % 2 == 1:
                    yield
            if own:
                psb = ph()
                for p in range(4):
                    mm(psb[:, 2 * p:2 * p + 2], RKR[:, p, :], HSEL)
                cp('act', BON[par][:], psb[:, 0:8])

        def prep_ssd():
            CW3 = PRM[:, PC["conv_w"]:PC["conv_w"] + 32].rearrange("p (g j) -> p g j", j=4)
            for half in range(2):
                gsl = slice(4 * half, 4 * half + 4)
                c0 = XC[:, :, :] if half == 0 else XBC[par][:, :, :]
                c1 = CT2[:, :, :]
                tt('pool', c0, xb[:, gsl, 0:128], CW3[:, gsl, 0].unsqueeze(2).to_broadcast([128, 4, 128]), ALU.mult)
                for j in range(1, 4):
                    tt('pool', c1, xb[:, gsl, j:j + 128], CW3[:, gsl, j].unsqueeze(2).to_broadcast([128, 4, 128]), ALU.mult)
                    tt('pool', c0, c0, c1, ALU.add)
                yield
            for g in range(8):
                dst = XC[:, g, :] if g < 4 else XBC[par][:, g - 4, :]
                act(dst, dst, AF.Silu, bias=P_("conv_b", 1, g))
            act(DT[par][:], DT[par][:], AF.Exp)
            act(DT[par][:], DT[par][:], AF.Ln, bias=1.0)
            tt('dve', DA[par][:], DT[par][:], AREP[:], ALU.mult)
            yield
            yield
            psc = ph()
            mm(psc[:, 0:8], MUI, DA[par][:])
            cp('act', CUMC[par][:], psc[:, 0:8])
            for g in range(4):
                pst = ph()
                tr(pst[:, 0:128], XC[:, g, :], IDENT)
                cp('act', XSTOK[par][:, g * 128:(g + 1) * 128], pst[:, 0:128])
            for g in range(2):
                pst = ph()
                tr(pst[:, 0:128], XBC[par][:, g, :], IDENT)
                cp('act', BTOK[par][:, g * 128:(g + 1) * 128], pst[:, 0:128])
            yield

        for _ in parallel(prep_rwkv(), prep_ssd()):
            yield
        if c + 1 < NCH:
            norm_block(c + 1)
        yield

    def rwkv_hb(c, hb):
        own = c >= OWN0
        par = c % 2
        ar, kt, bt, vtok, a0t, r0t = AR[par], KT[par], BT[par], VTOK[par], A0T[par], R0T[par]
        heads = list(range(4 * hb, 4 * hb + 4))
        nn, pp, ttb, arb, kab, axb = NN[hb], PP[hb], TTb[hb], ARB[hb], KAb[hb], AXb[hb]

        def h8(t):
            return t[:].rearrange("p a (h d) -> p (a h) d", h=2)[:, 4 * hb:4 * hb + 4, :]
        def par_view(t, q):
            return t[:].rearrange("p (a b) n -> p a b n", b=2)[:, :, q, :]
        for q in range(2):
            bk1 = pb()
            bk2 = pb()
            bk3 = pb()
            for i in range(2):
                h = heads[2 * i + q]
                p, hr = h // 2, slice(64 * (h % 2), 64 * (h % 2) + 64)
                rhs = ar[hr, p, :, :].rearrange("p a b -> p (a b)")
                mm(bk1[:, i * 256:(i + 1) * 256], bt[hr, p, :], rhs)
                mm(bk2[:, i * 256:(i + 1) * 256], kt[hr, p, :], rhs)
                mm(bk3[:, i * 128:(i + 1) * 128], ar[hr, p, 0, :], bt[hr, p, :])
            v1 = bk1.rearrange("p (h t n) -> p h t n", h=2, t=2)
            tt('dve', par_view(pp[0], q), v1[:, :, 0, :], MU2[:, 0:128].unsqueeze(1).to_broadcast([128, 2, 128]), ALU.mult)
            tt('dve', par_view(arb, q), v1[:, :, 1, :], MU2[:, 128:256].unsqueeze(1).to_broadcast([128, 2, 128]), ALU.mult)
            tt('dve', par_view(kab, q), bk2.rearrange("p (h n) -> p h n", h=2),
               MU2.unsqueeze(1).to_broadcast([128, 2, 256]), ALU.mult)
            tt('dve', par_view(nn[0], q), bk3[:, 0:256].rearrange("p (h n) -> p h n", h=2),
               SLm.unsqueeze(1).to_broadcast([128, 2, 128]), ALU.mult)
            yield
        tt('dve', ttb[:], pp[0][:], IDENT.unsqueeze(1).to_broadcast([128, 4, 128]), ALU.add)
        cp('act', axb[:, :, 0:64], h8(a0t))
        bkx = pb()
        for i, h in enumerate(heads):
            p, hr = h // 2, slice(64 * (h % 2), 64 * (h % 2) + 64)
            mm(bkx[:, i * 64:(i + 1) * 64], kab[:, i, 0:128], vtok[:, p, hr])
        cp('act', axb[:, :, 64:128], bkx[:, 0:256].rearrange("p (h d) -> p h d", h=4))
        yield
        ci = 0
        for lvl in range(1, 7):
            ncur, pcur, nnx, pnx = nn[ci], pp[ci], nn[1 - ci], pp[1 - ci]
            bka = pb()
            for i in range(4):
                mm(bka[:, i * 128:(i + 1) * 128], pcur[:, i, :], ncur[:, i, :])
            cp('act', nnx[:], bka.rearrange("p (h n) -> p h n", h=4))
            if lvl < 6:
                bkb = pb()
                for i in range(4):
                    mm(bkb[:, i * 128:(i + 1) * 128], ncur[:, i, :], pcur[:, i, :])
                cp('act', pnx[:], bkb.rearrange("p (h n) -> p h n", h=4))
            yield
            bkc = pb()
            for i in range(4):
                mm(bkc[:, i * 128:(i + 1) * 128], nnx[:, i, :], ttb[:, i, :])
            tt('dve', ttb[:], ttb[:], bkc.rearrange("p (h n) -> p h n", h=4), ALU.add)
            yield
            ci = 1 - ci
        bkw = pb()
        for i in range(4):
            mm(bkw[:, i * 128:(i + 1) * 128], ttb[:, i, :], axb[:, i, :])
        vw = bkw.rearrange("p (h n) -> p h n", h=4)
        cp('act', h8(WW), vw[:, :, 0:64])
        cp('dve', h8(UU), vw[:, :, 64:128])
        yield
        if own:
            bkq = pb()
            for i, h in enumerate(heads):
                p, hr = h // 2, slice(64 * (h % 2), 64 * (h % 2) + 64)
                mm(bkq[:, i * 128:i * 128 + 64], arb[:, i, :], WW[:, p, hr])
                mm(bkq[:, i * 128 + 64:(i + 1) * 128], arb[:, i, :], UU[:, p, hr], start=True, stop=False)
                mm(bkq[:, i * 128 + 64:(i + 1) * 128], kab[:, i, 128:256], vtok[:, p, hr], start=False, stop=True)
            vq = bkq.rearrange("p (h n) -> p h n", h=4)
            tt('dve', h8(QTOK), vq[:, :, 0:64], h8(r0t), ALU.add)
            cp('act', h8(Y0), vq[:, :, 64:128])
            yield

    def rwkv_pairs(c):
        own = c >= OWN0
        par = c % 2
        ar, kt, bt, vtok, a0t, r0t, ktt, btt = AR[par], KT[par], BT[par], VTOK[par], A0T[par], R0T[par], KTT[par], BTT[par]
        hs, hsn = HS[par], HS[1 - par]
        hsb, hsbn = HSb[par], HSb[1 - par]
        g127 = G127[par]
        bkm = pb()
        for p in range(4):
            mm(bkm[:, p * 128:(p + 1) * 128], IDB[:], IDB[:], start=True, stop=False)
            mm(bkm[:, p * 128:(p + 1) * 128], WW[:, p, :], btt[:, p, :], start=False, stop=True)
        tt('dve', MT[:], bkm.rearrange("p (a n) -> p a n", a=4), BLK.unsqueeze(1).to_broadcast([128, 4, 128]), ALU.mult)
        bkl = pb()
        for p in range(4):
            mm(bkl[:, p * 128:(p + 1) * 128], btt[:, p, :], UU[:, p, :], start=True, stop=False)
            mm(bkl[:, p * 128:(p + 1) * 128], ktt[:, p, :], vtok[:, p, :], start=False, stop=True)
        vl = bkl.rearrange("p (a n) -> p a n", a=4)
        for hh in range(2):
            hr = slice(64 * hh, 64 * hh + 64)
            tt('dve', LL[hr, :, :], vl[hr, :, 64 * hh:64 * hh + 64], g127[hr, :].unsqueeze(2).to_broadcast([64, 4, 64]), ALU.mult)
        yield
        if own:
            bkt = pb()
            for p in range(4):
                trb(bkt[:, p * 128:(p + 1) * 128], QTOK[:, p, :])
            cp('act', QT[:], bkt.rearrange("p (a n) -> p a n", a=4))
            Y4 = YTOK.rearrange("p (a h d) -> p a h d", a=4, h=2)
            for hh in range(2):
                hr = slice(64 * hh, 64 * hh + 64)
                bky = pb()
                for p in range(4):
                    mm(bky[:, p * 64:(p + 1) * 64], QT[hr, p, :], hsb[hr, p, :])
                tt('dve', Y4[:, :, hh, :], bky[:, 0:256].rearrange("p (a d) -> p a d", a=4), Y0[:, :, 64 * hh:64 * hh + 64], ALU.add)
            yield
        bkh = pb()
        for p in range(4):
            mm(bkh[:, p * 64:(p + 1) * 64], MT[:, p, :], hsb[:, p, :])
        tt('dve', hsn[:], bkh[:, 0:256].rearrange("p (a d) -> p a d", a=4), g127[:].unsqueeze(2).to_broadcast([128, 4, 64]), ALU.mult)
        tt('dve', hsn[:], hsn[:], LL[:], ALU.add)
        yield
        if c == 15:
            ts('dve', hsn[:], hsn[:], P_("flag"), ALU.mult)
        cp('act', hsbn[:], hsn[:])
        if own:
            Y3 = YTOK.rearrange("p (h d) -> p h d", h=8)
            rsum('dve', ST1[:], Y3)
            for h in range(8):
                kb.op('act', lambda: nc.scalar.activation(out=JUNK[:], in_=YTOK[:, h * 64:(h + 1) * 64], func=AF.Square,
                                                          accum_out=ST2[:, h:h + 1]), [YTOK], [JUNK[:], ST2[:]])
            ts('dve', ST1[:], ST1[:], 1.0 / 64, ALU.mult)
            tt('dve', ST3[:], ST1[:], ST1[:], ALU.mult)
            stt('dve', ST2[:], ST2[:], 1.0 / 64, ST3[:], ALU.mult, ALU.subtract)
            ts('dve', ST2[:], ST2[:], 64e-5, ALU.add)
            rsq(ST2[:])
            tt('dve', Y3, Y3, ST1[:].unsqueeze(2).to_broadcast([128, 8, 64]), ALU.subtract)
            tt('dve', Y3, Y3, ST2[:].unsqueeze(2).to_broadcast([128, 8, 64]), ALU.mult)
            tt('pool', YTOK, YTOK, R_("gn_w", 512), ALU.mult)
            tt('pool', YTOK, YTOK, R_("gn_b", 512), ALU.add)
            yield
            for h in range(8):
                p, hr = h // 2, slice(64 * (h % 2), 64 * (h % 2) + 64)
                stt('dve', YTOK[:, h * 64:(h + 1) * 64], vtok[:, p, hr], BON[par][:, h:h + 1], YTOK[:, h * 64:(h + 1) * 64],
                    ALU.mult, ALU.add)
            tt('pool', MIXTOK[:, 0:512], YTOK, GOUT[par][:], ALU.mult)
            yield

    def stage2_ssd(c):
        own = c >= OWN0
        par = c % 2
        dt_, da, cumc, xstok, btok, xbc = DT[par], DA[par], CUMC[par], XSTOK[par], BTOK[par], XBC[par]
        if own:
            act(ECUM[:], cumc[:], AF.Exp)
            for g in range(2):
                pcb = ph()
                mm(pcb[:, 0:128], xbc[:, g, :], xbc[:, 2 + g, :])
                tt('dve', CBM[:, g, :], pcb[:, 0:128], MUI, ALU.mult)
        hss, hssn = HSS[par], HSS[1 - par]
        for g in range(2):
            hs4 = slice(4 * g, 4 * g + 4)
            xs4 = xstok[:, g * 256:(g + 1) * 256].rearrange("p (h d) -> p h d", h=4)
            tt('dve', TD4[:], MUI.unsqueeze(1).to_broadcast([128, 4, 128]), da[:, hs4].unsqueeze(2).to_broadcast([128, 4, 128]), ALU.mult)
            bkr = pb()
            mm(bkr[:, :], ONES, TD4[:].rearrange("p h n -> p (h n)"))
            vr = bkr.rearrange("p (h n) -> p h n", h=4)
            tt('dve', SEG4[:], vr, cumc[:, hs4].unsqueeze(2).to_broadcast([128, 4, 128]), ALU.subtract)
            ts('dve', SEG4[:], SEG4[:], 0.0, ALU.add, 0.0, ALU.min)
            act(LD4[:], SEG4[:], AF.Exp)
            act(CDEC[:, hs4], vr[:, :, 127], AF.Exp)
            tt('dve', TE[:, hs4], LD4[:, :, 127], dt_[:, hs4], ALU.mult)
            tt('dve', XW[:, g * 256:(g + 1) * 256].rearrange("p (h d) -> p h d", h=4), xs4,
               TE[:, hs4].unsqueeze(2).to_broadcast([128, 4, 64]), ALU.mult)
            yield
            if own:
                tt('dve', WT4[:], LD4[:], dt_[:, hs4].unsqueeze(2).to_broadcast([128, 4, 128]), ALU.mult)
                tt('dve', WT4[:], WT4[:], CBM[:, g, :].unsqueeze(1).to_broadcast([128, 4, 128]), ALU.mult)
                bky = pb()
                for i in range(4):
                    h = 4 * g + i
                    mm(bky[:, i * 128:i * 128 + 64], WT4[:, i, :], xstok[:, h * 64:(h + 1) * 64])
                    mm(bky[:, i * 128 + 64:(i + 1) * 128], xbc[:, 2 + g, :], hss[:, h, :])
                vy = bky.rearrange("p (h n) -> p h n", h=4)
                tt('dve', YOFF4[:], vy[:, :, 64:128], ECUM[:, hs4].unsqueeze(2).to_broadcast([128, 4, 64]), ALU.mult)
                tt('dve', YS[:, g * 256:(g + 1) * 256].rearrange("p (h d) -> p h d", h=4), vy[:, :, 0:64], YOFF4[:], ALU.add)
                yield
            bks = pb()
            for i in range(4):
                h = 4 * g + i
                mm(bks[:, i * 64:(i + 1) * 64], btok[:, g * 128:(g + 1) * 128], XW[:, h * 64:(h + 1) * 64])
            tt('dve', hssn[:, hs4, :], hss[:, hs4, :], CDEC[:, hs4].unsqueeze(2).to_broadcast([128, 4, 64]), ALU.mult)
            tt('dve', hssn[:, hs4, :], hssn[:, hs4, :], bks[:, 0:256].rearrange("p (h d) -> p h d", h=4), ALU.add)
            yield
        if c == 15:
            ts('dve', hssn[:], hssn[:], P_("flag"), ALU.mult)
        if own:
            for h in range(8):
                stt('dve', YS[:, h * 64:(h + 1) * 64], xstok[:, h * 64:(h + 1) * 64], P_("dskp", 1, h), YS[:, h * 64:(h + 1) * 64],
                    ALU.mult, ALU.add)
            tt('dve', YS, YS, ZS[par][:], ALU.mult)
            tt('pool', XW[:], YS, YS, ALU.mult)
            rsum('dve', ST4[:], XW[:].rearrange("p (g d) -> p g d", g=2))
            ts('dve', ST4[:], ST4[:], 1.0 / 256, ALU.mult, 1e-6, ALU.add)
            rsq(ST4[:])
            for g in range(2):
                stt('dve', MIXTOK[:, 512 + g * 256:512 + (g + 1) * 256], YS[:, g * 256:(g + 1) * 256], ST4[:, g:g + 1],
                    R_("ssdn", 256, g * 256), ALU.mult, ALU.mult)
            yield

    def stage2_out(c):
        if c >= OWN0:
            oc = c - OWN0
            for j in range(8):
                pst = ph()
                tr(pst[:, 0:128], MIXTOK[:, j * 128:(j + 1) * 128], IDENT)
                cp('act', MXS[:, j, :], pst[:, 0:128])
            dma(mix_scr[:, :, oc * 128:(oc + 1) * 128], MXS[:])
        return
        yield

    def chain(*gens):
        for g in gens:
            for _ in g:
                yield

    def parallel(*gens):
        gens = list(gens)
        while gens:
            for g in list(gens):
                try:
                    next(g)
                except StopIteration:
                    gens.remove(g)
            yield

    def weighted(items):
        st = [[g, 0, float(n) * sp] for (g, n, sp) in items]
        while st:
            st.sort(key=lambda x: (x[1] + 1) / x[2])
            x = st[0]
            try:
                next(x[0])
                x[1] += 1
            except StopIteration:
                st.remove(x)

    dma(XT[:], xT_d[:, :, 0:128])
    norm_block(0)
    for c in range(NCH + 1):
        items = []
        if c >= 1:
            items.append((chain(parallel(rwkv_hb(c - 1, 0), rwkv_hb(c - 1, 1)), rwkv_pairs(c - 1)), 23, 1.0))
            items.append((stage2_ssd(c - 1), 7, 1.0))
        if c < NCH:
            items.append((stage1(c), 19, 1.0))
        weighted(items)
        if c >= 1:
            for _ in stage2_out(c - 1):
                pass

    kb.barrier()
    for t in reversed(p1):
        t.__exit__(None, None, None)
    if debug == "mix0":
        DBb = sb("DBb", [128, 8, TOWN], BF16)
        dma(DBb[:], mix_scr)
        DB = sb("DB", [128, 8, TOWN])
        cp('dve', DB[:], DBb[:])
        dma(dbg_d, DB[:])
        OUT = sb("OUTS", [128, 8, 2048])
        kb.op('dve', lambda: nc.vector.memset(OUT[:], 0.0), [], [OUT[:]])
        dma(out_d, OUT[:])
        kb.finish('sp')
        return nc

    TILES = [(0, 512), (512, 512), (1024, 512), (1536, 512), (2048, 128)]
    MTILES = [(128, 512), (640, 512), (1152, 512), (1664, 512)]
    HT = sb("HT", [128, 8, TOWN])
    ACTA = sb("ACTA", [128, 8, TOWN], BF16)
    WA = sb("WA", [128, 8, 1024], BF16)
    WB = sb("WB", [128, 8, 1024], BF16)
    SQT = sb("SQT", [128, 8, 128])
    RS = sb("RS", [128, 128])
    REP2 = sb("REP2", [128, 144])
    for t_ in (HT, ACTA):
        kb.split[t_.name] = (8 * TOWN, TOWN, 512)
    dma(REP2[:], rep_d[:, RC["qn"]:RC["qn"] + 144])
    dma(HT[:], xT_d[:, :, OWN0 * 128:NSLOT])
    dma(ACTA[:], mix_scr)

    def dbg_dump():
        dma(dbg_d, HT[:])
        kb.finish('sp')
        print("instructions:", kb.ninst, flush=True)
        return nc

    def rmsnorm_blk(gname, n, xnf=None):
        t0 = n * 128
        act(SQT[:], HT[:, :, t0:t0 + 128], AF.Square)
        ps = pb()
        for kc in range(8):
            mm(ps[:, 0:128], ONES, SQT[:, kc, :], start=(kc == 0), stop=(kc == 7))
        ts('dve', RS[:], ps[:, 0:128], 1.0 / DM, ALU.mult, 1e-6, ALU.add)
        rsq(RS[:])
        for kc in range(8):
            if xnf is None:
                stt('dve', ACTA[:, kc, t0:t0 + 128], HT[:, kc, t0:t0 + 128], P_(gname, 1, kc), RS[:], ALU.mult, ALU.mult)
            else:
                stt('dve', xnf[:, kc, :], HT[:, kc, t0:t0 + 128], P_(gname, 1, kc), RS[:], ALU.mult, ALU.mult)
                cp('act', ACTA[:, kc, t0:t0 + 128], xnf[:, kc, :])

    def ffn_passes(passes, WD, SGa, SGb, ACTH):
        def load_gu(ps_):
            f0, nf = ps_["f0"], ps_["nf"]
            for kc in range(8):
                kb.dma('pool', WA[:, kc, 0:nf * 128], ps_["wg"][kc * 128:(kc + 1) * 128, f0 * 128:(f0 + nf) * 128])
                kb.dma('pool', WB[:, kc, 0:nf * 128], ps_["wu"][kc * 128:(kc + 1) * 128, f0 * 128:(f0 + nf) * 128])
        load_gu(passes[0])
        for i, ps_ in enumerate(passes):
            f0, nf, tiles, rb = ps_["f0"], ps_["nf"], ps_["tiles"], ps_.get("rb")
            if ps_.get("pre") is not None:
                ps_["pre"]()
            for f in range(nf):
                kb.dma('pool', WD[:, f, :], ps_["wd"][(f0 + f) * 128:(f0 + f + 1) * 128, :])
            for (t0, tn) in tiles:
                for f in range(nf):
                    pg = pb()
                    for kc in range(8):
                        mm(pg[:, 0:tn], WA[:, kc, f * 128:(f + 1) * 128], ACTA[:, kc, t0:t0 + tn], start=(kc == 0), stop=(kc == 7))
                    pu = pb()
                    for kc in range(8):
                        mm(pu[:, 0:tn], WB[:, kc, f * 128:(f + 1) * 128], ACTA[:, kc, t0:t0 + tn], start=(kc == 0), stop=(kc == 7))
                    act(SGa[:, 0:tn], pg[:, 0:tn], AF.Silu)
                    if rb is None:
                        tt('dve', ACTH[:, f, t0:t0 + tn], pu[:, 0:tn], SGa[:, 0:tn], ALU.mult)
                    else:
                        tt('dve', SGb[:, 0:tn], pu[:, 0:tn], SGa[:, 0:tn], ALU.mult)
                        tt('dve', ACTH[:, f, t0:t0 + tn], SGb[:, 0:tn], rb[:, t0 - 128:t0 - 128 + tn], ALU.mult)
            if i + 1 < len(passes):
                load_gu(passes[i + 1])
            for (t0, tn) in tiles:
                for m in range(8):
                    ps = pb()
                    for f in range(nf):
                        mm(ps[:, 0:tn], WD[:, f, m * 128:(m + 1) * 128], ACTH[:, f, t0:t0 + tn], start=(f == 0), stop=(f == nf - 1))
                    tt('dve', HT[:, m, t0:t0 + tn], HT[:, m, t0:t0 + tn], ps[:, 0:tn], ALU.add)

    def scope():
        lst = []

        def alloc(name, shape, dt=F32):
            t = nc.sbuf_tensor(name, list(shape), dt)
            h = t.__enter__()
            lst.append(t)
            return h

        def close():
            kb.barrier()
            for t in reversed(lst):
                t.__exit__(None, None, None)
        return alloc, close

    for kc in range(8):
        kb.dma('pool', WA[:, kc, :], w_out_d[kc * 128:(kc + 1) * 128, :])
    for (t0, tn) in TILES:
        for m in range(8):
            ps = pb()
            for kc in range(8):
                mm(ps[:, 0:tn], WA[:, kc, m * 128:(m + 1) * 128], ACTA[:, kc, t0:t0 + tn], start=(kc == 0), stop=(kc == 7))
            tt('dve', HT[:, m, t0:t0 + tn], HT[:, m, t0:t0 + tn], ps[:, 0:tn], ALU.add)
    if debug == "h1":
        return dbg_dump()
    for n in range(NOWN):
        rmsnorm_blk("g_ffn0", n)
    a2, close2 = scope()
    WD = a2("WD", [128, 8, 1024], BF16)
    ACTH2 = a2("ACTH2", [128, 8, TOWN], BF16)
    kb.split[ACTH2.name] = (8 * TOWN, TOWN, 512)
    SGa = a2("SGa", [128, 512])
    SGb = a2("SGb", [128, 512])
    ffn_passes([dict(wg=ffg_d, wu=ffu_d, wd=ffd_d, f0=f0, nf=nf, tiles=TILES) for (f0, nf) in ((0, 8), (8, 8), (16, 6))],
               WD, SGa, SGb, ACTH2)
    close2()
    if debug == "h2":
        return dbg_dump()

    a3, close3 = scope()
    BQ = a3("a3_BQ", [128, 1536])
    dma(BQ[:], bq_d)
    POSI = a3("a3_POSI", [128, NOWN], I32)
    dma(POSI[:], pos_d)
    POSF = a3("a3_POSF", [128, NOWN])
    TFR = a3("a3_TFR", [128, NOWN, 8])
    TFI = a3("a3_TFI", [128, NOWN, 8], I32)
    TF2 = a3("a3_TF2", [128, NOWN, 8])
    S1 = a3("a3_S1", [128, NOWN, 8])
    COS = a3("a3_COS", [128, NOWN, 8])
    SIN = a3("a3_SIN", [128, NOWN, 8])
    FRQ = CST[:, C_FRQ:C_FRQ + 8]
    cp('dve', POSF[:], POSI[:])
    tt('dve', TFR[:], POSF[:].unsqueeze(2).to_broadcast([128, NOWN, 8]), FRQ.unsqueeze(1).to_broadcast([128, NOWN, 8]), ALU.mult)
    cp('dve', TFI[:], TFR[:])
    cp('dve', TF2[:], TFI[:])
    tt('dve', TFR[:], TFR[:], TF2[:], ALU.subtract)
    act(S1[:], TFR[:], AF.Sin, scale=float(np.pi))
    act(TF2[:], TFR[:], AF.Sin, scale=float(np.pi / 2))
    tt('dve', TF2[:], TF2[:], TF2[:], ALU.mult)
    ts('dve', TF2[:], TF2[:], -2.0, ALU.mult, 1.0, ALU.add)
    tt('dve', SIN[:], S1[:], TF2[:], ALU.mult)
    ts('dve', SIN[:], SIN[:], 2.0, ALU.mult)
    tt('dve', COS[:], S1[:], S1[:], ALU.mult)
    ts('dve', COS[:], COS[:], -2.0, ALU.mult, 1.0, ALU.add)
    QN = REP2[:, 0:64]
    KN = REP2[:, 64:128]
    SINK = REP2[:, 128:144]
    NEGB = a3("a3_NEGB", [128, 1])
    TMPB = a3("a3_TMPB", [128, 64])
    MXB = a3("a3_MXB", [128, 2])
    tt('dve', TMPB[:], QN, QN, ALU.mult)
    kb.op('dve', lambda: nc.vector.reduce_max(out=MXB[:, 0:1], in_=TMPB[:], axis=AX.X), [TMPB[:]], [MXB[:]])
    tt('dve', TMPB[:], KN, KN, ALU.mult)
    kb.op('dve', lambda: nc.vector.reduce_max(out=MXB[:, 1:2], in_=TMPB[:], axis=AX.X), [TMPB[:]], [MXB[:]])
    tt('dve', NEGB[:], MXB[:, 0:1], MXB[:, 1:2], ALU.mult)
    act(NEGB[:], NEGB[:], AF.Sqrt)
    ts('dve', NEGB[:], NEGB[:], -8.0, ALU.mult)
    ESK = a3("a3_ESK", [128, 16])
    act(ESK[:], SINK, AF.Exp, bias=NEGB[:])
    KT = a3("a3_KT", [128, 2, TOWN], BF16)
    VE = a3("a3_VE", [128, NOWN, 4, 65], BF16)
    kb.op('dve', lambda: nc.vector.memset(VE[:], 1.0), [], [VE[:]])
    QKVB = a3("a3_QKVB", [128, 1536])
    QSQ = a3("a3_QSQ", [128, 1024])
    RQ = a3("a3_RQ", [128, 20])
    RT = [a3("a3_RT%d" % i, [128, 16, 8]) for i in range(4)]
    QP = a3("a3_QP", [128, 8, 128])
    QT = a3("a3_QT", [128, 8, 128], BF16)
    PTF = a3("a3_PTF", [128, 2, 512])
    PT = a3("a3_PT", [128, 2, 512], BF16)
    DEN = a3("a3_DEN", [128, 4])
    OTOK = a3("a3_OTOK", [128, 1024])
    MASK2 = CST[:, C_M2:C_M2 + 256]
    for n in range(NOWN):
        rmsnorm_blk("g_mix1", n)
    for kc in range(8):
        kb.dma('pool', WA[:, kc, :], wqkv_d[kc * 128:(kc + 1) * 128, 0:1024])
        kb.dma('pool', WB[:, kc, 0:512], wqkv_d[kc * 128:(kc + 1) * 128, 1024:1536])

    def qk_norm_rope(e, X3, nh, gain, n, rq, sq, rts):
        sq3 = sq[:, 0:nh * 64].rearrange("p (h d) -> p h d", h=nh)
        tt(e, sq3, X3, X3, ALU.mult)
        rsum('dve', rq, sq3)
        ts(e, rq, rq, 1.0 / 64, ALU.mult, 1e-6, ALU.add)
        rsq(rq)
        tt(e, X3, X3, rq.unsqueeze(2).to_broadcast([128, nh, 64]), ALU.mult)
        tt(e, X3, X3, gain.unsqueeze(1).to_broadcast([128, nh, 64]), ALU.mult)
        c = COS[:, n, :].unsqueeze(1).to_broadcast([128, nh, 8])
        sn = SIN[:, n, :].unsqueeze(1).to_broadcast([128, nh, 8])
        x1, x2 = X3[:, :, 0:8], X3[:, :, 8:16]
        r0, r1, r2, r3 = [rts[i][:, 0:nh, :] for i in range(4)]
        tt(e, r0, x1, c, ALU.mult)
        tt(e, r1, x2, sn, ALU.mult)
        tt(e, r2, x2, c, ALU.mult)
        tt(e, r3, x1, sn, ALU.mult)
        tt(e, x1, r0, r1, ALU.subtract)
        tt(e, x2, r2, r3, ALU.add)

    QTs = [QT, a3("a3_QT1", [128, 8, 128], BF16)]
    KSQ = a3("a3_KSQ", [128, 256])
    RTK = [a3("a3_RTK%d" % i, [128, 4, 8]) for i in range(4)]

    def attn_A(n):
        t0 = n * 128
        qt = QTs[n % 2]
        cgs = (2,) if n == 0 else (2, 0, 1)
        for cg in cgs:
            ps = pb()
            for kc in range(8):
                w = WA[:, kc, cg * 512:(cg + 1) * 512] if cg < 2 else WB[:, kc, 0:512]
                mm(ps[:, :], ACTA[:, kc, t0:t0 + 128], w, start=(kc == 0), stop=(kc == 7))
            tt('dve', QKVB[:, cg * 512:(cg + 1) * 512], ps[:, :], BQ[:, cg * 512:(cg + 1) * 512], ALU.add)
            yield
        K3 = QKVB[:, 1024:1280].rearrange("p (h d) -> p h d", h=4)
        qk_norm_rope('dve', K3, 4, KN, n, RQ[:, 16:20], KSQ, RTK)
        cp('act', VE[:, n, :, 0:64], QKVB[:, 1280:1536].rearrange("p (h d) -> p h d", h=4))
        yield
        for u in range(2):
            pst = ph()
            tr(pst[:, 0:128], QKVB[:, 1024 + u * 128:1024 + (u + 1) * 128], IDENT)
            cp('act', KT[:, u, t0:t0 + 128], pst[:, 0:128])
        if n == 0:
            return
        Q3 = QKVB[:, 0:1024].rearrange("p (h d) -> p h d", h=16)
        qk_norm_rope('pool', Q3, 16, QN, n, RQ[:, 0:16], QSQ, RT)
        yield
        Q4 = QKVB[:, 0:1024].rearrange("p (kv g d) -> p kv g d", kv=4, g=4)
        for u in range(2):
            cp('pool', QP[:, u * 4:(u + 1) * 4, :].rearrange("p g (j d) -> p g j d", j=2),
               Q4[:, 2 * u:2 * u + 2, :, :].rearrange("p j g d -> p g j d"))
        yield
        for t in range(8):
            pst = ph()
            tr(pst[:, 0:128], QP[:, t, :], IDENT)
            cp('act', qt[:, t, :], pst[:, 0:128])
            if t == 3:
                yield
        yield

    def attn_B(n):
        t0 = n * 128
        qt = QTs[n % 2]
        for kv in range(4):
            u, half = kv // 2, kv % 2
            hr = slice(64 * half, 64 * half + 64)
            qrhs = qt[hr, u * 4:(u + 1) * 4, :].rearrange("p g n -> p (g n)")
            bkp = pb()
            mm(bkp[:, :], KT[hr, u, t0 - 128:t0], qrhs)
            bkc = pb()
            mm(bkc[:, :], KT[hr, u, t0:t0 + 128], qrhs)
            act(PTF[:, 0, :], bkp[:, :], AF.Exp, bias=NEGB[:], scale=0.125)
            act(PTF[:, 1, :], bkc[:, :], AF.Exp, bias=NEGB[:], scale=0.125)
            tt('dve', PT[:, 0, :].rearrange("p (g n) -> p g n", g=4), PTF[:, 0, :].rearrange("p (g n) -> p g n", g=4),
               MASK2[:, 0:128].unsqueeze(1).to_broadcast([128, 4, 128]), ALU.mult)
            tt('dve', PT[:, 1, :].rearrange("p (g n) -> p g n", g=4), PTF[:, 1, :].rearrange("p (g n) -> p g n", g=4),
               MASK2[:, 128:256].unsqueeze(1).to_broadcast([128, 4, 128]), ALU.mult)
            if n == 1:
                ts('dve', PT[:, 0, :], PT[:, 0, :], P_("flag"), ALU.mult)
            yield
            pso = pb()
            for g in range(4):
                mm(pso[:, g * 65:(g + 1) * 65], PT[:, 0, g * 128:(g + 1) * 128], VE[:, n - 1, kv, :], start=True, stop=False)
                mm(pso[:, g * 65:(g + 1) * 65], PT[:, 1, g * 128:(g + 1) * 128], VE[:, n, kv, :], start=False, stop=True)
            vo = pso[:, 0:260].rearrange("p (g d) -> p g d", g=4)
            tt('dve', DEN[:], vo[:, :, 64], ESK[:, kv * 4:(kv + 1) * 4], ALU.add)
            kb.op('dve', lambda: nc.vector.reciprocal(out=DEN[:], in_=DEN[:]), [DEN[:]], [DEN[:]])
            tt('dve', OTOK[:, kv * 256:(kv + 1) * 256].rearrange("p (g d) -> p g d", g=4), vo[:, :, 0:64],
               DEN[:].unsqueeze(2).to_broadcast([128, 4, 64]), ALU.mult)
            yield
        for j in range(8):
            pst = ph()
            tr(pst[:, 0:128], OTOK[:, j * 128:(j + 1) * 128], IDENT)
            cp('act', ACTA[:, j, t0 - 128:t0], pst[:, 0:128])
            if j == 3:
                yield
        yield

    def weighted3(items):
        st = [[g, 0, float(n_)] for (g, n_) in items]
        while st:
            st.sort(key=lambda x: (x[1] + 1) / x[2])
            x = st[0]
            try:
                next(x[0])
                x[1] += 1
            except StopIteration:
                st.remove(x)

    for n in range(NOWN + 1):
        items = []
        if n - 1 >= 1:
            items.append((attn_B(n - 1), 10))
        if n < NOWN:
            items.append((attn_A(n), 9))
        weighted3(items)
    for kc in range(8):
        kb.dma('pool', WA[:, kc, :], wo_d[kc * 128:(kc + 1) * 128, :])
    for (t0, tn) in MTILES:
        for m in range(8):
            ps = pb()
            for kc in range(8):
                mm(ps[:, 0:tn], WA[:, kc, m * 128:(m + 1) * 128], ACTA[:, kc, t0 - 128:t0 - 128 + tn], start=(kc == 0), stop=(kc == 7))
            stt('dve', HT[:, m, t0:t0 + tn], ps[:, 0:tn], P_("b_o", 1, m), HT[:, m, t0:t0 + tn], ALU.add, ALU.add)
    close3()
    if debug == "h3":
        return dbg_dump()

    a4, close4 = scope()
    WD = a4("WD4", [128, 8, 1024], BF16)
    ACTH4 = a4("ACTH4", [128, 6, TOWN], BF16)
    kb.split[ACTH4.name] = (6 * TOWN, TOWN, 512)
    SGa = a4("SGa4", [128, 512])
    SGb = a4("SGb4", [128, 512])
    XNF = a4("XNF", [128, 8, 128])
    WR = a4("WR", [128, 8, 8])
    dma(WR[:], wr_d.rearrange("(kc p) e -> p kc e", p=128))
    LG = a4("LG", [128, 8])
    MX8 = a4("MX8", [128, 8])
    NV1 = a4("NV1", [128, 1])
    MSK = a4("MSK", [128, 8])
    EX = a4("EX", [128, 8])
    SME = a4("SME", [128, 1])
    COMB = a4("COMB", [128, 16, 8])
    DG = a4("DG", [128, 128])
    RBs = [a4("RB%d" % i, [128, 2048], BF16) for i in range(2)]
    for n in range(1, NOWN):
        rmsnorm_blk("g_ffn1", n, xnf=XNF)
        ps = ph()
        for kc in range(8):
            mm(ps[:, 0:8], XNF[:, kc, :], WR[:, kc, :], start=(kc == 0), stop=(kc == 7))
        cp('dve', LG[:], ps[:, 0:8])
        kb.op('dve', lambda: nc.vector.max(out=MX8[:], in_=LG[:]), [LG[:]], [MX8[:]])
        ts('dve', MSK[:], LG[:], MX8[:, 1:2], ALU.is_ge)
        ts('dve', NV1[:], MX8[:, 0:1], -1.0, ALU.mult)
        act(EX[:], LG[:], AF.Exp, bias=NV1[:])
        tt('dve', EX[:], EX[:], MSK[:], ALU.mult)
        rsum('dve', SME[:], EX[:])
        kb.op('dve', lambda: nc.vector.reciprocal(out=SME[:], in_=SME[:]), [SME[:]], [SME[:]])
        ts('dve', COMB[:, n - 1, :], EX[:], SME[:], ALU.mult)
    def build_rb(e):
        RB = RBs[e % 2]
        for q4 in range(4):
            ps = pb()
            for b in range(4):
                blk = q4 * 4 + b
                ts('dve', DG[:], IDENT, COMB[:, blk, e:e + 1], ALU.mult)
                mm(ps[:, b * 128:(b + 1) * 128], ONES, DG[:])
            cp('act', RB[:, q4 * 512:(q4 + 1) * 512], ps[:, :])

    passes = []
    for e in range(NEXP):
        passes.append(dict(wg=eg_d[e], wu=eu_d[e], wd=ed_d[e], f0=0, nf=6, tiles=MTILES, rb=RBs[e % 2]))
        passes.append(dict(wg=eg_d[e], wu=eu_d[e], wd=ed_d[e], f0=6, nf=5, tiles=MTILES, rb=RBs[e % 2],
                           pre=(lambda e=e: build_rb(e + 1)) if e + 1 < NEXP else None))
    build_rb(0)
    ffn_passes(passes, WD, SGa, SGb, ACTH4)
    if debug == "h4":
        return dbg_dump()
    dma(out_d, HT[:, :, 128:TOWN])
    kb.finish('sp')
    print("instructions:", kb.ninst, flush=True)
    return nc


def _consts():
    i = np.arange(128)
    cst = np.zeros((128, NCST), np.float32)
    cst[:, C_ID:C_ID + 128] = np.eye(128)
    mui = (i[None, :] >= i[:, None]).astype(np.float32)
    mus = (i[None, :] > i[:, None]).astype(np.float32)
    cst[:, C_MUI:C_MUI + 128] = mui
    cst[:, C_MUS:C_MUS + 128] = mus
    cst[:, C_MUI2:C_MUI2 + 128] = mui
    cst[:, C_SL:C_SL + 128] = (i[None, :] < i[:, None])
    cst[:, C_ONES:C_ONES + 128] = 1.0
    blk = (i[None, :] // 64 == i[:, None] // 64).astype(np.float32)
    cst[:, C_BLK:C_BLK + 128] = blk
    cst[:, C_HSEL] = (i < 64)
    cst[:, C_HSEL + 1] = (i >= 64)
    cst[:, C_M2:C_M2 + 128] = (i[None, :] < i[:, None])
    cst[:, C_M2 + 128:C_M2 + 256] = mui
    inv_freq = 500000.0 ** (-np.arange(0, 16, 2, dtype=np.float64) / 16.0)
    cst[:, C_FRQ:C_FRQ + 8] = (inv_freq / (2 * np.pi))[None, :]
    return cst


def _fm(v, n):
    return np.ascontiguousarray(np.asarray(v, np.float32).reshape(n, 128).T)


def _prepare(inp):
    f = lambda k: np.asarray(inp[k], np.float32)
    prm = np.zeros((128, NPRM), np.float32)
    prm[:, PC["g_mix0"]:PC["g_mix0"] + 8] = _fm(f("ev_norm_mix")[0], 8)
    prm[:, PC["g_ffn0"]:PC["g_ffn0"] + 8] = _fm(f("ev_norm_ffn")[0], 8)
    prm[:, PC["g_mix1"]:PC["g_mix1"] + 8] = _fm(f("od_norm_mix")[0], 8)
    prm[:, PC["g_ffn1"]:PC["g_ffn1"] + 8] = _fm(f("od_norm_ffn")[0], 8)
    prm[:, PC["a0"]:PC["a0"] + 4] = _fm(f("ev_a0")[0], 4)
    cw = f("ev_conv_w")[0]
    prm[:, PC["conv_w"]:PC["conv_w"] + 32] = cw.T.reshape(8, 128, 4).transpose(1, 0, 2).reshape(128, 32)
    prm[:, PC["conv_b"]:PC["conv_b"] + 8] = _fm(f("ev_conv_b")[0], 8)
    prm[:, PC["b_qkv"]:PC["b_qkv"] + 12] = _fm(f("od_b_qkv")[0], 12)
    prm[:, PC["b_o"]:PC["b_o"] + 8] = _fm(f("od_b_o")[0], 8)
    rep = np.zeros((128, NREP), np.float32)

    def setrep(n, v):
        v = np.asarray(v, np.float32).reshape(-1)
        rep[:, RC[n]:RC[n] + v.size] = v[None, :]

    setrep("w0", f("ev_w0")[0])
    setrep("gn_w", f("ev_gn_w")[0])
    setrep("gn_b", f("ev_gn_b")[0])
    setrep("ssdn", f("ev_ssd_norm")[0])
    prm[:, PC["dskp"]:PC["dskp"] + 8] = f("ev_d_skip")[0][None, :]
    setrep("dtb", f("ev_dt_bias")[0])
    setrep("alog", f("ev_a_log")[0])
    setrep("qn", f("od_q_norm")[0])
    setrep("kn", f("od_k_norm")[0])
    setrep("sink", f("od_sinks")[0])
    bc = _fm(f("ev_mu_shift")[0], 14)
    prm[:, PC["kk"]:PC["kk"] + 4] = _fm(f("ev_k_k")[0], 4)
    prm[:, PC["ka"]:PC["ka"] + 4] = _fm(f("ev_k_a")[0], 4)
    prm[:, PC["rk"]:PC["rk"] + 4] = _fm(f("ev_r_k")[0].reshape(-1), 4)
    shared = {
        "rep": rep, "cst": _consts(), "bc": bc,
        "w_in": np.ascontiguousarray(f("ev_w_in")[0]),
        "wdu": np.ascontiguousarray(f("ev_w_decay_up")[0]),
        "wiu": np.ascontiguousarray(f("ev_w_iclr_up")[0]),
        "wgu": np.ascontiguousarray(f("ev_w_gate_up")[0]),
        "w_out": np.ascontiguousarray(f("ev_w_out")[0]),
        "ffg": np.ascontiguousarray(f("ev_ffn_gate")[0]),
        "ffu": np.ascontiguousarray(f("ev_ffn_up")[0]),
        "ffd": np.ascontiguousarray(f("ev_ffn_down")[0]),
        "wqkv": np.ascontiguousarray(f("od_w_qkv")[0]),
        "wo": np.ascontiguousarray(f("od_w_o")[0]),
        "bq": np.ascontiguousarray(np.broadcast_to(f("od_b_qkv")[0][None, :], (128, 1536))),
        "wr": np.ascontiguousarray(f("od_router")[0]),
        "eg": np.ascontiguousarray(f("od_exp_gate")[0]),
        "eu": np.ascontiguousarray(f("od_exp_up")[0]),
        "ed": np.ascontiguousarray(f("od_exp_down")[0]),
    }
    positions = np.asarray(inp["positions"]).astype(np.int32)
    x = f("x")
    maps = []
    for c in range(8):
        b, half = c // 2, c % 2
        xs = np.zeros((NSLOT, DM), np.float32)
        if half == 1:
            xs[:] = x[b]
        else:
            xs[2048:] = x[b, :2048]
        xT = np.ascontiguousarray(xs.T.reshape(8, 128, NSLOT).transpose(1, 0, 2))
        p = prm.copy()
        p[:, PC["flag"]] = float(half)
        m = dict(shared)
        m["xT"] = xT
        m["prm"] = p
        ps_ = np.zeros((NSLOT,), np.int32)
        if half == 1:
            ps_[:] = positions[b]
        else:
            ps_[2048:] = positions[b, :2048]
        m["pos"] = np.ascontiguousarray(ps_[OWN0 * 128:].reshape(NOWN, 128).T)
        maps.append(m)
    return maps


def kernel(**inputs):
    debug = os.environ.get("KDEBUG", "")
    maps = _prepare(inputs)
    nc = build_nc(debug=debug)
    res = run_bass_kernel_spmd(nc, maps, core_ids=list(range(8)))
    out = np.zeros((4, 4096, DM), np.float32)
    for c in range(8):
        b, half = c // 2, c % 2
        oT = res.results[c]["outT"]
        out[b, half * 2048:(half + 1) * 2048] = oT.transpose(2, 1, 0).reshape(2048, DM)
    if debug:
        kernel.dbg = [res.results[c]["dbg"] for c in range(8)]
    return out
```
